# Optimizing a Trainium2 kernel written in Bass

```python
import jax, jax.numpy as jnp
from jax import lax
import numpy as np

D_MODEL = 4096
BATCH = 1
SEQ = 16384
DEPTH = 4

GRID_W = 64
CTX_LEN = 256
N_MIXERS = 4
HEAD_DIM = 128
N_HEADS = D_MODEL // HEAD_DIM
N_KV_HEADS = N_HEADS // 4
GQA_REP = N_HEADS // N_KV_HEADS
WINDOW = 128
BLOCK = 128
Q_LORA = D_MODEL // 4
KV_LORA = D_MODEL // 8
QK_NOPE = 128
QK_ROPE = 64
V_HEAD = 128
CONV_W = 3
D_FF = (5 * D_MODEL) // 4
N_EXPERTS = 8
TOP_K = 2
D_FF_EXPERT = D_MODEL // 4
EPS = 1e-6
ROPE_THETA = 10000.0
NEG_INF = -1e30

kernel_name = 'hybrid_interleaved_diffusion_backbone'


def rms_norm(x, g):
    xf = x.astype(jnp.float32)
    y = xf * lax.rsqrt(jnp.mean(xf * xf, axis=-1, keepdims=True) + EPS)
    return (y * g.astype(jnp.float32)).astype(x.dtype)


def modulate(h, g, shift, scale):
    return rms_norm(h, g) * (1 + scale) + shift


def axial_rope(row, col, rot_dim):
    n_freq = rot_dim // 4
    inv = ROPE_THETA ** (-jnp.arange(n_freq, dtype=jnp.float32) / n_freq)
    ang = jnp.concatenate([row[:, None].astype(jnp.float32) * inv[None, :],
                           col[:, None].astype(jnp.float32) * inv[None, :]], axis=-1)
    return jnp.cos(ang), jnp.sin(ang)


def apply_rope(x, cos, sin):
    xp = x.reshape(x.shape[:-1] + (x.shape[-1] // 2, 2)).astype(jnp.float32)
    x1, x2 = xp[..., 0], xp[..., 1]
    bshape = (1, cos.shape[0]) + (1,) * (x.ndim - 3) + (cos.shape[1],)
    c = cos.reshape(bshape)
    s = sin.reshape(bshape)
    out = jnp.stack([x1 * c - x2 * s, x1 * s + x2 * c], axis=-1)
    return out.reshape(x.shape).astype(x.dtype)


def attend(q, k, v, scale, sink=None, mask=None):
    s = jnp.einsum('bqgrd,bkgd->bgrqk', q, k, preferred_element_type=jnp.float32) * scale
    if mask is not None:
        s = jnp.where(mask, s, NEG_INF)
    if sink is None:
        p = jax.nn.softmax(s, axis=-1)
    else:
        sk = sink.astype(jnp.float32)[None, :, :, None, None]
        m = jnp.maximum(jnp.max(s, axis=-1, keepdims=True), sk)
        e = jnp.exp(s - m)
        p = e / (jnp.sum(e, axis=-1, keepdims=True) + jnp.exp(sk - m))
    return jnp.einsum('bgrqk,bkgd->bqgrd', p.astype(v.dtype), v)


def blocked_dense_attention(q, k_lat, v_lat, k_ctx, v_ctx, scale):
    b, s, g, r, dk = q.shape
    nb = s // BLOCK
    k_all = jnp.concatenate([k_ctx, k_lat], axis=1)
    v_all = jnp.concatenate([v_ctx, v_lat], axis=1)
    qb = jnp.moveaxis(q.reshape(b, nb, BLOCK, g, r, dk), 1, 0)
    out = lax.map(lambda qi: attend(qi, k_all, v_all, scale), qb)
    return jnp.moveaxis(out, 0, 1).reshape(b, s, g, r, -1)


def gqa_project(u, w_in):
    b, n, _ = u.shape
    q, k, v = jnp.split(u @ w_in, [N_HEADS * HEAD_DIM, (N_HEADS + N_KV_HEADS) * HEAD_DIM], axis=-1)
    return (q.reshape(b, n, N_KV_HEADS, GQA_REP, HEAD_DIM),
            k.reshape(b, n, N_KV_HEADS, HEAD_DIM),
            v.reshape(b, n, N_KV_HEADS, HEAD_DIM))


def window_gqa_mixer(u_lat, u_ctx, w_in, sink, w_out, rope, with_ctx_out):
    b, s, _ = u_lat.shape
    l = u_ctx.shape[1]
    nb = s // BLOCK
    scale = HEAD_DIM ** -0.5
    sink = sink.reshape(N_KV_HEADS, GQA_REP)
    q, k, v = gqa_project(u_lat, w_in)
    q = apply_rope(q, *rope)
    k = apply_rope(k, *rope)
    qc, kc, vc = gqa_project(u_ctx, w_in)
    pad = ((0, 0), (BLOCK, BLOCK), (0, 0), (0, 0))
    kb = jnp.pad(k, pad).reshape(b, nb + 2, BLOCK, N_KV_HEADS, HEAD_DIM)
    vb = jnp.pad(v, pad).reshape(b, nb + 2, BLOCK, N_KV_HEADS, HEAD_DIM)
    kw = jnp.concatenate([kb[:, :-2], kb[:, 1:-1], kb[:, 2:]], axis=2)
    vw = jnp.concatenate([vb[:, :-2], vb[:, 1:-1], vb[:, 2:]], axis=2)
    qpos = jnp.arange(s).reshape(nb, BLOCK)
    kpos = jnp.arange(-BLOCK, s + BLOCK).reshape(nb + 2, BLOCK)
    kwin = jnp.concatenate([kpos[:-2], kpos[1:-1], kpos[2:]], axis=1)
    rel = kwin[:, None, :] - qpos[:, :, None]
    valid = (jnp.abs(rel) <= WINDOW) & (kwin[:, None, :] >= 0) & (kwin[:, None, :] < s)
    valid = jnp.concatenate([jnp.ones((nb, BLOCK, l), dtype=bool), valid], axis=-1)

    def one_block(args):
        qi, ki, vi, mi = args
        return attend(qi, jnp.concatenate([kc, ki], axis=1), jnp.concatenate([vc, vi], axis=1),
                      scale, sink=sink, mask=mi)

    qb = jnp.moveaxis(q.reshape(b, nb, BLOCK, N_KV_HEADS, GQA_REP, HEAD_DIM), 1, 0)
    o = lax.map(one_block, (qb, jnp.moveaxis(kw, 1, 0), jnp.moveaxis(vw, 1, 0), valid))
    y_lat = jnp.moveaxis(o, 0, 1).reshape(b, s, N_HEADS * HEAD_DIM) @ w_out
    y_ctx = None
    if with_ctx_out:
        y_ctx = attend(qc, kc, vc, scale, sink=sink).reshape(b, l, N_HEADS * HEAD_DIM) @ w_out
    return y_lat, y_ctx


def mla_project(u, w_in, g_q, g_kv, w_uq, w_ukv, rope):
    b, n, _ = u.shape
    cq, ckv, kr = jnp.split(u @ w_in, [Q_LORA, Q_LORA + KV_LORA], axis=-1)
    q = (rms_norm(cq, g_q) @ w_uq).reshape(b, n, N_HEADS, QK_NOPE + QK_ROPE)
    kv = (rms_norm(ckv, g_kv) @ w_ukv).reshape(b, n, N_HEADS, QK_NOPE + V_HEAD)
    q_nope, q_rot = jnp.split(q, [QK_NOPE], axis=-1)
    k_nope, v = jnp.split(kv, [QK_NOPE], axis=-1)
    kr = kr[:, :, None, :]
    if rope is not None:
        q_rot = apply_rope(q_rot, *rope)
        kr = apply_rope(kr, *rope)
    q = jnp.concatenate([q_nope, q_rot], axis=-1)[:, :, :, None, :]
    k = jnp.concatenate([k_nope, jnp.broadcast_to(kr, (b, n, N_HEADS, QK_ROPE))], axis=-1)
    return q, k, v


def mla_mixer(u_lat, u_ctx, w_in, g_q, g_kv, w_uq, w_ukv, w_out, rope, with_ctx_out):
    b, s, _ = u_lat.shape
    l = u_ctx.shape[1]
    scale = (QK_NOPE + QK_ROPE) ** -0.5
    q, k, v = mla_project(u_lat, w_in, g_q, g_kv, w_uq, w_ukv, rope)
    qc, kc, vc = mla_project(u_ctx, w_in, g_q, g_kv, w_uq, w_ukv, None)
    o = blocked_dense_attention(q, k, v, kc, vc, scale)
    y_lat = o.reshape(b, s, N_HEADS * V_HEAD) @ w_out
    y_ctx = None
    if with_ctx_out:
        y_ctx = attend(qc, kc, vc, scale).reshape(b, l, N_HEADS * V_HEAD) @ w_out
    return y_lat, y_ctx


def qknorm_gqa_mixer(u_lat, u_ctx, w_in, g_q, g_k, w_out, rope, with_ctx_out):
    b, s, _ = u_lat.shape
    l = u_ctx.shape[1]
    scale = HEAD_DIM ** -0.5
    q, k, v = gqa_project(u_lat, w_in)
    q = apply_rope(rms_norm(q, g_q), *rope)
    k = apply_rope(rms_norm(k, g_k), *rope)
    qc, kc, vc = gqa_project(u_ctx, w_in)
    qc = rms_norm(qc, g_q)
    kc = rms_norm(kc, g_k)
    o = blocked_dense_attention(q, k, v, kc, vc, scale)
    y_lat = o.reshape(b, s, N_HEADS * HEAD_DIM) @ w_out
    y_ctx = None
    if with_ctx_out:
        y_ctx = attend(qc, kc, vc, scale).reshape(b, l, N_HEADS * HEAD_DIM) @ w_out
    return y_lat, y_ctx


def short_conv_mixer(u, w_in, conv_w, w_out):
    bg, cg, v = jnp.split(u @ w_in, 3, axis=-1)
    z = lax.conv_general_dilated(cg * v, conv_w[:, None, :], window_strides=(1,),
                                 padding=((CONV_W // 2, CONV_W // 2),),
                                 dimension_numbers=('NWC', 'WIO', 'NWC'),
                                 feature_group_count=D_MODEL)
    return (bg * z) @ w_out


def swiglu(u, w_in, w_out):
    g, up = jnp.split(u @ w_in, 2, axis=-1)
    return (jax.nn.silu(g) * up) @ w_out


def moe_swiglu(u, router, w_in, w_out):
    logits = (u @ router).astype(jnp.float32)
    top_val, top_idx = lax.top_k(logits, TOP_K)
    gate = jax.nn.softmax(top_val, axis=-1)
    combine = jnp.einsum('bnk,bnke->bne', gate,
                         jax.nn.one_hot(top_idx, N_EXPERTS, dtype=jnp.float32)).astype(u.dtype)
    gu = jnp.einsum('bnd,edf->bnef', u, w_in)
    g, up = jnp.split(gu, 2, axis=-1)
    act = jax.nn.silu(g) * up * combine[..., None]
    return jnp.einsum('bnef,efd->bnd', act, w_out)


def setup_inputs(seed: int = 0) -> dict:
    key = jax.random.key(seed)
    keys = jax.random.split(key, 32)
    counter = [0]

    def nrm(shape, scale=1.0):
        k = keys[counter[0]]
        counter[0] += 1
        return jax.random.normal(k, shape, jnp.float32) * scale

    def gain(shape):
        return 1.0 + 0.02 * nrm(shape)

    d = D_MODEL
    n_win = len(range(0, DEPTH, N_MIXERS))
    n_mla = len(range(1, DEPTH, N_MIXERS))
    n_qkn = len(range(2, DEPTH, N_MIXERS))
    n_conv = len(range(3, DEPTH, N_MIXERS))
    n_dense = (DEPTH + 1) // 2
    n_moe = DEPTH // 2
    gqa_cols = (N_HEADS + 2 * N_KV_HEADS) * HEAD_DIM
    return {
        'x': nrm((BATCH, SEQ, d)),
        'c': nrm((BATCH, d)),
        'ctx': nrm((BATCH, CTX_LEN, d)),
        'c_ctx': nrm((d,)),
        'ada_w': nrm((DEPTH, d, 6 * d), 0.5 * d ** -0.5),
        'ada_b': nrm((DEPTH, 6 * d), 0.01),
        'g_mix_pre': gain((DEPTH, d)),
        'g_mix_post': gain((DEPTH, d)),
        'g_ffn_pre': gain((DEPTH, d)),
        'g_ffn_post': gain((DEPTH, d)),
        'win_w_in': nrm((n_win, d, gqa_cols), d ** -0.5),
        'win_sink': nrm((n_win, N_HEADS), 1.0),
        'win_w_out': nrm((n_win, N_HEADS * HEAD_DIM, d), (N_HEADS * HEAD_DIM) ** -0.5),
        'mla_w_in': nrm((n_mla, d, Q_LORA + KV_LORA + QK_ROPE), d ** -0.5),
        'mla_g_q': gain((n_mla, Q_LORA)),
        'mla_g_kv': gain((n_mla, KV_LORA)),
        'mla_w_uq': nrm((n_mla, Q_LORA, N_HEADS * (QK_NOPE + QK_ROPE)), Q_LORA ** -0.5),
        'mla_w_ukv': nrm((n_mla, KV_LORA, N_HEADS * (QK_NOPE + V_HEAD)), KV_LORA ** -0.5),
        'mla_w_out': nrm((n_mla, N_HEADS * V_HEAD, d), (N_HEADS * V_HEAD) ** -0.5),
        'qkn_w_in': nrm((n_qkn, d, gqa_cols), d ** -0.5),
        'qkn_g_q': gain((n_qkn, HEAD_DIM)),
        'qkn_g_k': gain((n_qkn, HEAD_DIM)),
        'qkn_w_out': nrm((n_qkn, N_HEADS * HEAD_DIM, d), (N_HEADS * HEAD_DIM) ** -0.5),
        'conv_w_in': nrm((n_conv, d, 3 * d), d ** -0.5),
        'conv_w': nrm((n_conv, CONV_W, d), CONV_W ** -0.5),
        'conv_w_out': nrm((n_conv, d, d), d ** -0.5),
        'ffn_w_in': nrm((n_dense, d, 2 * D_FF), d ** -0.5),
        'ffn_w_out': nrm((n_dense, D_FF, d), D_FF ** -0.5),
        'moe_router': nrm((n_moe, d, N_EXPERTS), d ** -0.5),
        'moe_w_in': nrm((n_moe, N_EXPERTS, d, 2 * D_FF_EXPERT), d ** -0.5),
        'moe_w_out': nrm((n_moe, N_EXPERTS, D_FF_EXPERT, d), D_FF_EXPERT ** -0.5),
    }


def reference(x, c, ctx, c_ctx, ada_w, ada_b, g_mix_pre, g_mix_post, g_ffn_pre, g_ffn_post,
              win_w_in, win_sink, win_w_out,
              mla_w_in, mla_g_q, mla_g_kv, mla_w_uq, mla_w_ukv, mla_w_out,
              qkn_w_in, qkn_g_q, qkn_g_k, qkn_w_out,
              conv_w_in, conv_w, conv_w_out,
              ffn_w_in, ffn_w_out, moe_router, moe_w_in, moe_w_out):
    b, s, _ = x.shape
    rows = s // GRID_W
    row = jnp.repeat(jnp.arange(rows, dtype=jnp.int32), GRID_W)
    col = jnp.tile(jnp.arange(GRID_W, dtype=jnp.int32), rows)
    rope_head = axial_rope(row, col, HEAD_DIM)
    rope_mla = axial_rope(row, col, QK_ROPE)

    h, hc = x, ctx
    for i in range(DEPTH):
        kind = i % N_MIXERS
        j = i // N_MIXERS
        f = i // 2
        last = i == DEPTH - 1
        mod_l = jnp.split((jax.nn.silu(c) @ ada_w[i] + ada_b[i])[:, None, :], 6, axis=-1)
        mod_c = jnp.split(jax.nn.silu(c_ctx) @ ada_w[i] + ada_b[i], 6, axis=-1)
        ctx_side = (not last) or kind != 3

        u_l = modulate(h, g_mix_pre[i], mod_l[0], mod_l[1])
        u_c = modulate(hc, g_mix_pre[i], mod_c[0], mod_c[1]) if ctx_side else None
        if kind == 0:
            y_l, y_c = window_gqa_mixer(u_l, u_c, win_w_in[j], win_sink[j], win_w_out[j],
                                        rope_head, not last)
        elif kind == 1:
            y_l, y_c = mla_mixer(u_l, u_c, mla_w_in[j], mla_g_q[j], mla_g_kv[j], mla_w_uq[j],
                                 mla_w_ukv[j], mla_w_out[j], rope_mla, not last)
        elif kind == 2:
            y_l, y_c = qknorm_gqa_mixer(u_l, u_c, qkn_w_in[j], qkn_g_q[j], qkn_g_k[j], qkn_w_out[j],
                                        rope_head, not last)
        else:
            y_l = short_conv_mixer(u_l, conv_w_in[j], conv_w[j], conv_w_out[j])
            y_c = None if last else short_conv_mixer(u_c, conv_w_in[j], conv_w[j], conv_w_out[j])
        h = h + mod_l[2] * rms_norm(y_l, g_mix_post[i])
        if not last:
            hc = hc + mod_c[2] * rms_norm(y_c, g_mix_post[i])

        u_l = modulate(h, g_ffn_pre[i], mod_l[3], mod_l[4])
        if i % 2 == 0:
            z_l = swiglu(u_l, ffn_w_in[f], ffn_w_out[f])
        else:
            z_l = moe_swiglu(u_l, moe_router[f], moe_w_in[f], moe_w_out[f])
        h = h + mod_l[5] * rms_norm(z_l, g_ffn_post[i])
        if not last:
            u_c = modulate(hc, g_ffn_pre[i], mod_c[3], mod_c[4])
            if i % 2 == 0:
                z_c = swiglu(u_c, ffn_w_in[f], ffn_w_out[f])
            else:
                z_c = moe_swiglu(u_c, moe_router[f], moe_w_in[f], moe_w_out[f])
            hc = hc + mod_c[5] * rms_norm(z_c, g_ffn_post[i])
    return h
```

```python
import numpy as np
from contextlib import ExitStack, contextmanager
import concourse.bass as bass
import concourse.mybir as mybir
from concourse.bass_utils import run_bass_kernel_spmd

F32 = mybir.dt.float32
BF16 = mybir.dt.bfloat16
AF = mybir.ActivationFunctionType
ALU = mybir.AluOpType
AX = mybir.AxisListType

ENGINES = ("pe", "act", "dve", "pool", "sp")
EMAP = {"pe": "tensor", "act": "scalar", "dve": "vector", "pool": "gpsimd", "sp": "sync"}


class Buf:
    __slots__ = ("name", "lw", "rs", "dsem", "dcnt")

    def __init__(self, name):
        self.name = name
        self.lw = None
        self.rs = {}
        self.dsem = None
        self.dcnt = 0


class Sched:
    def __init__(self, nc, stack):
        self.nc = nc
        self.stack = stack
        self.ops = {e: [] for e in ENGINES}
        self.sems = {}
        self.cnt = {}
        self.known = {e: {} for e in ENGINES}
        self.free_sems = []
        self.nsem = 0
        self.ninst = 0
        for e in ENGINES:
            self._mksem("E_" + e)

    def _mksem(self, key):
        h = self.stack.enter_context(self.nc.semaphore(key))
        self.sems[key] = h
        self.cnt[key] = 0
        self.nsem += 1
        return h

    def _deps(self, reads, writes):
        deps = {}
        for b in reads:
            if b.lw is not None:
                k, v = b.lw
                if deps.get(k, 0) < v:
                    deps[k] = v
        for b in writes:
            if b.lw is not None:
                k, v = b.lw
                if deps.get(k, 0) < v:
                    deps[k] = v
            for k, v in b.rs.items():
                if deps.get(k, 0) < v:
                    deps[k] = v
        return deps

    def _commit(self, tok, reads, writes):
        k, v = tok
        for b in reads:
            if b.rs.get(k, 0) < v:
                b.rs[k] = v
        for b in writes:
            b.lw = tok
            b.rs = {}

    def _waits(self, eng, deps):
        kn = self.known[eng]
        out = []
        for k, v in deps.items():
            if kn.get(k, 0) < v:
                kn[k] = v
                out.append((self.sems[k], v))
        return out

    def op(self, eng, fn, reads=(), writes=()):
        waits = self._waits(eng, self._deps(reads, writes))
        key = "E_" + eng
        self.cnt[key] += 1
        tok = (key, self.cnt[key])
        self.ops[eng].append((waits, fn, (self.sems[key], 1, 0)))
        self._commit(tok, reads, writes)
        self.ninst += 1 + len(waits)
        return tok

    def dma(self, eng, fn, sembuf, n=1, reads=(), writes=(), inc=16):
        deps = self._deps(reads, writes)
        if sembuf.dsem is None:
            if self.free_sems:
                sembuf.dsem = self.free_sems.pop()
            else:
                sembuf.dsem = "B%d" % self.nsem
                self._mksem(sembuf.dsem)
            sembuf.dcnt = self.cnt[sembuf.dsem]
        k = sembuf.dsem
        if self.cnt[k] and deps.get(k, 0) < self.cnt[k]:
            deps[k] = self.cnt[k]
        waits = self._waits(eng, deps)
        self.cnt[k] += inc * n
        sembuf.dcnt = self.cnt[k]
        tok = (k, self.cnt[k])
        self.ops[eng].append((waits, fn, (self.sems[k], inc, n)))
        self._commit(tok, reads, writes)
        self.ninst += n + len(waits)
        return tok

    def release(self, bufs):
        for b in bufs:
            if b.dsem is not None:
                self.free_sems.append(b.dsem)
                b.dsem = None

    def barrier(self):
        finals = {k: v for k, v in self.cnt.items() if v > 0}
        for e in ENGINES:
            waits = self._waits(e, finals)
            if waits:
                self.ops[e].append((waits, None, None))

    def finish(self):
        self.barrier()
        with self.nc.Block() as block:
            for e in ENGINES:
                ops = self.ops[e]

                def body(eng, ops=ops):
                    for waits, fn, sig in ops:
                        for s, v in waits:
                            eng.wait_ge(s, v)
                        if fn is None:
                            continue
                        r = fn(eng)
                        if sig[2] == 0:
                            r.then_inc(sig[0], sig[1])
                        else:
                            rr = r if isinstance(r, (list, tuple)) else [r]
                            assert len(rr) == sig[2], (len(rr), sig[2])
                            for x in rr:
                                x.then_inc(sig[0], sig[1])

                getattr(block, EMAP[e])(body)


class Ctx:
    def __init__(self, nc, stack):
        self.nc = nc
        self.S = Sched(nc, stack)
        self.stack = stack
        self.scopes = []
        self.uid = 0

    @contextmanager
    def phase(self):
        st = ExitStack()
        self.scopes.append((st, []))
        try:
            yield
        finally:
            self.S.barrier()
            st_, bufs = self.scopes.pop()
            self.S.release(bufs)
            st.close()

    def _name(self, name):
        self.uid += 1
        return "%s_%d" % (name, self.uid)

    def sb(self, name, shape, dtype):
        st, bufs = self.scopes[-1]
        t = st.enter_context(self.nc.sbuf_tensor(self._name(name), list(shape), dtype))
        b = Buf(name)
        bufs.append(b)
        return t, b

    def ps(self, name, shape, dtype=F32):
        st, bufs = self.scopes[-1]
        t = st.enter_context(self.nc.psum_tensor(self._name(name), list(shape), dtype))
        b = Buf(name)
        bufs.append(b)
        return t, b

    def newbuf(self, name):
        b = Buf(name)
        self.scopes[-1][1].append(b)
        return b

    def dram(self, name, shape, dtype, kind="Internal"):
        return self.nc.dram_tensor(name, list(shape), dtype, kind=kind).ap()


def ph_prenorm(C, h, uTb, gA, scA, shA, tiles, Dm, eps=1e-6, u32=None):
    S, nc = C.S, C.nc
    KC = Dm // 128
    ncls = len(scA)
    with C.phase():
        make_eps(C, eps)
        G, bG = C.sb("G", [128, Dm], F32)
        ident, bI = C.sb("ident", [128, 128], BF16)
        A, B = [], []
        for c in range(ncls):
            A.append(C.sb("A%d" % c, [128, Dm], F32))
            B.append(C.sb("B%d" % c, [128, Dm], F32))
        S.dma("sp", lambda e: [e.dma_start(out=G[:], in_=gA.partition_broadcast(128))], bG, writes=[bG])
        for c in range(ncls):
            d_, s_ = bcast_src(A[c][0][:], scA[c])
            S.dma("sp", lambda e, d_=d_, s_=s_: [e.dma_start(out=d_, in_=s_)], A[c][1], writes=[A[c][1]])
            d_, s_ = bcast_src(B[c][0][:], shA[c])
            S.dma("sp", lambda e, d_=d_, s_=s_: [e.dma_start(out=d_, in_=s_)], B[c][1], writes=[B[c][1]])
            S.op("dve", lambda e, c=c: e.scalar_tensor_tensor(out=A[c][0][:], in0=A[c][0][:], scalar=1.0,
                                                               in1=G[:], op0=ALU.add, op1=ALU.mult),
                 reads=[bG], writes=[A[c][1]])
        make_identity(C, ident, bI)
        NB = 2
        hb = [C.sb("h%d" % i, [128, Dm], F32) for i in range(NB)]
        sq = C.sb("sq", [128, Dm], BF16)
        ss = [C.sb("ss%d" % i, [128, 1], F32) for i in range(NB)]
        rs = [C.sb("rs%d" % i, [128, 1], F32) for i in range(NB)]
        ub = [C.sb("u%d" % i, [128, Dm], BF16) for i in range(NB)]
        uT = []
        for i in range(NB):
            t_, b_ = C.sb("uT%d" % i, [128, KC, 128], BF16)
            uT.append((t_, [b_] + [C.newbuf("uTg") for _ in range((KC + 3) // 4 - 1)]))
        pst = [C.ps("pst%d" % i, [128, 4, 128], BF16) for i in range(4)]
        for it, (ti, cls) in enumerate(tiles):
            i = it % NB
            ht, bh = hb[i]
            S.dma("sp", lambda e, ht=ht, ti=ti: [e.dma_start(out=ht[:], in_=h[ti * 128:(ti + 1) * 128, :])],
                  bh, writes=[bh])
            S.op("act", lambda e, i=i, ht=ht: e.activation(out=sq[0][:], in_=ht[:], func=AF.Square,
                                                           accum_out=ss[i][0][:]),
                 reads=[bh], writes=[sq[1], ss[i][1]])
            rstd_ops(C, ss[i], rs[i], Dm, eps)
            S.op("dve", lambda e, i=i, ht=ht, cls=cls: e.scalar_tensor_tensor(
                out=ht[:], in0=ht[:], scalar=rs[i][0][:], in1=A[cls][0][:], op0=ALU.mult, op1=ALU.mult),
                reads=[rs[i][1], A[cls][1]], writes=[bh])
            S.op("pool", lambda e, ht=ht, cls=cls: e.tensor_tensor(out=ht[:], in0=ht[:], in1=B[cls][0][:], op=ALU.add),
                 reads=[B[cls][1]], writes=[bh])
            if u32 is not None:
                S.dma("sp", lambda e, ht=ht, ti=ti: [e.dma_start(out=u32[ti * 128:(ti + 1) * 128, :], in_=ht[:])],
                      bh, reads=[bh])
            S.op("act", lambda e, i=i, ht=ht: e.copy(out=ub[i][0][:], in_=ht[:]), reads=[bh], writes=[ub[i][1]])
            transpose_tile(C, ub[i], uT[i], pst, ident, bI, KC)
            S.dma("sp", lambda e, i=i, ti=ti: [e.dma_start(out=uTb[ti], in_=uT[i][0][:])],
                  uT[i][1][0], reads=uT[i][1])


def bcast_src(dst, vec):
    if len(vec.shape) == 1:
        return dst, vec.partition_broadcast(128)
    a = vec.shape[0]
    return dst.rearrange("p (a b) -> p a b", a=a), vec.partition_broadcast(128)


def rstd_ops(C, ss, rs, n, eps, w=1):
    S = C.S
    if not hasattr(C, "epsb"):
        raise RuntimeError("eps tile missing")
    S.op("act", lambda e: e.activation(out=rs[0][:, 0:w], in_=ss[0][:, 0:w], func=AF.Sqrt,
                                       bias=C.epsb[0][:], scale=1.0 / n),
         reads=[ss[1], C.epsb[1]], writes=[rs[1]])
    S.op("dve", lambda e: e.reciprocal(out=rs[0][:, 0:w], in_=rs[0][:, 0:w]), reads=[rs[1]], writes=[rs[1]])


def make_eps(C, eps):
    t, b = C.sb("epsb", [128, 1], F32)
    C.S.op("pool", lambda e: e.memset(t[:], eps), writes=[b])
    C.epsb = (t, b)


def make_identity(C, ident, bI):
    S = C.S
    S.op("pool", lambda e: e.memset(ident[:], 0.0), writes=[bI])
    S.op("pool", lambda e: e.affine_select(out=ident[:], in_=ident[:], pattern=[[-1, 128]],
                                           compare_op=ALU.not_equal, fill=1.0, base=0,
                                           channel_multiplier=1),
         reads=[bI], writes=[bI])


def transpose_tile(C, src, dst, pst, ident, bI, KC, evac=("act", "dve")):
    S = C.S
    st, bs = src
    dt_, bds = dst
    ng = (KC + 3) // 4
    for g in range(ng):
        k0 = g * 4
        kn = min(4, KC - k0)
        pt, bp = pst[g % len(pst)]
        bd = bds[g]

        def tr(e, k0=k0, kn=kn, pt=pt):
            r = None
            for j in range(kn):
                r = e.transpose(pt[:, j, :], st[:, (k0 + j) * 128:(k0 + j + 1) * 128], ident[:])
            return r
        S.op("pe", tr, reads=[bs, bI], writes=[bp])
        ev = evac[g % len(evac)]
        if ev == "act":
            S.op("act", lambda e, k0=k0, kn=kn, pt=pt: e.copy(out=dt_[:, k0:k0 + kn, :], in_=pt[:, 0:kn, :]),
                 reads=[bp], writes=[bd])
        else:
            S.op("dve", lambda e, k0=k0, kn=kn, pt=pt: e.tensor_copy(out=dt_[:, k0:k0 + kn, :], in_=pt[:, 0:kn, :]),
                 reads=[bp], writes=[bd])


class WLoader:
    def __init__(self, C, KC, nblk=2, KP=4, nstage=3, width=512, cast_engs=("pool",)):
        self.C, self.KC, self.KP = C, KC, min(KP, KC)
        self.width = width
        self.np_ = (KC + self.KP - 1) // self.KP
        self.blk = []
        for i in range(nblk):
            t, b = C.sb("wb%d" % i, [128, KC, width], BF16)
            self.blk.append((t, [b] + [C.newbuf("wbp") for _ in range(self.np_ - 1)]))
        self.stage = [C.sb("wst%d" % i, [128, self.KP, width], F32) for i in range(nstage)]
        self.ib = 0
        self.ist = 0
        self.cast_engs = cast_engs
        self.ic = 0

    def load(self, Wcols):
        S = self.C.S
        t, bufs = self.blk[self.ib % len(self.blk)]
        self.ib += 1
        w = Wcols.shape[1]
        for p in range(self.np_):
            k0 = p * self.KP
            kn = min(self.KP, self.KC - k0)
            stt, stb = self.stage[self.ist % len(self.stage)]
            self.ist += 1
            src = Wcols[k0 * 128:(k0 + kn) * 128, :].rearrange("(c p) n -> p c n", p=128)
            S.dma("sp", lambda e, stt=stt, src=src, kn=kn, w=w: [e.dma_start(out=stt[:, 0:kn, 0:w], in_=src)],
                  stb, writes=[stb])
            ce = self.cast_engs[self.ic % len(self.cast_engs)]
            self.ic += 1
            S.op(ce, lambda e, t=t, stt=stt, k0=k0, kn=kn, w=w: e.tensor_copy(out=t[:, k0:k0 + kn, 0:w],
                                                                              in_=stt[:, 0:kn, 0:w]),
                 reads=[stb], writes=[bufs[p]])
        return t, bufs


def load_xT(C, xTb, tiles, KC, name="X", k0=0):
    S = C.S
    n = len(tiles)
    X, b0 = C.sb(name, [128, KC, n * 128], BF16)
    bufs = [b0] + [C.newbuf(name) for _ in range(n - 1)]
    for j, ti in enumerate(tiles):
        src = (xTb(ti) if callable(xTb) else xTb[ti])[:, k0:k0 + KC, :]
        S.dma("sp", lambda e, j=j, src=src: [e.dma_start(out=X[:, :, j * 128:(j + 1) * 128], in_=src)],
              bufs[j], writes=[bufs[j]])
    return X, bufs


def ph_linear_tok(C, xTb, W, out, groups, KC, N, out_dtype=F32, k0=0, width=512):
    S = C.S
    NB = (N + width - 1) // width

    def do_group(grp):
        with C.phase():
            X, xb = load_xT(C, xTb, grp, KC, k0=k0)
            WL = WLoader(C, KC, width=width)
            pss = [C.ps("pl%d" % i, [128, 512], F32) for i in range(4)]
            ost = [C.sb("ost%d" % i, [128, 512], out_dtype) for i in range(4)]
            it = 0
            for nb in range(NB):
                n0 = nb * width
                w = min(width, N - n0)
                wt, wbufs = WL.load(W[:, n0:n0 + w])
                for j, ti in enumerate(grp):
                    pt, pb = pss[it % 4]
                    ot, ob = ost[it % 4]

                    def mm(e, pt=pt, j=j, wt=wt, w=w):
                        r = None
                        for k in range(KC):
                            r = e.matmul(pt[:, 0:w], X[:, k, j * 128:(j + 1) * 128], wt[:, k, 0:w],
                                         start=(k == 0), stop=(k == KC - 1))
                        return r
                    S.op("pe", mm, reads=[xb[j]] + wbufs, writes=[pb])
                    if it % 2 == 0:
                        S.op("act", lambda e, ot=ot, pt=pt, w=w: e.copy(out=ot[:, 0:w], in_=pt[:, 0:w]),
                             reads=[pb], writes=[ob])
                    else:
                        S.op("dve", lambda e, ot=ot, pt=pt, w=w: e.tensor_copy(out=ot[:, 0:w], in_=pt[:, 0:w]),
                             reads=[pb], writes=[ob])
                    S.dma("sp", lambda e, ot=ot, ti=ti, n0=n0, w=w: [e.dma_start(
                        out=out[ti * 128:(ti + 1) * 128, n0:n0 + w], in_=ot[:, 0:w])], ob, reads=[ob])
                    it += 1
    for grp in groups:
        do_group(grp)


def ph_linear_feat(C, xTb, segs, outTb, groups, KC, combT=None, k0=0, store_fn=None):
    S = C.S

    def do_group(grp):
        n = len(grp)
        ntok = n * 128
        chunks = [(c0, min(4, n - c0)) for c0 in range(0, n, 4)]
        with C.phase():
            X, xb = load_xT(C, xTb, grp, KC, k0=k0)
            glu = segs[0]["wu"] is not None
            WL = WLoader(C, KC, nblk=4 if glu else 2, width=max(sg_["wg"].shape[1] for sg_ in segs))
            psg = [C.ps("pg%d" % i, [128, 512], F32) for i in range(3)]
            psu = [C.ps("pu%d" % i, [128, 512], F32) for i in range(3)] if glu else None
            sg = [C.sb("sg%d" % i, [128, 512], F32) for i in range(2)]
            ost = [C.sb("oA%d" % i, [128, 4, 512], BF16) for i in range(2)]
            cmb = None
            cur_e = None
            if combT is not None:
                cmb = C.sb("cmb", [128, ntok], F32)
            it = 0
            io = 0
            for sgm in segs:
                w = sgm["wg"].shape[1]
                ns = w // 128
                wgt, wgb = WL.load(sgm["wg"])
                if glu:
                    wut, wub = WL.load(sgm["wu"])
                if combT is not None and sgm["e"] != cur_e:
                    cur_e = sgm["e"]
                    S.dma("sp", lambda e, ce=cur_e: [e.dma_start(
                        out=cmb[0][:], in_=combT[ce, grp[0] * 128:grp[0] * 128 + ntok].partition_broadcast(128))],
                        cmb[1], writes=[cmb[1]])
                for (c0, cn) in chunks:
                    cw = cn * 128
                    ot, ob = ost[io % 2]
                    io += 1
                    for s in range(ns):
                        pg, pgb = psg[it % 3]
                        rb = xb[c0:c0 + cn]

                        def mmg(e, pg=pg, s=s, c0=c0, cw=cw, wgt=wgt):
                            r = None
                            for k in range(KC):
                                r = e.matmul(pg[:, 0:cw], wgt[:, k, s * 128:(s + 1) * 128],
                                             X[:, k, c0 * 128:c0 * 128 + cw], start=(k == 0), stop=(k == KC - 1))
                            return r
                        S.op("pe", mmg, reads=rb + wgb, writes=[pgb])
                        if glu:
                            pu, pub = psu[it % 3]

                            def mmu(e, pu=pu, s=s, c0=c0, cw=cw, wut=wut):
                                r = None
                                for k in range(KC):
                                    r = e.matmul(pu[:, 0:cw], wut[:, k, s * 128:(s + 1) * 128],
                                                 X[:, k, c0 * 128:c0 * 128 + cw], start=(k == 0), stop=(k == KC - 1))
                                return r
                            S.op("pe", mmu, reads=rb + wub, writes=[pub])
                            sgt, sgb = sg[it % 2]
                            S.op("act", lambda e, sgt=sgt, pg=pg, cw=cw: e.activation(out=sgt[:, 0:cw], in_=pg[:, 0:cw],
                                                                                       func=AF.Silu),
                                 reads=[pgb], writes=[sgb])
                            if combT is not None:
                                S.op("pool", lambda e, sgt=sgt, c0=c0, cw=cw: e.tensor_tensor(
                                    out=sgt[:, 0:cw], in0=sgt[:, 0:cw], in1=cmb[0][:, c0 * 128:c0 * 128 + cw],
                                    op=ALU.mult), reads=[cmb[1]], writes=[sgb])
                            S.op("dve", lambda e, ot=ot, s=s, sgt=sgt, pu=pu, cw=cw: e.tensor_tensor(
                                out=ot[:, s, 0:cw], in0=sgt[:, 0:cw], in1=pu[:, 0:cw], op=ALU.mult),
                                reads=[sgb, pub], writes=[ob])
                        else:
                            if it % 2 == 0:
                                S.op("act", lambda e, ot=ot, s=s, pg=pg, cw=cw: e.copy(out=ot[:, s, 0:cw], in_=pg[:, 0:cw]),
                                     reads=[pgb], writes=[ob])
                            else:
                                S.op("dve", lambda e, ot=ot, s=s, pg=pg, cw=cw: e.tensor_copy(out=ot[:, s, 0:cw],
                                                                                              in_=pg[:, 0:cw]),
                                     reads=[pgb], writes=[ob])
                        it += 1
                    fc = sgm["fc"]

                    if store_fn is not None:
                        def st2(e, ot=ot, c0=c0, cn=cn, fc=fc, ns=ns):
                            return store_fn(e, ot, grp, c0, cn, fc, ns)
                        S.dma("sp", st2, ob, n=ns, reads=[ob])
                    else:
                        def st(e, ot=ot, c0=c0, cn=cn, fc=fc, ns=ns):
                            return [e.dma_start(out=outTb[grp[c0 + j]][:, fc:fc + ns, :],
                                                in_=ot[:, 0:ns, j * 128:(j + 1) * 128]) for j in range(cn)]
                        S.dma("sp", st, ob, n=cn, reads=[ob])
    for grp in groups:
        do_group(grp)


def ph_postnorm(C, y, h, hout, gA, gateA, tiles, Dm, eps=1e-6):
    S = C.S
    ncls = len(gateA)
    with C.phase():
        make_eps(C, eps)
        G, bG = C.sb("G", [128, Dm], F32)
        GG = [C.sb("GG%d" % c, [128, Dm], F32) for c in range(ncls)]
        S.dma("sp", lambda e: [e.dma_start(out=G[:], in_=gA.partition_broadcast(128))], bG, writes=[bG])
        for c in range(ncls):
            d_, s_ = bcast_src(GG[c][0][:], gateA[c])
            S.dma("sp", lambda e, d_=d_, s_=s_: [e.dma_start(out=d_, in_=s_)], GG[c][1], writes=[GG[c][1]])
            S.op("pool", lambda e, c=c: e.tensor_tensor(out=GG[c][0][:], in0=GG[c][0][:], in1=G[:], op=ALU.mult),
                 reads=[bG], writes=[GG[c][1]])
        NB = 2
        yb = [C.sb("y%d" % i, [128, Dm], F32) for i in range(NB)]
        hb = [C.sb("hh%d" % i, [128, Dm], F32) for i in range(NB)]
        sq = [C.sb("sq%d" % i, [128, Dm], BF16) for i in range(NB)]
        ss = [C.sb("ss%d" % i, [128, 1], F32) for i in range(NB)]
        rs = [C.sb("rs%d" % i, [128, 1], F32) for i in range(NB)]
        for it, (ti, cls) in enumerate(tiles):
            i = it % NB
            yt, by = yb[i]
            ht, bh = hb[i]
            S.dma("sp", lambda e, yt=yt, ti=ti: [e.dma_start(out=yt[:], in_=y[ti * 128:(ti + 1) * 128, :])],
                  by, writes=[by])
            S.dma("sp", lambda e, ht=ht, ti=ti: [e.dma_start(out=ht[:], in_=h[ti * 128:(ti + 1) * 128, :])],
                  bh, writes=[bh])
            S.op("act", lambda e, i=i, yt=yt: e.activation(out=sq[i][0][:], in_=yt[:], func=AF.Square,
                                                           accum_out=ss[i][0][:]),
                 reads=[by], writes=[sq[i][1], ss[i][1]])
            rstd_ops(C, ss[i], rs[i], Dm, eps)
            S.op("dve", lambda e, i=i, yt=yt, cls=cls: e.scalar_tensor_tensor(
                out=yt[:], in0=yt[:], scalar=rs[i][0][:], in1=GG[cls][0][:], op0=ALU.mult, op1=ALU.mult),
                reads=[rs[i][1], GG[cls][1]], writes=[by])
            S.op("pool", lambda e, yt=yt, ht=ht: e.tensor_tensor(out=ht[:], in0=yt[:], in1=ht[:], op=ALU.add),
                 reads=[by], writes=[bh])
            S.dma("sp", lambda e, ht=ht, ti=ti: [e.dma_start(out=hout[ti * 128:(ti + 1) * 128, :], in_=ht[:])],
                  bh, reads=[bh])


def rope_ops(C, x, bx, H, dh, cs, sn, bcs, out, bout, tmp):
    S = C.S
    hp = dh // 2
    xv = x.rearrange("p (h i two) -> p h i two", h=H, two=2)
    ov = out.rearrange("p (h i two) -> p h i two", h=H, two=2)
    xe, xo = xv[:, :, :, 0], xv[:, :, :, 1]
    oe, oo = ov[:, :, :, 0], ov[:, :, :, 1]
    cb = cs.unsqueeze(1).broadcast_to([128, H, hp])
    sb_ = sn.unsqueeze(1).broadcast_to([128, H, hp])
    (t1, b1), (t2, b2) = tmp
    t1v = t1[:, 0:H * hp].rearrange("p (h i) -> p h i", h=H)
    t2v = t2[:, 0:H * hp].rearrange("p (h i) -> p h i", h=H)
    S.op("dve", lambda e: e.tensor_tensor(out=t1v, in0=xe, in1=cb, op=ALU.mult), reads=[bx, bcs], writes=[b1])
    S.op("pool", lambda e: e.tensor_tensor(out=t2v, in0=xo, in1=sb_, op=ALU.mult), reads=[bx, bcs], writes=[b2])
    S.op("dve", lambda e: e.tensor_tensor(out=oe, in0=t1v, in1=t2v, op=ALU.subtract), reads=[b1, b2], writes=[bout])
    S.op("pool", lambda e: e.tensor_tensor(out=t1v, in0=xe, in1=sb_, op=ALU.mult), reads=[bx, bcs], writes=[b1])
    S.op("dve", lambda e: e.tensor_tensor(out=t2v, in0=xo, in1=cb, op=ALU.mult), reads=[bx, bcs], writes=[b2])
    S.op("pool", lambda e: e.tensor_tensor(out=oo, in0=t1v, in1=t2v, op=ALU.add), reads=[b1, b2], writes=[bout])


def ph_qkpost(C, qkv, qTb, kTb, vout, tiles, HQ, HK, dh, rope_cs, rope_sn, qknorm=None, eps=1e-6):
    S = C.S
    H = HQ + HK
    W = H * dh
    hp = dh // 2
    with C.phase():
        make_eps(C, eps)
        ident, bI = C.sb("ident", [128, 128], BF16)
        make_identity(C, ident, bI)
        Gqk = None
        if qknorm is not None:
            g1 = C.sb("g1", [128, 2, dh], F32)
            S.dma("sp", lambda e: [e.dma_start(out=g1[0][:, 0, :], in_=qknorm[0].partition_broadcast(128)),
                                   e.dma_start(out=g1[0][:, 1, :], in_=qknorm[1].partition_broadcast(128))],
                  g1[1], n=2, writes=[g1[1]])
            Gqk = C.sb("Gqk", [128, H, dh], F32)
            S.op("pool", lambda e: e.tensor_copy(out=Gqk[0][:, 0:HQ, :],
                                                 in_=g1[0][:, 0:1, :].broadcast_to([128, HQ, dh])),
                 reads=[g1[1]], writes=[Gqk[1]])
            S.op("pool", lambda e: e.tensor_copy(out=Gqk[0][:, HQ:H, :],
                                                 in_=g1[0][:, 1:2, :].broadcast_to([128, HK, dh])),
                 reads=[g1[1]], writes=[Gqk[1]])
        NB = 2
        xb = [C.sb("x%d" % i, [128, W + HK * dh], F32) for i in range(NB)]
        csb = [C.sb("cs%d" % i, [128, 2, hp], F32) for i in range(NB)]
        qb = [C.sb("qk%d" % i, [128, W], BF16) for i in range(NB)]
        vb = [C.sb("v%d" % i, [128, HK * dh], BF16) for i in range(NB)]
        tmp = [C.sb("rt%d" % i, [128, max(H * hp, W if qknorm is not None else 0)], F32) for i in range(2)]
        ss = C.sb("ssq", [128, H], F32)
        rs = C.sb("rsq", [128, H], F32)
        qT = []
        for i in range(NB):
            t_, b_ = C.sb("qT%d" % i, [128, H, 128], BF16)
            qT.append((t_, [b_] + [C.newbuf("qTg") for _ in range((H + 3) // 4 - 1)]))
        pst = [C.ps("pst%d" % i, [128, 4, 128], BF16) for i in range(4)]
        for it, (ti, use_rope) in enumerate(tiles):
            i = it % NB
            xt, bx = xb[i]
            S.dma("sp", lambda e, xt=xt, ti=ti: [e.dma_start(out=xt[:], in_=qkv[ti * 128:(ti + 1) * 128, :])],
                  bx, writes=[bx])
            S.op("act", lambda e, i=i, xt=xt: e.copy(out=vb[i][0][:], in_=xt[:, W:W + HK * dh]),
                 reads=[bx], writes=[vb[i][1]])
            S.dma("sp", lambda e, i=i, ti=ti: [e.dma_start(out=vout[ti * 128:(ti + 1) * 128, :], in_=vb[i][0][:])],
                  vb[i][1], reads=[vb[i][1]])
            xq = xt[:, 0:W]
            if qknorm is not None:
                sqv = tmp[0][0][:, 0:W]
                S.op("pool", lambda e, xq=xq, sqv=sqv: e.tensor_tensor(out=sqv, in0=xq, in1=xq, op=ALU.mult),
                     reads=[bx], writes=[tmp[0][1]])
                S.op("dve", lambda e, sqv=sqv: e.tensor_reduce(out=ss[0][:], in_=sqv.rearrange("p (h d) -> p h d", h=H),
                                                               axis=AX.X, op=ALU.add),
                     reads=[tmp[0][1]], writes=[ss[1]])
                rstd_ops(C, ss, rs, dh, eps, w=H)
                xq3 = xq.rearrange("p (h d) -> p h d", h=H)
                S.op("dve", lambda e, xq3=xq3: e.tensor_tensor(out=xq3, in0=xq3,
                                                               in1=rs[0][:, 0:H].unsqueeze(2).broadcast_to([128, H, dh]),
                                                               op=ALU.mult),
                     reads=[rs[1]], writes=[bx])
                S.op("pool", lambda e, xq3=xq3: e.tensor_tensor(out=xq3, in0=xq3, in1=Gqk[0][:], op=ALU.mult),
                     reads=[Gqk[1]], writes=[bx])
            if use_rope:
                ct, bc = csb[i]
                S.dma("sp", lambda e, ct=ct, ti=ti: [
                    e.dma_start(out=ct[:, 0, :], in_=rope_cs[ti * 128:(ti + 1) * 128, :]),
                    e.dma_start(out=ct[:, 1, :], in_=rope_sn[ti * 128:(ti + 1) * 128, :])], bc, n=2, writes=[bc])
                rope_ops(C, xq, bx, H, dh, ct[:, 0, :], ct[:, 1, :], bc, qb[i][0][:], qb[i][1], tmp)
            else:
                S.op("act", lambda e, i=i, xq=xq: e.copy(out=qb[i][0][:], in_=xq), reads=[bx], writes=[qb[i][1]])
            transpose_tile(C, qb[i], qT[i], pst, ident, bI, H)
            S.dma("sp", lambda e, i=i, ti=ti: [e.dma_start(out=qTb[ti], in_=qT[i][0][:, 0:HQ, :]),
                                               e.dma_start(out=kTb[ti], in_=qT[i][0][:, HQ:H, :])],
                  qT[i][1][0], n=2, reads=qT[i][1])


class AttnRes:
    def __init__(self, C):
        self.C = C
        self.psS = [C.ps("aS%d" % i, [128, 512], F32) for i in range(3)]
        self.psO = [C.ps("aO%d" % i, [128, 512], F32) for i in range(2)]
        self.psR = [C.ps("aR%d" % i, [128, 512], F32) for i in range(1)]
        self.pT = [C.sb("pT%d" % i, [128, 512], BF16) for i in range(3)]
        self.acc = [C.sb("acc%d" % i, [128, 512], F32) for i in range(2)]
        self.rcp = [C.sb("rcp%d" % i, [128, 512], F32) for i in range(2)]
        self.oo = [C.sb("oo%d" % i, [128, 512], BF16) for i in range(2)]
        self.ones = C.sb("ones", [128, 128], F32)
        C.S.op("pool", lambda e: e.memset(self.ones[0][:], 1.0), writes=[self.ones[1]])
        self.iS = 0
        self.iJ = 0


def attn_job(C, R, kbs, s_terms, v_lhsT, N, scale, out_fn, nout, sink_ap=None, sink_bufs=()):
    S = C.S
    j = R.iJ
    R.iJ += 1
    pO, bO = R.psO[j % 2]
    acc, bacc = R.acc[j % 2]
    nk = len(kbs)
    pend = []

    def emit_pv(idx, kb, pt, bpt):
        vl, vbufs = v_lhsT(kb)
        S.op("pe", lambda e: e.matmul(pO[:, 0:N], vl, pt[:, 0:N], start=(idx == 0), stop=(idx == nk - 1)),
             reads=[bpt] + list(vbufs), writes=[bO])
        if idx == 0:
            S.op("dve", lambda e: e.tensor_copy(out=acc[:, 0:N], in_=pt[:, 0:N]), reads=[bpt], writes=[bacc])
        else:
            S.op("dve", lambda e: e.tensor_tensor(out=acc[:, 0:N], in0=acc[:, 0:N], in1=pt[:, 0:N], op=ALU.add),
                 reads=[bpt], writes=[bacc])

    for idx, kb in enumerate(kbs):
        pS, bS = R.psS[R.iS % 3]
        pt, bpt = R.pT[R.iS % 3]
        R.iS += 1
        terms = s_terms(kb)

        def smm(e, terms=terms, pS=pS):
            r = None
            for ti_, (l, rr, _) in enumerate(terms):
                r = e.matmul(pS[:, 0:N], l, rr, start=(ti_ == 0), stop=(ti_ == len(terms) - 1))
            return r
        rb = []
        for (_, _, b) in terms:
            rb += list(b)
        S.op("pe", smm, reads=rb, writes=[bS])
        S.op("act", lambda e, pS=pS, pt=pt: e.activation(out=pt[:, 0:N], in_=pS[:, 0:N], func=AF.Exp, scale=scale),
             reads=[bS], writes=[bpt])
        pend.append((idx, kb, pt, bpt))
        if len(pend) > 1:
            emit_pv(*pend.pop(0))
    while pend:
        emit_pv(*pend.pop(0))
    pR, bR = R.psR[0]
    S.op("pe", lambda e: e.matmul(pR[:, 0:N], R.ones[0][:], acc[:, 0:N], start=True, stop=True),
         reads=[bacc, R.ones[1]], writes=[bR])
    rc, brc = R.rcp[j % 2]
    if sink_ap is not None:
        S.op("dve", lambda e: e.tensor_tensor(out=rc[:, 0:N], in0=pR[:, 0:N], in1=sink_ap, op=ALU.add),
             reads=[bR] + list(sink_bufs), writes=[brc])
        S.op("dve", lambda e: e.reciprocal(out=rc[:, 0:N], in_=rc[:, 0:N]), reads=[brc], writes=[brc])
    else:
        S.op("dve", lambda e: e.reciprocal(out=rc[:, 0:N], in_=pR[:, 0:N]), reads=[bR], writes=[brc])
    ot, bo = R.oo[j % 2]
    S.op("dve", lambda e: e.tensor_tensor(out=ot[:, 0:N], in0=pO[:, 0:N], in1=rc[:, 0:N], op=ALU.mult),
         reads=[bO, brc], writes=[bo])
    S.dma("sp", lambda e: out_fn(e, ot), bo, n=nout, reads=[bo])


def ph_attn_window(C, qTb, kblocks, vblocks, oTb, sink, edge, NLT, NCT, HQ, HK, scale, halo=None):
    S = C.S
    rep = HQ // HK
    NKB = NLT + 2 + NCT
    N = rep * 128
    with C.phase():
        R = AttnRes(C)
        ident, bI = C.sb("ident", [128, 128], BF16)
        make_identity(C, ident, bI)
        kT = C.sb("kT", [128, NKB, HK, 128], BF16)
        vv = C.sb("vv", [128, NKB, HK * 128], BF16)
        kl = [kb for kb in range(NKB) if kblocks[kb] is not None]
        S.dma("sp", lambda e: [e.dma_start(out=kT[0][:, kb], in_=kblocks[kb]) for kb in kl], kT[1], n=len(kl),
              writes=[kT[1]])
        S.dma("sp", lambda e: [e.dma_start(out=vv[0][:, kb], in_=vblocks[kb]) for kb in kl], vv[1], n=len(kl),
              writes=[vv[1]])
        if halo is not None:
            Hg, sel = halo
            NCc = Hg.shape[0]
            selt = C.sb("selt", [128, 2, NCc], F32)
            S.dma("sp", lambda e: [e.dma_start(out=selt[0][:], in_=sel.partition_broadcast(128))], selt[1],
                  writes=[selt[1]])
            cand = [C.sb("cand%d" % i, [128, NCc, HK * 128], BF16) for i in range(2)]
            jobs = [(kT[0][:, 0].rearrange("p h k -> p (h k)"), kT[1], 1, 0),
                    (kT[0][:, NLT + 1].rearrange("p h k -> p (h k)"), kT[1], 0, 1),
                    (vv[0][:, 0], vv[1], 3, 0), (vv[0][:, NLT + 1], vv[1], 2, 1)]
            for ji, (dst, bd, which, side) in enumerate(jobs):
                ct, cb_ = cand[ji % 2]
                S.dma("sp", lambda e, ct=ct, which=which: [e.dma_start(
                    out=ct[:], in_=Hg[:, which].rearrange("c p f -> p c f"))], cb_, writes=[cb_])
                for c in range(NCc):
                    if c == 0:
                        S.op("dve", lambda e, dst=dst, ct=ct, side=side: e.tensor_scalar(
                            out=dst, in0=ct[:, 0, :], scalar1=selt[0][:, side, 0:1], scalar2=None, op0=ALU.mult),
                            reads=[cb_, selt[1]], writes=[bd])
                    else:
                        S.op("dve", lambda e, dst=dst, ct=ct, side=side, c=c: e.scalar_tensor_tensor(
                            out=dst, in0=ct[:, c, :], scalar=selt[0][:, side, c:c + 1], in1=dst,
                            op0=ALU.mult, op1=ALU.add), reads=[cb_, selt[1]], writes=[bd])
        es = C.sb("es", [128, HQ], F32)
        S.dma("sp", lambda e: [e.dma_start(out=es[0][:], in_=sink.partition_broadcast(128))], es[1], writes=[es[1]])
        S.op("act", lambda e: e.activation(out=es[0][:], in_=es[0][:], func=AF.Exp), reads=[es[1]], writes=[es[1]])
        eg = C.sb("edge", [128, 2], F32)
        S.dma("sp", lambda e: [e.dma_start(out=eg[0][:], in_=edge.partition_broadcast(128))], eg[1], writes=[eg[1]])
        mk = {}
        for nm in ("prev", "next", "prev0", "nextL"):
            mk[nm] = C.sb("m" + nm, [128, rep, 128], BF16)
        mf = C.sb("mf", [128, rep, 128], F32)
        for nm, cm, pat in (("prev", 1, [[0, rep], [-1, 128]]), ("next", -1, [[0, rep], [1, 128]])):
            S.op("pool", lambda e: e.memset(mf[0][:], 0.0), writes=[mf[1]])
            S.op("pool", lambda e, cm=cm, pat=pat: e.affine_select(out=mf[0][:], in_=mf[0][:], pattern=pat,
                                                                   compare_op=ALU.is_ge, fill=-30000.0, base=0,
                                                                   channel_multiplier=cm),
                 reads=[mf[1]], writes=[mf[1]])
            S.op("dve", lambda e, nm=nm: e.tensor_copy(out=mk[nm][0][:], in_=mf[0][:]), reads=[mf[1]], writes=[mk[nm][1]])
            en, col = ("prev0", 0) if nm == "prev" else ("nextL", 1)
            S.op("dve", lambda e, en=en, col=col: e.tensor_scalar(out=mk[en][0][:], in0=mf[0][:],
                                                                  scalar1=eg[0][:, col:col + 1], scalar2=None,
                                                                  op0=ALU.add),
                 reads=[mf[1], eg[1]], writes=[mk[en][1]])
        qb = [C.sb("q%d" % i, [128, HQ, 128], BF16) for i in range(2)]
        NT = NLT + NCT
        for t in range(NT):
            qt, bq = qb[t % 2]
            S.dma("sp", lambda e, qt=qt, t=t: [e.dma_start(out=qt[:], in_=qTb[t])], bq, writes=[bq])
            if t < NLT:
                kbs = [NLT + 2 + c for c in range(NCT)] + [t, t + 1, t + 2]
            else:
                kbs = [NLT + 2 + c for c in range(NCT)]
            for g in range(HK):
                rhs = qt[:, g * rep:(g + 1) * rep, :]

                def s_terms(kb, g=g, rhs=rhs, bq=bq, t=t):
                    terms = [(kT[0][:, kb, g, :], rhs, [kT[1], bq])]
                    if t < NLT and kb == t:
                        m = mk["prev0"] if t == 0 else mk["prev"]
                        terms.append((ident[:], m[0][:], [bI, m[1]]))
                    if t < NLT and kb == t + 2:
                        m = mk["nextL"] if t == NLT - 1 else mk["next"]
                        terms.append((ident[:], m[0][:], [bI, m[1]]))
                    return terms

                def v_lhsT(kb, g=g):
                    return vv[0][:, kb, g * 128:(g + 1) * 128], [vv[1]]
                sk = es[0][:, g * rep:(g + 1) * rep].unsqueeze(2).broadcast_to([128, rep, 128])

                def out_fn(e, ot, t=t, g=g):
                    return [e.dma_start(out=oTb[t][:, g * rep:(g + 1) * rep, :], in_=ot[:, 0:N])]
                attn_job(C, R, kbs, s_terms, v_lhsT, N, scale, out_fn, 1, sink_ap=sk, sink_bufs=[es[1]])


def ph_attn_tok(C, qTb, oTb, NH, kv_of, k_loader, v_loader, NLT, NCT, NKB, scale, qrTb=None, kr_loader=None):
    S = C.S
    with C.phase():
        R = AttnRes(C)
        kT = C.sb("kT", [128, NKB * 128], BF16)
        vv = C.sb("vv", [128, NKB, 128], BF16)
        kr = None
        if kr_loader is not None:
            kr = C.sb("kr", [64, NKB * 128], BF16)
            S.dma("sp", lambda e: kr_loader[0](e, kr[0], 0), kr[1], n=kr_loader[1], writes=[kr[1]])
        qb = [C.sb("q%d" % i, [128, 512], BF16) for i in range(2)]
        qrb = [C.sb("qr%d" % i, [64, 512], BF16) for i in range(2)] if qrTb is not None else None
        jobs = [(t0, min(4, NLT - t0), None) for t0 in range(0, NLT, 4)]
        if NCT:
            jobs.append((NLT, NCT, list(range(NKB - NCT, NKB))))
        cur_kv = None
        ij = 0
        for hh in range(NH):
            kv = kv_of(hh)
            if kv != cur_kv:
                cur_kv = kv
                S.dma("sp", lambda e, kv=kv: k_loader[0](e, kT[0], kv), kT[1], n=k_loader[1], writes=[kT[1]])
                S.dma("sp", lambda e, kv=kv: v_loader[0](e, vv[0], kv), vv[1], n=v_loader[1], writes=[vv[1]])
            for (t0, cn, kbl) in jobs:
                N = cn * 128
                qt, bq = qb[ij % 2]
                S.dma("sp", lambda e, qt=qt, hh=hh, t0=t0, cn=cn, N=N: [e.dma_start(
                    out=qt[:, 0:N].rearrange("p (t k) -> p t k", k=128),
                    in_=qTb[t0:t0 + cn][:, :, hh, :].rearrange("t p k -> p t k"))], bq, writes=[bq])
                if qrTb is not None:
                    qrt, bqr = qrb[ij % 2]
                    po = (hh % 2) * 64
                    S.dma("sp", lambda e, qrt=qrt, hh=hh, t0=t0, cn=cn, N=N, po=po: [e.dma_start(
                        out=qrt[:, 0:N].rearrange("p (t k) -> p t k", k=128),
                        in_=qrTb[t0:t0 + cn][:, po:po + 64, hh // 2, :].rearrange("t p k -> p t k"))],
                        bqr, writes=[bqr])
                ij += 1
                kbs = kbl if kbl is not None else list(range(NKB))

                def s_terms(kb, qt=qt, bq=bq, N=N):
                    terms = [(kT[0][:, kb * 128:(kb + 1) * 128], qt[:, 0:N], [kT[1], bq])]
                    if qrTb is not None:
                        terms.append((kr[0][:, kb * 128:(kb + 1) * 128], qrt[:, 0:N], [kr[1], bqr]))
                    return terms

                def v_lhsT(kb):
                    return vv[0][:, kb, :], [vv[1]]

                def out_fn(e, ot, hh=hh, t0=t0, cn=cn, N=N):
                    return [e.dma_start(out=oTb[t0:t0 + cn][:, :, hh, :].rearrange("t p k -> p t k"),
                                        in_=ot[:, 0:N].rearrange("p (t k) -> p t k", k=128))]
                attn_job(C, R, kbs, s_terms, v_lhsT, N, scale, out_fn, 1)


def ph_allgather(C, src2d, dst2d, ncores):
    S = C.S
    with C.phase():
        cb = C.newbuf("cc")
        S.dma("pool", lambda e: [e.collective_compute("AllGather", ALU.bypass, replica_groups=[list(range(ncores))],
                                                      ins=[src2d], outs=[dst2d])], cb, n=1, inc=1)


def ph_copy_rows(C, pairs, width, dtype):
    S = C.S
    with C.phase():
        tb = [C.sb("cp%d" % i, [128, width], dtype) for i in range(2)]
        for i, (dst, src) in enumerate(pairs):
            t, b = tb[i % 2]
            r = src.shape[0]
            S.dma("sp", lambda e, t=t, src=src, r=r: [e.dma_start(out=t[0:r, :], in_=src)], b, writes=[b])
            S.dma("sp", lambda e, t=t, dst=dst, r=r: [e.dma_start(out=dst, in_=t[0:r, :])], b, reads=[b])


def ph_halo_rows(C, Gh, selM, hdst, ncores, Dm):
    S = C.S
    R2 = 2 * ncores
    with C.phase():
        gs = C.sb("gs", [R2, Dm], F32)
        sm = C.sb("sm", [R2, 2], F32)
        ho = C.sb("ho", [128, Dm], F32)
        S.dma("sp", lambda e: [e.dma_start(out=gs[0][:], in_=Gh)], gs[1], writes=[gs[1]])
        S.dma("sp", lambda e: [e.dma_start(out=sm[0][:], in_=selM)], sm[1], writes=[sm[1]])
        S.op("pool", lambda e: e.memset(ho[0][:], 0.0), writes=[ho[1]])
        ps = [C.ps("ph%d" % i, [128, 512], F32) for i in range(2)]
        for i, n0 in enumerate(range(0, Dm, 512)):
            pt, pb = ps[i % 2]
            S.op("pe", lambda e, pt=pt, n0=n0: e.matmul(pt[0:2, :], sm[0][:], gs[0][:, n0:n0 + 512], start=True, stop=True),
                 reads=[gs[1], sm[1]], writes=[pb])
            S.op("act", lambda e, pt=pt, n0=n0: e.copy(out=ho[0][0:2, n0:n0 + 512], in_=pt[0:2, :]),
                 reads=[pb], writes=[ho[1]])
        S.dma("sp", lambda e: [e.dma_start(out=hdst, in_=ho[0][:])], ho[1], reads=[ho[1]])


def ph_attn_full(C, qTh, kTh, vh, oTh, HL, NKV, TQL, TQC, NKB, scale, qrTh=None, krT=None, ctx_kb=()):
    S = C.S
    with C.phase():
        R = AttnRes(C)
        kT = C.sb("kT", [128, NKB * 128], BF16)
        vv = C.sb("vv", [128, NKB, 128], BF16)
        kr = None
        if krT is not None:
            kr = C.sb("kr", [64, NKB * 128], BF16)
            S.dma("sp", lambda e: [e.dma_start(out=kr[0][:], in_=krT)], kr[1], writes=[kr[1]])
        qb = [C.sb("q%d" % i, [128, 512], BF16) for i in range(2)]
        qrb = [C.sb("qr%d" % i, [64, 512], BF16) for i in range(2)] if qrTh is not None else None
        jobs = [(c0, 512, None) for c0 in range(0, TQL, 512)]
        if TQC:
            jobs.append((TQL, TQC, list(ctx_kb)))
        cur_kv = None
        ij = 0
        for hh in range(HL):
            kv = hh if NKV == HL else 0
            if kv != cur_kv:
                cur_kv = kv
                S.dma("sp", lambda e, kv=kv: [e.dma_start(out=kT[0][:], in_=kTh[kv])], kT[1], writes=[kT[1]])
                S.dma("sp", lambda e, kv=kv: [e.dma_start(out=vv[0][:], in_=vh[kv])], vv[1], writes=[vv[1]])
            for (c0, N, kbl) in jobs:
                qt, bq = qb[ij % 2]
                S.dma("sp", lambda e, qt=qt, hh=hh, c0=c0, N=N: [e.dma_start(out=qt[:, 0:N], in_=qTh[hh][:, c0:c0 + N])],
                      bq, writes=[bq])
                if qrTh is not None:
                    qrt, bqr = qrb[ij % 2]
                    S.dma("sp", lambda e, qrt=qrt, hh=hh, c0=c0, N=N: [e.dma_start(out=qrt[:, 0:N],
                                                                                  in_=qrTh[hh][:, c0:c0 + N])],
                          bqr, writes=[bqr])
                ij += 1
                kbs = kbl if kbl is not None else list(range(NKB))

                def s_terms(kb, qt=qt, bq=bq, N=N):
                    terms = [(kT[0][:, kb * 128:(kb + 1) * 128], qt[:, 0:N], [kT[1], bq])]
                    if qrTh is not None:
                        terms.append((kr[0][:, kb * 128:(kb + 1) * 128], qrt[:, 0:N], [kr[1], bqr]))
                    return terms

                def v_lhsT(kb):
                    return vv[0][:, kb, :], [vv[1]]

                def out_fn(e, ot, hh=hh, c0=c0, N=N):
                    return [e.dma_start(out=oTh[hh][:, c0:c0 + N], in_=ot[:, 0:N])]
                attn_job(C, R, kbs, s_terms, v_lhsT, N, scale, out_fn, 1)


def ph_ada(C, cT, W, b, out, L, Dm, NC):
    S = C.S
    KC = Dm // 128
    KP = min(8, KC)
    with C.phase():
        sc = C.sb("sc", [128, KC, 2], F32)
        S.dma("sp", lambda e: [e.dma_start(out=sc[0][:], in_=cT)], sc[1], writes=[sc[1]])
        S.op("act", lambda e: e.activation(out=sc[0][:], in_=sc[0][:], func=AF.Silu), reads=[sc[1]], writes=[sc[1]])
        bb = C.sb("bb", [2, L, NC], F32)
        S.dma("sp", lambda e: [e.dma_start(out=bb[0][:], in_=b.partition_broadcast(2))], bb[1], writes=[bb[1]])
        npieces = (KC + KP - 1) // KP
        wst = [C.sb("aw%d" % i, [128, KP, 512], F32) for i in range(4)]
        ps = [C.ps("pa%d" % i, [128, 512], F32) for i in range(2)]
        ob = [C.sb("ao%d" % i, [2, 512], F32) for i in range(2)]
        ist = 0
        it = 0
        for l in range(L):
            for n0 in range(0, NC, 512):
                w = min(512, NC - n0)
                pt, pb = ps[it % 2]
                for p in range(npieces):
                    k0 = p * KP
                    kn = min(KP, KC - k0)
                    wt, wb = wst[ist % 4]
                    ist += 1
                    src = W[l][k0 * 128:(k0 + kn) * 128, n0:n0 + w].rearrange("(c p) n -> p c n", p=128)
                    S.dma("sp", lambda e, wt=wt, src=src, kn=kn, w=w: [e.dma_start(out=wt[:, 0:kn, 0:w], in_=src)],
                          wb, writes=[wb])

                    def mm(e, wt=wt, pt=pt, k0=k0, kn=kn, w=w):
                        r = None
                        for k in range(kn):
                            r = e.matmul(pt[0:2, 0:w], sc[0][:, k0 + k, :], wt[:, k, 0:w],
                                         start=(k0 + k == 0), stop=(k0 + k == KC - 1))
                        return r
                    S.op("pe", mm, reads=[sc[1], wb], writes=[pb])
                ot, obb = ob[it % 2]
                S.op("dve", lambda e, ot=ot, pt=pt, l=l, n0=n0, w=w: e.tensor_tensor(
                    out=ot[:, 0:w], in0=pt[0:2, 0:w], in1=bb[0][:, l, n0:n0 + w], op=ALU.add),
                    reads=[pb, bb[1]], writes=[obb])
                S.dma("sp", lambda e, ot=ot, l=l, n0=n0, w=w: [e.dma_start(out=out[l][:, n0:n0 + w], in_=ot[:, 0:w])],
                      obb, reads=[obb])
                it += 1


def ph_mla_post(C, call, cqTb, ckrTb, g_q, g_kv, tiles, QL, KL, RD, rope_cs, rope_sn, eps=1e-6):
    S = C.S
    Wc = QL + KL
    KCc = Wc // 128
    hp = RD // 2
    with C.phase():
        make_eps(C, eps)
        ident, bI = C.sb("ident", [128, 128], BF16)
        make_identity(C, ident, bI)
        G = C.sb("G", [128, Wc], F32)
        S.dma("sp", lambda e: [e.dma_start(out=G[0][:, 0:QL], in_=g_q.partition_broadcast(128)),
                               e.dma_start(out=G[0][:, QL:Wc], in_=g_kv.partition_broadcast(128))],
              G[1], n=2, writes=[G[1]])
        NB = 2
        xb = [C.sb("x%d" % i, [128, Wc + RD], F32) for i in range(NB)]
        sq = C.sb("sq", [128, Wc], BF16)
        ss = [C.sb("ss%d" % i, [128, 2], F32) for i in range(NB)]
        rs = [C.sb("rs%d" % i, [128, 2], F32) for i in range(NB)]
        cn = [C.sb("cn%d" % i, [128, Wc], BF16) for i in range(NB)]
        krb = [C.sb("krb%d" % i, [128, 128], BF16) for i in range(NB)]
        csb = [C.sb("cs%d" % i, [128, 2, hp], F32) for i in range(NB)]
        tmp = [C.sb("rt%d" % i, [128, hp], F32) for i in range(2)]
        cT, krT = [], []
        for i in range(NB):
            t_, b_ = C.sb("cT%d" % i, [128, KCc, 128], BF16)
            cT.append((t_, [b_] + [C.newbuf("cTg") for _ in range((KCc + 3) // 4 - 1)]))
            t_, b_ = C.sb("krT%d" % i, [128, 1, 128], BF16)
            krT.append((t_, [b_]))
        pst = [C.ps("pst%d" % i, [128, 4, 128], BF16) for i in range(4)]
        for i in range(NB):
            S.op("pool", lambda e, i=i: e.memset(krb[i][0][:], 0.0), writes=[krb[i][1]])
        for it, (ti, use_rope) in enumerate(tiles):
            i = it % NB
            xt, bx = xb[i]
            S.dma("sp", lambda e, xt=xt, ti=ti: [e.dma_start(out=xt[:], in_=call[ti * 128:(ti + 1) * 128, :])],
                  bx, writes=[bx])
            S.op("act", lambda e, i=i, xt=xt: e.activation(out=sq[0][:, 0:QL], in_=xt[:, 0:QL], func=AF.Square,
                                                           accum_out=ss[i][0][:, 0:1]),
                 reads=[bx], writes=[sq[1], ss[i][1]])
            S.op("act", lambda e, i=i, xt=xt: e.activation(out=sq[0][:, QL:Wc], in_=xt[:, QL:Wc], func=AF.Square,
                                                           accum_out=ss[i][0][:, 1:2]),
                 reads=[bx], writes=[sq[1], ss[i][1]])
            S.op("dve", lambda e, i=i: e.tensor_scalar(out=ss[i][0][:, 0:1], in0=ss[i][0][:, 0:1],
                                                       scalar1=float(KL) / QL, scalar2=None, op0=ALU.mult),
                 reads=[ss[i][1]], writes=[ss[i][1]])
            rstd_ops(C, ss[i], rs[i], KL, eps, w=2)
            S.op("dve", lambda e, i=i, xt=xt: e.scalar_tensor_tensor(
                out=cn[i][0][:, 0:QL], in0=xt[:, 0:QL], scalar=rs[i][0][:, 0:1], in1=G[0][:, 0:QL],
                op0=ALU.mult, op1=ALU.mult), reads=[bx, rs[i][1], G[1]], writes=[cn[i][1]])
            S.op("dve", lambda e, i=i, xt=xt: e.scalar_tensor_tensor(
                out=cn[i][0][:, QL:Wc], in0=xt[:, QL:Wc], scalar=rs[i][0][:, 1:2], in1=G[0][:, QL:Wc],
                op0=ALU.mult, op1=ALU.mult), reads=[bx, rs[i][1], G[1]], writes=[cn[i][1]])
            transpose_tile(C, cn[i], cT[i], pst, ident, bI, KCc)
            S.dma("sp", lambda e, i=i, ti=ti: [e.dma_start(out=cqTb[ti], in_=cT[i][0][:, 0:QL // 128, :]),
                                               e.dma_start(out=ckrTb[ti][:, 0:KL // 128, :], in_=cT[i][0][:, QL // 128:KCc, :])],
                  cT[i][1][0], n=2, reads=cT[i][1])
            xk = xt[:, Wc:Wc + RD]
            if use_rope:
                ct, bc = csb[i]
                S.dma("sp", lambda e, ct=ct, ti=ti: [
                    e.dma_start(out=ct[:, 0, :], in_=rope_cs[ti * 128:(ti + 1) * 128, :]),
                    e.dma_start(out=ct[:, 1, :], in_=rope_sn[ti * 128:(ti + 1) * 128, :])], bc, n=2, writes=[bc])
                rope_ops(C, xk, bx, 1, RD, ct[:, 0, :], ct[:, 1, :], bc, krb[i][0][:, 0:RD], krb[i][1], tmp)
            else:
                S.op("act", lambda e, i=i, xk=xk: e.copy(out=krb[i][0][:, 0:RD], in_=xk), reads=[bx], writes=[krb[i][1]])
            transpose_tile(C, krb[i], krT[i], pst, ident, bI, 1)
            S.dma("sp", lambda e, i=i, ti=ti: [e.dma_start(out=ckrTb[ti][:, KL // 128:KL // 128 + 1, :], in_=krT[i][0][:])],
                  krT[i][1][0], reads=krT[i][1])


def ph_rope_heads(C, x, outTb, tiles, H, dh, rope_cs, rope_sn):
    S = C.S
    W = H * dh
    KC = W // 128
    hp = dh // 2
    with C.phase():
        ident, bI = C.sb("ident", [128, 128], BF16)
        make_identity(C, ident, bI)
        NB = 2
        xb = [C.sb("x%d" % i, [128, W], F32) for i in range(NB)]
        ob = [C.sb("o%d" % i, [128, W], BF16) for i in range(NB)]
        csb = [C.sb("cs%d" % i, [128, 2, hp], F32) for i in range(NB)]
        tmp = [C.sb("rt%d" % i, [128, H * hp], F32) for i in range(2)]
        oT = []
        for i in range(NB):
            t_, b_ = C.sb("oT%d" % i, [128, KC, 128], BF16)
            oT.append((t_, [b_] + [C.newbuf("oTg") for _ in range((KC + 3) // 4 - 1)]))
        pst = [C.ps("pst%d" % i, [128, 4, 128], BF16) for i in range(4)]
        for it, (ti, use_rope) in enumerate(tiles):
            i = it % NB
            xt, bx = xb[i]
            S.dma("sp", lambda e, xt=xt, ti=ti: [e.dma_start(out=xt[:], in_=x[ti * 128:(ti + 1) * 128, :])],
                  bx, writes=[bx])
            if use_rope:
                ct, bc = csb[i]
                S.dma("sp", lambda e, ct=ct, ti=ti: [
                    e.dma_start(out=ct[:, 0, :], in_=rope_cs[ti * 128:(ti + 1) * 128, :]),
                    e.dma_start(out=ct[:, 1, :], in_=rope_sn[ti * 128:(ti + 1) * 128, :])], bc, n=2, writes=[bc])
                rope_ops(C, xt[:], bx, H, dh, ct[:, 0, :], ct[:, 1, :], bc, ob[i][0][:], ob[i][1], tmp)
            else:
                S.op("act", lambda e, i=i, xt=xt: e.copy(out=ob[i][0][:], in_=xt[:]), reads=[bx], writes=[ob[i][1]])
            transpose_tile(C, ob[i], oT[i], pst, ident, bI, KC)
            S.dma("sp", lambda e, i=i, ti=ti: [e.dma_start(out=outTb[ti], in_=oT[i][0][:])], oT[i][1][0], reads=oT[i][1])


def ph_router(C, u32, routerT, combT, tiles, Dm, E):
    S = C.S
    with C.phase():
        Rt = C.sb("Rt", [128, E, Dm], F32)
        S.dma("sp", lambda e: [e.dma_start(out=Rt[0][:, ee, :], in_=routerT[ee].partition_broadcast(128))
                               for ee in range(E)], Rt[1], n=E, writes=[Rt[1]])
        identf = C.sb("identf", [128, 128], F32)
        S.op("pool", lambda e: e.memset(identf[0][:], 0.0), writes=[identf[1]])
        S.op("pool", lambda e: e.affine_select(out=identf[0][:], in_=identf[0][:], pattern=[[-1, 128]],
                                               compare_op=ALU.not_equal, fill=1.0, base=0, channel_multiplier=1),
             reads=[identf[1]], writes=[identf[1]])
        NB = 2
        ub = [C.sb("u%d" % i, [128, Dm], F32) for i in range(NB)]
        junk = C.sb("junk", [128, Dm], BF16)
        lg = [C.sb("lg%d" % i, [128, E], F32) for i in range(NB)]
        mx = [C.sb("mx%d" % i, [128, 8], F32) for i in range(NB)]
        gt = [C.sb("gt%d" % i, [128, 4], F32) for i in range(NB)]
        cb = [C.sb("cb%d" % i, [128, 2, E], F32) for i in range(NB)]
        cto = [C.sb("cto%d" % i, [E, 128], F32) for i in range(NB)]
        pc = [C.ps("pc%d" % i, [128, 128], F32) for i in range(2)]
        assert E == 8
        for it, ti in enumerate(tiles):
            i = it % NB
            ut, bu = ub[i]
            S.dma("sp", lambda e, ut=ut, ti=ti: [e.dma_start(out=ut[:], in_=u32[ti * 128:(ti + 1) * 128, :])],
                  bu, writes=[bu])
            for ee in range(E):
                S.op("dve", lambda e, ut=ut, ee=ee, i=i: e.scalar_tensor_tensor(
                    out=junk[0][:], in0=ut[:], scalar=1.0, in1=Rt[0][:, ee, :],
                    op0=ALU.mult, op1=ALU.mult, accum_out=lg[i][0][:, ee:ee + 1]),
                    reads=[bu, Rt[1]], writes=[junk[1], lg[i][1]])
            S.op("dve", lambda e, i=i: e.max(out=mx[i][0][:], in_=lg[i][0][:]), reads=[lg[i][1]], writes=[mx[i][1]])
            S.op("dve", lambda e, i=i: e.tensor_tensor(out=gt[i][0][:, 0:1], in0=mx[i][0][:, 0:1], in1=mx[i][0][:, 1:2],
                                                       op=ALU.subtract), reads=[mx[i][1]], writes=[gt[i][1]])
            S.op("act", lambda e, i=i: e.activation(out=gt[i][0][:, 1:2], in_=gt[i][0][:, 0:1], func=AF.Sigmoid),
                 reads=[gt[i][1]], writes=[gt[i][1]])
            S.op("dve", lambda e, i=i: e.tensor_scalar(out=gt[i][0][:, 2:3], in0=gt[i][0][:, 1:2], scalar1=-1.0,
                                                       scalar2=1.0, op0=ALU.mult, op1=ALU.add),
                 reads=[gt[i][1]], writes=[gt[i][1]])
            S.op("dve", lambda e, i=i: e.tensor_scalar(out=cb[i][0][:, 0, :], in0=lg[i][0][:], scalar1=mx[i][0][:, 0:1],
                                                       scalar2=gt[i][0][:, 1:2], op0=ALU.is_equal, op1=ALU.mult),
                 reads=[lg[i][1], mx[i][1], gt[i][1]], writes=[cb[i][1]])
            S.op("dve", lambda e, i=i: e.tensor_scalar(out=cb[i][0][:, 1, :], in0=lg[i][0][:], scalar1=mx[i][0][:, 1:2],
                                                       scalar2=gt[i][0][:, 2:3], op0=ALU.is_equal, op1=ALU.mult),
                 reads=[lg[i][1], mx[i][1], gt[i][1], cb[i][1]], writes=[cb[i][1]])
            S.op("dve", lambda e, i=i: e.tensor_tensor(out=cb[i][0][:, 0, :], in0=cb[i][0][:, 0, :], in1=cb[i][0][:, 1, :],
                                                       op=ALU.add), reads=[cb[i][1]], writes=[cb[i][1]])
            pt, pb = pc[it % 2]
            S.op("pe", lambda e, i=i, pt=pt: e.matmul(pt[0:E, :], cb[i][0][:, 0, :], identf[0][:], start=True, stop=True),
                 reads=[cb[i][1], identf[1]], writes=[pb])
            S.op("act", lambda e, i=i, pt=pt: e.copy(out=cto[i][0][:], in_=pt[0:E, :]), reads=[pb], writes=[cto[i][1]])
            S.dma("sp", lambda e, i=i, ti=ti: [e.dma_start(out=combT[:, ti * 128:(ti + 1) * 128], in_=cto[i][0][:])],
                  cto[i][1], reads=[cto[i][1]])


def ph_conv(C, xTb, W, cw, outTb, groups, halo_tile, edge, Dm):
    S = C.S
    KC = Dm // 128
    first_tile = groups[0][0]
    last_tile = groups[-1][-1]

    def do_group(grp):
        n = len(grp)
        ntok = n * 128
        chunks = [(c0, min(3, n - c0)) for c0 in range(0, n, 3)]
        with C.phase():
            X, b0 = C.sb("Xc", [128, KC, ntok + 2], BF16)
            xb = [b0] + [C.newbuf("Xc") for _ in range(n + 1)]
            for j, ti in enumerate(grp):
                S.dma("sp", lambda e, j=j, ti=ti: [e.dma_start(out=X[:, :, 1 + j * 128:1 + (j + 1) * 128], in_=xTb[ti])],
                      xb[j], writes=[xb[j]])
            eg = C.sb("edge", [128, 2], F32)
            S.dma("sp", lambda e: [e.dma_start(out=eg[0][:], in_=edge.partition_broadcast(128))], eg[1], writes=[eg[1]])
            for side, col, bidx in ((0, 0, n), (1, ntok + 1, n + 1)):
                if side == 0:
                    src = xTb[halo_tile][:, :, 0:1] if grp[0] == first_tile else xTb[grp[0] - 1][:, :, 127:128]
                    is_edge = grp[0] == first_tile
                else:
                    src = xTb[halo_tile][:, :, 1:2] if grp[-1] == last_tile else xTb[grp[-1] + 1][:, :, 0:1]
                    is_edge = grp[-1] == last_tile
                S.dma("sp", lambda e, src=src, col=col: [e.dma_start(out=X[:, :, col:col + 1], in_=src,
                                                                     allow_slow_non_contiguous=True)],
                      xb[bidx], writes=[xb[bidx]])
                if is_edge:
                    S.op("dve", lambda e, col=col, side=side: e.tensor_scalar(
                        out=X[:, :, col:col + 1], in0=X[:, :, col:col + 1], scalar1=eg[0][:, side:side + 1],
                        scalar2=None, op0=ALU.mult), reads=[eg[1]], writes=[xb[bidx]])
            cwt = C.sb("cw", [128, KC, 3], F32)
            S.dma("sp", lambda e: [e.dma_start(out=cwt[0][:], in_=cw)], cwt[1], writes=[cwt[1]])
            WL = WLoader(C, KC, nblk=6, width=128, KP=8, nstage=3)
            psb = [C.ps("cb%d" % i, [128, 512], F32) for i in range(2)]
            psc = [C.ps("cc%d" % i, [128, 512], F32) for i in range(2)]
            psv = [C.ps("cv%d" % i, [128, 512], F32) for i in range(2)]
            vsb = [C.sb("vs%d" % i, [128, 512], F32) for i in range(2)]
            pr = [C.sb("pr%d" % i, [128, 512], F32) for i in range(2)]
            zz = [C.sb("zz%d" % i, [128, 512], F32) for i in range(2)]
            ost = [C.sb("oc%d" % i, [128, 384], BF16) for i in range(2)]
            it = 0
            for f in range(KC):
                wts = [WL.load(W[:, q * Dm + f * 128:q * Dm + (f + 1) * 128]) for q in range(3)]
                for (c0, cn) in chunks:
                    cw_ = cn * 128
                    i = it % 2
                    it += 1
                    rb = list(xb[c0:c0 + cn])
                    if c0 == 0:
                        rb.append(xb[n])
                    else:
                        rb.append(xb[c0 - 1])
                    if c0 + cn == n:
                        rb.append(xb[n + 1])
                    else:
                        rb.append(xb[c0 + cn])
                    pss = (psb[i], psc[i], psv[i])
                    for q in range(3):
                        def mm(e, q=q, c0=c0, cw_=cw_, pt=pss[q][0], wt=wts[q][0]):
                            r = None
                            for k in range(KC):
                                r = e.matmul(pt[:, 0:cw_ + 2], wt[:, k, 0:128], X[:, k, c0 * 128:c0 * 128 + cw_ + 2],
                                             start=(k == 0), stop=(k == KC - 1))
                            return r
                        S.op("pe", mm, reads=rb + wts[q][1], writes=[pss[q][1]])
                    n2 = cw_ + 2
                    S.op("act", lambda e, i=i, n2=n2: e.copy(out=vsb[i][0][:, 0:n2], in_=psv[i][0][:, 0:n2]),
                         reads=[psv[i][1]], writes=[vsb[i][1]])
                    S.op("dve", lambda e, i=i, n2=n2: e.tensor_tensor(out=pr[i][0][:, 0:n2], in0=psc[i][0][:, 0:n2],
                                                                      in1=vsb[i][0][:, 0:n2], op=ALU.mult),
                         reads=[psc[i][1], vsb[i][1]], writes=[pr[i][1]])
                    S.op("dve", lambda e, i=i, f=f, cw_=cw_: e.tensor_scalar(
                        out=zz[i][0][:, 0:cw_], in0=pr[i][0][:, 1:cw_ + 1], scalar1=cwt[0][:, f, 1:2], scalar2=None,
                        op0=ALU.mult), reads=[pr[i][1], cwt[1]], writes=[zz[i][1]])
                    S.op("dve", lambda e, i=i, f=f, cw_=cw_: e.scalar_tensor_tensor(
                        out=zz[i][0][:, 0:cw_], in0=pr[i][0][:, 0:cw_], scalar=cwt[0][:, f, 0:1], in1=zz[i][0][:, 0:cw_],
                        op0=ALU.mult, op1=ALU.add), reads=[pr[i][1], cwt[1]], writes=[zz[i][1]])
                    S.op("dve", lambda e, i=i, f=f, cw_=cw_: e.scalar_tensor_tensor(
                        out=zz[i][0][:, 0:cw_], in0=pr[i][0][:, 2:cw_ + 2], scalar=cwt[0][:, f, 2:3], in1=zz[i][0][:, 0:cw_],
                        op0=ALU.mult, op1=ALU.add), reads=[pr[i][1], cwt[1]], writes=[zz[i][1]])
                    S.op("dve", lambda e, i=i, cw_=cw_: e.tensor_tensor(
                        out=ost[i][0][:, 0:cw_], in0=zz[i][0][:, 0:cw_], in1=psb[i][0][:, 1:cw_ + 1], op=ALU.mult),
                        reads=[zz[i][1], psb[i][1]], writes=[ost[i][1]])

                    def st(e, i=i, c0=c0, cn=cn, f=f):
                        return [e.dma_start(out=outTb[grp[c0 + j]][:, f, :], in_=ost[i][0][:, j * 128:(j + 1) * 128])
                                for j in range(cn)]
                    S.dma("sp", st, ost[i][1], n=cn, reads=[ost[i][1]])
    for grp in groups:
        do_group(grp)


import ml_dtypes

NPBF = ml_dtypes.bfloat16


class Cfg:
    def __init__(self, D=4096, SEQ=16384, CTX=256, NC=8, GRID_W=64, QL=None, KL=None, DFF=None, DFE=None):
        self.D, self.SEQ, self.CTX, self.NC, self.GRID_W = D, SEQ, CTX, NC, GRID_W
        self.HD = 128
        self.NH = D // 128
        self.NKV = self.NH // 4
        self.REP = 4
        self.QL = QL or D // 4
        self.KL = KL or D // 8
        self.NOPE, self.ROPE, self.VH = 128, 64, 128
        self.DFF = DFF or (5 * D) // 4
        self.E = 8
        self.DFE = DFE or D // 4
        self.L = 4
        self.KC = D // 128
        self.TL = SEQ // NC
        self.NLT = self.TL // 128
        self.NCT = CTX // 128
        self.T = self.TL + CTX
        self.NT = self.NLT + self.NCT
        self.HL = self.NH // NC
        self.TQ = SEQ + CTX
        self.NKB = self.TQ // 128
        self.GT = 9


def _groups(tiles, gt):
    ng = (len(tiles) + gt - 1) // gt
    per = (len(tiles) + ng - 1) // ng
    return [tiles[i:i + per] for i in range(0, len(tiles), per)]


class Prog:
    def __init__(self, name):
        self.name = name
        self.nc = bass.Bass("TRN2", target_bir_lowering=False)
        self.stack = ExitStack()
        self.C = Ctx(self.nc, self.stack)
        self.ins = {}
        self.outs = {}

    def inp(self, name, shape, dtype=F32):
        ap = self.C.dram(name, shape, dtype, "ExternalInput")
        self.ins[name] = (tuple(shape), dtype)
        return ap

    def out(self, name, shape, dtype=F32):
        ap = self.C.dram(name, shape, dtype, "ExternalOutput")
        self.outs[name] = (tuple(shape), dtype)
        return ap

    def tmp(self, name, shape, dtype=F32):
        self.ntmp = getattr(self, "ntmp", 0) + 1
        return self.C.dram("%s_t%d" % (name, self.ntmp), shape, dtype, "Internal")

    def finish(self):
        self.C.S.finish()
        self.stack.close()
        return self

    def run(self, in_maps):
        n = len(in_maps)
        maps = []
        for m in in_maps:
            mm = {}
            for k, (shape, dt) in self.ins.items():
                a = m[k]
                want = NPBF if dt == BF16 else np.float32
                a = np.ascontiguousarray(a)
                assert a.dtype == want, (k, a.dtype)
                assert tuple(a.shape) == shape, (k, a.shape, shape)
                mm[k] = a
            maps.append(mm)
        res = run_bass_kernel_spmd(self.nc, maps, core_ids=list(range(n)))
        return res.results


def lat_ctx_tiles(cfg, ctx=True):
    t = [(i, 0) for i in range(cfg.NLT)]
    if ctx:
        t += [(cfg.NLT + i, 1) for i in range(cfg.NCT)]
    return t


def emit_prenorm(P, cfg, h, uTb, g, mod, shift_i, scale_i, tiles, u32=None):
    ncls = 2
    ph_prenorm(P.C, h, uTb, g, [mod[c, scale_i] for c in range(ncls)], [mod[c, shift_i] for c in range(ncls)],
               tiles, cfg.D, u32=u32)


def emit_postnorm(P, cfg, y, h, hout, g, mod, gate_i, tiles):
    ph_postnorm(P.C, y, h, hout, g, [mod[c, gate_i] for c in range(2)], tiles, cfg.D)


def emit_dense_ffn(P, cfg, h1, hout, g_pre, g_post, mod, w_in, w_out, tiles):
    C = P.C
    tl = [t for t, _ in tiles]
    ntl = max(tl) + 1
    uTb = P.tmp("f_uTb", [ntl, 128, cfg.KC, 128], BF16)
    aTb = P.tmp("f_aTb", [ntl, 128, cfg.DFF // 128, 128], BF16)
    z = P.tmp("f_z", [ntl * 128, cfg.D], F32)
    emit_prenorm(P, cfg, h1, uTb, g_pre, mod, 3, 4, tiles)
    segs = []
    F = cfg.DFF
    for n0 in range(0, F, 256):
        w = min(256, F - n0)
        segs.append(dict(wg=w_in[:, n0:n0 + w], wu=w_in[:, F + n0:F + n0 + w], fc=n0 // 128, e=None))
    groups = _groups(tl, cfg.GT)
    ph_linear_feat(C, uTb, segs, aTb, groups, cfg.KC)
    ph_linear_tok(C, aTb, w_out, z, groups, F // 128, cfg.D)
    emit_postnorm(P, cfg, z, h1, hout, g_post, mod, 5, tiles)


def emit_moe_ffn(P, cfg, h1, hout, g_pre, g_post, mod, routerT, w_in, w_out, tiles):
    C = P.C
    tl = [t for t, _ in tiles]
    ntl = max(tl) + 1
    E, DFE = cfg.E, cfg.DFE
    uTb = P.tmp("m_uTb", [ntl, 128, cfg.KC, 128], BF16)
    u32 = P.tmp("m_u32", [ntl * 128, cfg.D], F32)
    combT = P.tmp("m_combT", [E, ntl * 128], F32)
    aTb = P.tmp("m_aTb", [ntl, 128, E * DFE // 128, 128], BF16)
    z = P.tmp("m_z", [ntl * 128, cfg.D], F32)
    emit_prenorm(P, cfg, h1, uTb, g_pre, mod, 3, 4, tiles, u32=u32)
    ph_router(C, u32, routerT, combT, tl, cfg.D, E)
    segs = []
    for e in range(E):
        for n0 in range(0, DFE, 256):
            w = min(256, DFE - n0)
            segs.append(dict(wg=w_in[e][:, n0:n0 + w], wu=w_in[e][:, DFE + n0:DFE + n0 + w],
                             fc=(e * DFE + n0) // 128, e=e))
    groups = _groups(tl, cfg.GT)
    ph_linear_feat(C, uTb, segs, aTb, groups, cfg.KC, combT=combT)
    KC2 = E * DFE // 128
    ph_linear_tok(C, aTb, w_out, z, _groups(tl, 4 if KC2 > 32 else cfg.GT), KC2, cfg.D, width=256 if KC2 > 32 else 512)
    emit_postnorm(P, cfg, z, h1, hout, g_post, mod, 5, tiles)


def common_inputs(P, cfg, nrows=None):
    h = P.inp("h", [nrows or cfg.T, cfg.D])
    mod = P.inp("mod", [2, 6, cfg.D])
    return h, mod


def build_ada(cfg):
    P = Prog("ada")
    NCOL = 6 * cfg.D // cfg.NC
    cT = P.inp("cT", [128, cfg.KC, 2])
    W = P.inp("ada_w", [cfg.L, cfg.D, NCOL])
    b = P.inp("ada_b", [cfg.L, NCOL])
    out = P.out("modp", [cfg.L, 2, NCOL])
    ph_ada(P.C, cT, W, b, out, cfg.L, cfg.D, NCOL)
    return P.finish()


def build_gqa_a(cfg, qknorm):
    P = Prog("gqa_a")
    C = P.C
    h, mod = common_inputs(P, cfg)
    g = P.inp("g_pre", [cfg.D])
    NQKV = (cfg.NH + 2 * cfg.NKV) * 128
    w_in = P.inp("w_in", [cfg.D, NQKV])
    rc = P.inp("rope_c", [cfg.TL, 64])
    rs_ = P.inp("rope_s", [cfg.TL, 64])
    qn = None
    if qknorm:
        qn = (P.inp("g_q", [128]), P.inp("g_k", [128]))
    qTb = P.out("qTb", [cfg.NT, 128, cfg.NH, 128], BF16)
    kTb = P.out("kTb", [cfg.NT, 128, cfg.NKV, 128], BF16)
    v = P.out("v", [cfg.T, cfg.NKV * 128], BF16)
    uTb = P.tmp("uTb", [cfg.NT, 128, cfg.KC, 128], BF16)
    qkv = P.tmp("qkv", [cfg.T, NQKV], F32)
    tiles = lat_ctx_tiles(cfg)
    emit_prenorm(P, cfg, h, uTb, g, mod, 0, 1, tiles)
    ph_linear_tok(C, uTb, w_in, qkv, _groups([t for t, _ in tiles], cfg.GT), cfg.KC, NQKV)
    ph_qkpost(C, qkv, qTb, kTb, v, [(t, c == 0) for t, c in tiles], cfg.NH, cfg.NKV, 128, rc, rs_, qknorm=qn)
    return P.finish()


def emit_out_proj(P, cfg, oTb, w_out, h, g_post, mod, tiles):
    tl = [t for t, _ in tiles]
    ntl = max(tl) + 1
    y = P.tmp("o_y", [ntl * 128, cfg.D], F32)
    h1 = P.tmp("o_h1", [ntl * 128, cfg.D], F32)
    ph_linear_tok(P.C, oTb, w_out, y, _groups(tl, cfg.GT), cfg.KC, cfg.D)
    emit_postnorm(P, cfg, y, h, h1, g_post, mod, 2, tiles)
    return h1


def ffn_inputs_dense(P, cfg):
    return dict(g_pre=P.inp("g_ffn_pre", [cfg.D]), g_post=P.inp("g_ffn_post", [cfg.D]),
                w_in=P.inp("ffn_w_in", [cfg.D, 2 * cfg.DFF]), w_out=P.inp("ffn_w_out", [cfg.DFF, cfg.D]))


def ffn_inputs_moe(P, cfg):
    return dict(g_pre=P.inp("g_ffn_pre", [cfg.D]), g_post=P.inp("g_ffn_post", [cfg.D]),
                routerT=P.inp("routerT", [cfg.E, cfg.D]),
                w_in=P.inp("moe_w_in", [cfg.E, cfg.D, 2 * cfg.DFE]), w_out=P.inp("moe_w_out", [cfg.E * cfg.DFE, cfg.D]))


def build_win_b(cfg):
    P = Prog("win_b")
    h, mod = common_inputs(P, cfg)
    NKB = cfg.NLT + 2 + cfg.NCT
    qTb = P.inp("qTb", [cfg.NT, 128, cfg.NH, 128], BF16)
    kTx = P.inp("kTx", [NKB, 128, cfg.NKV, 128], BF16)
    vx = P.inp("vx", [NKB, 128, cfg.NKV * 128], BF16)
    sink = P.inp("sink", [cfg.NH])
    edge = P.inp("edge", [2])
    w_out = P.inp("w_out", [cfg.D, cfg.D])
    g_post = P.inp("g_mix_post", [cfg.D])
    f = ffn_inputs_dense(P, cfg)
    hout = P.out("hout", [cfg.T, cfg.D])
    oTb = P.tmp("oTb", [cfg.NT, 128, cfg.NH, 128], BF16)
    tiles = lat_ctx_tiles(cfg)
    ph_attn_window(P.C, qTb, [kTx[kb] for kb in range(NKB)], [vx[kb] for kb in range(NKB)], oTb, sink, edge,
                   cfg.NLT, cfg.NCT, cfg.NH, cfg.NKV, 128 ** -0.5)
    h1 = emit_out_proj(P, cfg, oTb, w_out, h, g_post, mod, tiles)
    emit_dense_ffn(P, cfg, h1, hout, f["g_pre"], f["g_post"], mod, f["w_in"], f["w_out"], tiles)
    return P.finish()


def build_mla_a(cfg):
    P = Prog("mla_a")
    C = P.C
    h, mod = common_inputs(P, cfg)
    g = P.inp("g_pre", [cfg.D])
    QL, KL, RD, NH = cfg.QL, cfg.KL, cfg.ROPE, cfg.NH
    w_in = P.inp("w_in", [cfg.D, QL + KL + RD])
    g_q = P.inp("g_q", [QL])
    g_kv = P.inp("g_kv", [KL])
    w_uqn = P.inp("w_uq_nope", [QL, NH * 128])
    w_uqr = P.inp("w_uq_rot", [QL, NH * RD])
    w_ukn = P.inp("w_ukv_nope", [KL, NH * 128])
    w_ukv = P.inp("w_ukv_v", [KL, NH * 128])
    rc = P.inp("rope_c", [cfg.TL, RD // 2])
    rs_ = P.inp("rope_s", [cfg.TL, RD // 2])
    qnTb = P.out("qnTb", [cfg.NT, 128, NH, 128], BF16)
    qrTb = P.out("qrTb", [cfg.NT, 128, NH * RD // 128, 128], BF16)
    knTb = P.out("knTb", [cfg.NT, 128, NH, 128], BF16)
    ckrTb = P.out("ckrTb", [cfg.NT, 128, KL // 128 + 1, 128], BF16)
    v = P.out("v", [cfg.T, NH * 128], BF16)
    uTb = P.tmp("uTb", [cfg.NT, 128, cfg.KC, 128], BF16)
    call = P.tmp("call", [cfg.T, QL + KL + RD], F32)
    cqTb = P.tmp("cqTb", [cfg.NT, 128, QL // 128, 128], BF16)
    qr = P.tmp("qr", [cfg.T, NH * RD], F32)
    tiles = lat_ctx_tiles(cfg)
    tl = [t for t, _ in tiles]
    groups = _groups(tl, cfg.GT)
    emit_prenorm(P, cfg, h, uTb, g, mod, 0, 1, tiles)
    ph_linear_tok(C, uTb, w_in, call, groups, cfg.KC, QL + KL + RD)
    ph_mla_post(C, call, cqTb, ckrTb, g_q, g_kv, [(t, c == 0) for t, c in tiles], QL, KL, RD, rc, rs_)
    segs = [dict(wg=w_uqn[:, n0:n0 + 512], wu=None, fc=n0 // 128, e=None) for n0 in range(0, NH * 128, 512)]
    ph_linear_feat(C, cqTb, segs, qnTb, groups, QL // 128, k0=0)
    ph_linear_tok(C, cqTb, w_uqr, qr, groups, QL // 128, NH * RD, k0=0)
    ph_rope_heads(C, qr, qrTb, [(t, c == 0) for t, c in tiles], NH, RD, rc, rs_)
    segs = [dict(wg=w_ukn[:, n0:n0 + 512], wu=None, fc=n0 // 128, e=None) for n0 in range(0, NH * 128, 512)]
    ph_linear_feat(C, ckrTb, segs, knTb, groups, KL // 128, k0=0)
    ph_linear_tok(C, ckrTb, w_ukv, v, groups, KL // 128, NH * 128, out_dtype=BF16, k0=0)
    return P.finish()


def build_attn_full(cfg, mla):
    P = Prog("attn_full")
    HL, TQ, NKB = cfg.HL, cfg.TQ, cfg.NKB
    NKVl = HL if mla else 1
    qTh = P.inp("qTh", [HL, 128, TQ], BF16)
    kTh = P.inp("kTh", [NKVl, 128, TQ], BF16)
    vh = P.inp("vh", [NKVl, 128, NKB, 128], BF16)
    qrTh = krT = None
    if mla:
        qrTh = P.inp("qrTh", [HL, 64, TQ], BF16)
        krT = P.inp("krT", [64, TQ], BF16)
    oTh = P.out("oTh", [HL, 128, TQ], BF16)
    scale = (192 if mla else 128) ** -0.5
    ctx_kb = [cfg.SEQ // 128 + i for i in range(cfg.NCT)]
    ph_attn_full(P.C, qTh, kTh, vh, oTh, HL, NKVl, cfg.SEQ, cfg.CTX, NKB, scale, qrTh=qrTh, krT=krT, ctx_kb=ctx_kb)
    return P.finish()


def build_tail(cfg, moe):
    P = Prog("tail")
    h, mod = common_inputs(P, cfg)
    oTb = P.inp("oTb", [cfg.NT, 128, cfg.NH, 128], BF16)
    w_out = P.inp("w_out", [cfg.D, cfg.D])
    g_post = P.inp("g_mix_post", [cfg.D])
    f = ffn_inputs_moe(P, cfg) if moe else ffn_inputs_dense(P, cfg)
    hout = P.out("hout", [cfg.T, cfg.D])
    tiles = lat_ctx_tiles(cfg)
    h1 = emit_out_proj(P, cfg, oTb, w_out, h, g_post, mod, tiles)
    if moe:
        emit_moe_ffn(P, cfg, h1, hout, f["g_pre"], f["g_post"], mod, f["routerT"], f["w_in"], f["w_out"], tiles)
    else:
        emit_dense_ffn(P, cfg, h1, hout, f["g_pre"], f["g_post"], mod, f["w_in"], f["w_out"], tiles)
    return P.finish()


def build_conv_layer(cfg):
    P = Prog("conv")
    C = P.C
    NLT = cfg.NLT
    h, mod = common_inputs(P, cfg, nrows=(NLT + 1) * 128)
    g = P.inp("g_pre", [cfg.D])
    W = P.inp("conv_w_in", [cfg.D, 3 * cfg.D])
    cw = P.inp("conv_w", [128, cfg.KC, 3])
    edge = P.inp("edge", [2])
    w_out = P.inp("w_out", [cfg.D, cfg.D])
    g_post = P.inp("g_mix_post", [cfg.D])
    f = ffn_inputs_moe(P, cfg)
    hout = P.out("hout", [NLT * 128, cfg.D])
    uTb = P.tmp("uTb", [NLT + 1, 128, cfg.KC, 128], BF16)
    yTb = P.tmp("yTb", [NLT, 128, cfg.KC, 128], BF16)
    tiles_h = [(i, 0) for i in range(NLT + 1)]
    tiles = [(i, 0) for i in range(NLT)]
    tl = list(range(NLT))
    emit_prenorm(P, cfg, h, uTb, g, mod, 0, 1, tiles_h)
    ph_conv(C, uTb, W, cw, yTb, _groups(tl, 8), NLT, edge, cfg.D)
    h1 = emit_out_proj(P, cfg, yTb, w_out, h, g_post, mod, tiles)
    emit_moe_ffn(P, cfg, h1, hout, f["g_pre"], f["g_post"], mod, f["routerT"], f["w_in"], f["w_out"], tiles)
    return P.finish()


_PROGS = {}


def _prog(key, fn):
    if key not in _PROGS:
        _PROGS[key] = fn()
    return _PROGS[key]


def rope_tables(cfg, rot_dim):
    n_freq = rot_dim // 4
    inv = (np.float32(10000.0) ** (-np.arange(n_freq, dtype=np.float32) / np.float32(n_freq))).astype(np.float32)
    t = np.arange(cfg.SEQ)
    row = (t // cfg.GRID_W).astype(np.float32)
    col = (t % cfg.GRID_W).astype(np.float32)
    ang = np.concatenate([row[:, None] * inv[None, :], col[:, None] * inv[None, :]], axis=-1).astype(np.float32)
    return np.cos(ang).astype(np.float32), np.sin(ang).astype(np.float32)


def blocked_to_feat(xTb, ntiles=None):
    a = xTb if ntiles is None else xTb[:ntiles]
    nt, _, fc, _ = a.shape
    return a.transpose(2, 1, 0, 3).reshape(fc * 128, nt * 128)


def feat_to_blocked(xT):
    F, T = xT.shape
    return np.ascontiguousarray(xT.reshape(F // 128, 128, T // 128, 128).transpose(2, 1, 0, 3))


def run_model(cfg, inp, debug=None):
    NC, D, L = cfg.NC, cfg.D, cfg.L
    f32 = np.float32
    x = np.asarray(inp["x"], f32)[0]
    ctx = np.asarray(inp["ctx"], f32)[0]
    g = lambda k: np.asarray(inp[k], f32)

    cvec = np.stack([g("c")[0], g("c_ctx")])
    cT = np.ascontiguousarray(cvec.reshape(2, cfg.KC, 128).transpose(2, 1, 0))
    NCOL = 6 * D // NC
    ada_w, ada_b = g("ada_w"), g("ada_b")
    P = _prog(("ada", id(cfg)), lambda: build_ada(cfg))
    res = P.run([dict(cT=cT, ada_w=ada_w[:, :, c * NCOL:(c + 1) * NCOL], ada_b=ada_b[:, c * NCOL:(c + 1) * NCOL])
                 for c in range(NC)])
    mod_all = np.concatenate([r["modp"] for r in res], axis=-1).reshape(L, 2, 6, D)
    if debug is not None:
        debug["mod"] = mod_all

    hs = [np.concatenate([x[c * cfg.TL:(c + 1) * cfg.TL], ctx], axis=0) for c in range(NC)]
    rc_h, rs_h = rope_tables(cfg, 128)
    rc_m, rs_m = rope_tables(cfg, cfg.ROPE)
    sl = lambda a, c: np.ascontiguousarray(a[c * cfg.TL:(c + 1) * cfg.TL])
    NLT, NCT, NT, TL = cfg.NLT, cfg.NCT, cfg.NT, cfg.TL

    def dense_ffn_w(i):
        fi = i // 2
        return dict(g_ffn_pre=g("g_ffn_pre")[i], g_ffn_post=g("g_ffn_post")[i],
                    ffn_w_in=g("ffn_w_in")[fi], ffn_w_out=g("ffn_w_out")[fi])

    def moe_ffn_w(i):
        fi = i // 2
        return dict(g_ffn_pre=g("g_ffn_pre")[i], g_ffn_post=g("g_ffn_post")[i],
                    routerT=np.ascontiguousarray(g("moe_router")[fi].T),
                    moe_w_in=g("moe_w_in")[fi], moe_w_out=g("moe_w_out")[fi].reshape(cfg.E * cfg.DFE, D))

    def gather_heads(blk_list, key):
        lat = [blocked_to_feat(r[key], NLT) for r in blk_list]
        cx = blocked_to_feat(blk_list[0][key][NLT:NT])
        return np.concatenate(lat + [cx], axis=1)

    def scatter_heads(o_all):
        outs = []
        for c in range(NC):
            cols = np.concatenate([o_all[:, c * TL:(c + 1) * TL], o_all[:, cfg.SEQ:]], axis=1)
            outs.append(feat_to_blocked(cols))
        return outs

    def v_blocks(v_all_head):
        return np.ascontiguousarray(v_all_head.reshape(cfg.NKB, 128, 128).transpose(1, 0, 2))

    i = 0
    mod = np.ascontiguousarray(mod_all[i])
    P = _prog(("gqa_a", id(cfg), False), lambda: build_gqa_a(cfg, False))
    ra = P.run([dict(h=hs[c], mod=mod, g_pre=g("g_mix_pre")[i], w_in=g("win_w_in")[0], rope_c=sl(rc_h, c),
                     rope_s=sl(rs_h, c)) for c in range(NC)])
    if debug is not None:
        debug["l0a"] = ra
    P = _prog(("win_b", id(cfg)), lambda: build_win_b(cfg))
    maps = []
    for c in range(NC):
        kT, vv = ra[c]["kTb"], ra[c]["v"].reshape(NT, 128, -1)
        zk, zv = np.zeros_like(kT[0:1]), np.zeros_like(vv[0:1])
        lk = ra[c - 1]["kTb"][NLT - 1:NLT] if c > 0 else zk
        lv = ra[c - 1]["v"].reshape(NT, 128, -1)[NLT - 1:NLT] if c > 0 else zv
        rk = ra[c + 1]["kTb"][0:1] if c < NC - 1 else zk
        rv = ra[c + 1]["v"].reshape(NT, 128, -1)[0:1] if c < NC - 1 else zv
        kTx = np.concatenate([lk, kT[:NLT], rk, kT[NLT:]], axis=0)
        vx = np.concatenate([lv, vv[:NLT], rv, vv[NLT:]], axis=0)
        edge = np.array([0.0 if c > 0 else -30000.0, 0.0 if c < NC - 1 else -30000.0], f32)
        m = dict(h=hs[c], mod=mod, qTb=ra[c]["qTb"], kTx=kTx, vx=vx, sink=g("win_sink")[0], edge=edge,
                 w_out=g("win_w_out")[0], g_mix_post=g("g_mix_post")[i])
        m.update(dense_ffn_w(i))
        maps.append(m)
    rb = P.run(maps)
    hs = [r["hout"] for r in rb]
    if debug is not None:
        debug["h0"] = hs

    i = 1
    mod = np.ascontiguousarray(mod_all[i])
    NH, RD = cfg.NH, cfg.ROPE
    w_uq = g("mla_w_uq")[0].reshape(cfg.QL, NH, 192)
    w_ukv = g("mla_w_ukv")[0].reshape(cfg.KL, NH, 256)
    mw = dict(w_in=g("mla_w_in")[0], g_q=g("mla_g_q")[0], g_kv=g("mla_g_kv")[0],
              w_uq_nope=np.ascontiguousarray(w_uq[:, :, :128]).reshape(cfg.QL, NH * 128),
              w_uq_rot=np.ascontiguousarray(w_uq[:, :, 128:]).reshape(cfg.QL, NH * RD),
              w_ukv_nope=np.ascontiguousarray(w_ukv[:, :, :128]).reshape(cfg.KL, NH * 128),
              w_ukv_v=np.ascontiguousarray(w_ukv[:, :, 128:]).reshape(cfg.KL, NH * 128))
    P = _prog(("mla_a", id(cfg)), lambda: build_mla_a(cfg))
    maps = []
    for c in range(NC):
        m = dict(h=hs[c], mod=mod, g_pre=g("g_mix_pre")[i], rope_c=sl(rc_m, c), rope_s=sl(rs_m, c))
        m.update(mw)
        maps.append(m)
    ra = P.run(maps)
    if debug is not None:
        debug["l1a"] = ra
    qn = gather_heads(ra, "qnTb").reshape(NH, 128, cfg.TQ)
    kn = gather_heads(ra, "knTb").reshape(NH, 128, cfg.TQ)
    qr = gather_heads(ra, "qrTb").reshape(NH, RD, cfg.TQ)
    kr = gather_heads(ra, "ckrTb")[cfg.KL:cfg.KL + RD]
    v_all = np.concatenate([r["v"][:TL] for r in ra] + [ra[0]["v"][TL:]], axis=0).reshape(cfg.TQ, NH, 128)
    P = _prog(("attn", id(cfg), True), lambda: build_attn_full(cfg, True))
    HL = cfg.HL
    maps = []
    for c in range(NC):
        hsl = slice(c * HL, (c + 1) * HL)
        maps.append(dict(qTh=qn[hsl], kTh=kn[hsl], qrTh=qr[hsl], krT=kr,
                         vh=np.stack([v_blocks(v_all[:, hh]) for hh in range(c * HL, (c + 1) * HL)])))
    ro = P.run(maps)
    o_all = np.concatenate([r["oTh"].reshape(HL * 128, cfg.TQ) for r in ro], axis=0)
    oTbs = scatter_heads(o_all)
    if debug is not None:
        debug["l1o"] = o_all
    P = _prog(("tail", id(cfg), True), lambda: build_tail(cfg, True))
    maps = []
    for c in range(NC):
        m = dict(h=hs[c], mod=mod, oTb=oTbs[c], w_out=g("mla_w_out")[0], g_mix_post=g("g_mix_post")[i])
        m.update(moe_ffn_w(i))
        maps.append(m)
    rb = P.run(maps)
    hs = [r["hout"] for r in rb]
    if debug is not None:
        debug["h1"] = hs

    i = 2
    mod = np.ascontiguousarray(mod_all[i])
    P = _prog(("gqa_a", id(cfg), True), lambda: build_gqa_a(cfg, True))
    ra = P.run([dict(h=hs[c], mod=mod, g_pre=g("g_mix_pre")[i], w_in=g("qkn_w_in")[0], rope_c=sl(rc_h, c),
                     rope_s=sl(rs_h, c), g_q=g("qkn_g_q")[0], g_k=g("qkn_g_k")[0]) for c in range(NC)])
    qn = gather_heads(ra, "qTb").reshape(NH, 128, cfg.TQ)
    kn = gather_heads(ra, "kTb").reshape(cfg.NKV, 128, cfg.TQ)
    v_all = np.concatenate([r["v"][:TL] for r in ra] + [ra[0]["v"][TL:]], axis=0).reshape(cfg.TQ, cfg.NKV, 128)
    P = _prog(("attn", id(cfg), False), lambda: build_attn_full(cfg, False))
    maps = []
    for c in range(NC):
        maps.append(dict(qTh=qn[c * HL:(c + 1) * HL], kTh=kn[c:c + 1], vh=v_blocks(v_all[:, c])[None]))
    ro = P.run(maps)
    o_all = np.concatenate([r["oTh"].reshape(HL * 128, cfg.TQ) for r in ro], axis=0)
    oTbs = scatter_heads(o_all)
    P = _prog(("tail", id(cfg), False), lambda: build_tail(cfg, False))
    maps = []
    for c in range(NC):
        m = dict(h=hs[c], mod=mod, oTb=oTbs[c], w_out=g("qkn_w_out")[0], g_mix_post=g("g_mix_post")[i])
        m.update(dense_ffn_w(i))
        maps.append(m)
    rb = P.run(maps)
    hs = [r["hout"] for r in rb]
    if debug is not None:
        debug["h2"] = hs

    i = 3
    mod = np.ascontiguousarray(mod_all[i])
    P = _prog(("conv", id(cfg)), lambda: build_conv_layer(cfg))
    cw = np.ascontiguousarray(g("conv_w")[0].reshape(3, cfg.KC, 128).transpose(2, 1, 0))
    maps = []
    for c in range(NC):
        halo = np.zeros((128, D), f32)
        if c > 0:
            halo[0] = hs[c - 1][TL - 1]
        if c < NC - 1:
            halo[1] = hs[c + 1][0]
        edge = np.array([1.0 if c > 0 else 0.0, 1.0 if c < NC - 1 else 0.0], f32)
        m = dict(h=np.concatenate([hs[c][:TL], halo], axis=0), mod=mod, g_pre=g("g_mix_pre")[i],
                 conv_w_in=g("conv_w_in")[0], conv_w=cw, edge=edge, w_out=g("conv_w_out")[0],
                 g_mix_post=g("g_mix_post")[i])
        m.update(moe_ffn_w(i))
        maps.append(m)
    rb = P.run(maps)
    out = np.concatenate([r["hout"] for r in rb], axis=0)[None]
    return out.astype(f32)


_CFG = Cfg()


def kernel(**inputs):
    return run_model(_CFG, inputs)


class ModL:
    def __init__(self, Gv, l):
        self.Gv, self.l = Gv, l

    def __getitem__(self, key):
        cls, j = key
        return self.Gv[self.l, cls, j]


def build_fused(cfg):
    P = Prog("fused")
    C = P.C
    NC, D, L, NT, NLT, NCT, TL, T = cfg.NC, cfg.D, cfg.L, cfg.NT, cfg.NLT, cfg.NCT, cfg.TL, cfg.T
    NH, NKV, KC, E, DFE, DFF = cfg.NH, cfg.NKV, cfg.KC, cfg.E, cfg.DFE, cfg.DFF
    QL, KL, RD = cfg.QL, cfg.KL, cfg.ROPE
    SEQ, NKB = cfg.SEQ, cfg.NKB
    CW = D // NC
    HR = (NT + 1) * 128
    h0 = P.inp("h0", [T, D])
    cT = P.inp("cT", [128, KC, 2])
    ada_w = P.inp("ada_w", [L, D, 6 * CW])
    ada_b = P.inp("ada_b", [L, 6 * CW])
    g_mix_pre = P.inp("g_mix_pre", [L, D])
    g_mix_post = P.inp("g_mix_post", [L, D])
    g_ffn_pre = P.inp("g_ffn_pre", [L, D])
    g_ffn_post = P.inp("g_ffn_post", [L, D])
    rch = P.inp("rope_ch", [TL, 64])
    rsh = P.inp("rope_sh", [TL, 64])
    rcm = P.inp("rope_cm", [TL, RD // 2])
    rsm = P.inp("rope_sm", [TL, RD // 2])
    NQKV = (NH + 2 * NKV) * 128
    win_w_in = P.inp("win_w_in", [D, NQKV])
    win_sink = P.inp("win_sink", [NH])
    win_w_out = P.inp("win_w_out", [D, D])
    sel = P.inp("sel", [2, NC])
    wedge = P.inp("wedge", [2])
    mla_w_in = P.inp("mla_w_in", [D, QL + KL + RD])
    mla_g_q = P.inp("mla_g_q", [QL])
    mla_g_kv = P.inp("mla_g_kv", [KL])
    w_uqn = P.inp("w_uq_nope", [QL, NH * 128])
    w_uqr = P.inp("w_uq_rot", [QL, NH * RD])
    w_ukn = P.inp("w_ukv_nope", [KL, NH * 128])
    w_ukv = P.inp("w_ukv_v", [KL, NH * 128])
    mla_w_out = P.inp("mla_w_out", [D, D])
    qkn_w_in = P.inp("qkn_w_in", [D, NQKV])
    qkn_g_q = P.inp("qkn_g_q", [128])
    qkn_g_k = P.inp("qkn_g_k", [128])
    qkn_w_out = P.inp("qkn_w_out", [D, D])
    conv_w_in = P.inp("conv_w_in", [D, 3 * D])
    conv_cw = P.inp("conv_cw", [128, KC, 3])
    conv_w_out = P.inp("conv_w_out", [D, D])
    cedge = P.inp("cedge", [2])
    selM = P.inp("selM", [2 * NC, 2])
    ffn_w_in = P.inp("ffn_w_in", [2, D, 2 * DFF])
    ffn_w_out = P.inp("ffn_w_out", [2, DFF, D])
    routerT = P.inp("routerT", [2, E, D])
    moe_w_in = P.inp("moe_w_in", [2, E, D, 2 * DFE])
    moe_w_out = P.inp("moe_w_out", [2, E * DFE, D])
    hout = P.out("hout", [TL, D])

    tiles = lat_ctx_tiles(cfg)
    tl = [t for t, _ in tiles]
    groups = _groups(tl, cfg.GT)
    rope_flags = [(t, c == 0) for t, c in tiles]

    modp = P.tmp("modp", [L * 2, 6 * CW])
    ph_ada(C, cT, ada_w, ada_b, modp.rearrange("(l m) n -> l m n", m=2), L, D, 6 * CW)
    Gmod = P.tmp("Gmod", [NC * L * 2, 6 * CW])
    ph_allgather(C, modp, Gmod, NC)
    Gv = Gmod.rearrange("(c l m) (j n) -> l m j c n", c=NC, l=L, m=2, j=6)

    mod = ModL(Gv, 0)
    uTb = P.tmp("uTb", [NT, 128, KC, 128], BF16)
    qkv = P.tmp("qkv", [T, NQKV], F32)
    qTb = P.tmp("qTb", [NT, 128, NH, 128], BF16)
    kTb = P.tmp("kTb", [NT, 128, NKV, 128], BF16)
    v = P.tmp("v", [T, NKV * 128], BF16)
    oTb = P.tmp("oTb", [NT, 128, NH, 128], BF16)
    emit_prenorm(P, cfg, h0, uTb, g_mix_pre[0], mod, 0, 1, tiles)
    ph_linear_tok(C, uTb, win_w_in, qkv, groups, KC, NQKV)
    ph_qkpost(C, qkv, qTb, kTb, v, rope_flags, NH, NKV, 128, rch, rsh)
    FW = NKV * 128
    Hsrc = P.tmp("Hsrc", [4 * 128, FW], BF16)
    ph_copy_rows(C, [(Hsrc[0:128], kTb[0].rearrange("p h k -> p (h k)")),
                     (Hsrc[128:256], kTb[NLT - 1].rearrange("p h k -> p (h k)")),
                     (Hsrc[256:384], v[0:128]), (Hsrc[384:512], v[TL - 128:TL])], FW, BF16)
    Hg = P.tmp("Hg", [NC * 512, FW], BF16)
    ph_allgather(C, Hsrc, Hg, NC)
    Hgv = Hg.rearrange("(c w p) f -> c w p f", c=NC, w=4)
    kblocks = [None] + [kTb[t] for t in range(NLT)] + [None] + [kTb[NLT + j] for j in range(NCT)]
    vblocks = [None] + [v[t * 128:(t + 1) * 128] for t in range(NLT)] + [None] + \
              [v[(NLT + j) * 128:(NLT + j + 1) * 128] for j in range(NCT)]
    ph_attn_window(C, qTb, kblocks, vblocks, oTb, win_sink, wedge, NLT, NCT, NH, NKV, 128 ** -0.5, halo=(Hgv, sel))
    h1 = emit_out_proj(P, cfg, oTb, win_w_out, h0, g_mix_post[0], mod, tiles)
    hL0 = P.tmp("hL0", [HR, D])
    emit_dense_ffn(P, cfg, h1, hL0, g_ffn_pre[0], g_ffn_post[0], mod, ffn_w_in[0], ffn_w_out[0], tiles)

    mod = ModL(Gv, 1)
    KVC = KL // 128
    call = P.tmp("call", [T, QL + KL + RD], F32)
    cqTb = P.tmp("cqTb", [NT, 128, QL // 128, 128], BF16)
    ckrTb = P.tmp("ckrTb", [NT, 128, KVC + 1, 128], BF16)
    qr = P.tmp("qr", [T, NH * RD], F32)
    qrTb = P.tmp("qrTb", [NT, 128, NH * RD // 128, 128], BF16)
    emit_prenorm(P, cfg, hL0, uTb, g_mix_pre[1], mod, 0, 1, tiles)
    ph_linear_tok(C, uTb, mla_w_in, call, groups, KC, QL + KL + RD)
    ph_mla_post(C, call, cqTb, ckrTb, mla_g_q, mla_g_kv, rope_flags, QL, KL, RD, rcm, rsm)
    segs = [dict(wg=w_uqn[:, n0:n0 + 512], wu=None, fc=n0 // 128, e=None) for n0 in range(0, NH * 128, 512)]
    ph_linear_feat(C, cqTb, segs, qTb, groups, QL // 128)
    ph_linear_tok(C, cqTb, w_uqr, qr, groups, QL // 128, NH * RD)
    ph_rope_heads(C, qr, qrTb, rope_flags, NH, RD, rcm, rsm)
    Gkr = P.tmp("Gkr", [NC * NLT * 128, (KVC + 1) * 128], BF16)
    ph_allgather(C, ckrTb[0:NLT].rearrange("t p c k -> (t p) (c k)"), Gkr, NC)
    Gkrv = Gkr.rearrange("(t p) (c k) -> t p c k", p=128, k=128)
    NLA = NC * NLT

    def xsrc(ti):
        return Gkrv[ti] if ti < NLA else ckrTb[NLT + ti - NLA]
    kTh = P.tmp("kTh", [NH, 128, NKB * 128], BF16)
    Vall = P.tmp("Vall", [NKB * 128, NH * 128], BF16)
    agroups = _groups(list(range(NKB)), 32)

    def kstore(e, ot, grp, c0, cn, fc, ns):
        return [e.dma_start(out=kTh[fc + s_][:, grp[c0] * 128:(grp[c0] + cn) * 128], in_=ot[:, s_, 0:cn * 128])
                for s_ in range(ns)]
    segs = [dict(wg=w_ukn[:, n0:n0 + 512], wu=None, fc=n0 // 128, e=None) for n0 in range(0, NH * 128, 512)]
    ph_linear_feat(C, xsrc, segs, None, agroups, KVC, store_fn=kstore)
    ph_linear_tok(C, xsrc, w_ukv, Vall, agroups, KVC, NH * 128, out_dtype=BF16)
    Vv = Vall.rearrange("(b p) f -> p b f", p=128)
    k_loader = (lambda e, kT, kv: [e.dma_start(out=kT[:], in_=kTh[kv])], 1)
    v_loader = (lambda e, vv, kv: [e.dma_start(out=vv[:], in_=Vv[:, :, kv * 128:(kv + 1) * 128])], 1)

    def krl(e, kr, _):
        return [e.dma_start(out=kr[:, 0:SEQ].rearrange("p (t k) -> p t k", k=128),
                            in_=Gkrv[:, 0:64, KVC, :].rearrange("t p k -> p t k")),
                e.dma_start(out=kr[:, SEQ:NKB * 128].rearrange("p (t k) -> p t k", k=128),
                            in_=ckrTb[NLT:NT][:, 0:64, KVC, :].rearrange("t p k -> p t k"))]
    ph_attn_tok(C, qTb, oTb, NH, lambda hh: hh, k_loader, v_loader, NLT, NCT, NKB, 192 ** -0.5, qrTb=qrTb,
                kr_loader=(krl, 2))
    h1 = emit_out_proj(P, cfg, oTb, mla_w_out, hL0, g_mix_post[1], mod, tiles)
    hL1 = P.tmp("hL1", [HR, D])
    emit_moe_ffn(P, cfg, h1, hL1, g_ffn_pre[1], g_ffn_post[1], mod, routerT[0], moe_w_in[0], moe_w_out[0], tiles)

    mod = ModL(Gv, 2)
    emit_prenorm(P, cfg, hL1, uTb, g_mix_pre[2], mod, 0, 1, tiles)
    ph_linear_tok(C, uTb, qkn_w_in, qkv, groups, KC, NQKV)
    ph_qkpost(C, qkv, qTb, kTb, v, rope_flags, NH, NKV, 128, rch, rsh, qknorm=(qkn_g_q, qkn_g_k))
    GK = P.tmp("GK", [NLA * 128, FW], BF16)
    ph_allgather(C, kTb[0:NLT].rearrange("t p h k -> (t p) (h k)"), GK, NC)
    GKv = GK.rearrange("(t p) (h k) -> t p h k", p=128, k=128)
    GV = P.tmp("GV", [SEQ, FW], BF16)
    ph_allgather(C, v[0:TL], GV, NC)
    GVv = GV.rearrange("(b p) f -> p b f", p=128)
    vcx = v[TL:T].rearrange("(b p) f -> p b f", p=128)
    NKBL = SEQ // 128

    def kl2(e, kT, g):
        return [e.dma_start(out=kT[:, 0:SEQ].rearrange("p (t k) -> p t k", k=128),
                            in_=GKv[:, :, g, :].rearrange("t p k -> p t k")),
                e.dma_start(out=kT[:, SEQ:NKB * 128].rearrange("p (t k) -> p t k", k=128),
                            in_=kTb[NLT:NT][:, :, g, :].rearrange("t p k -> p t k"))]

    def vl2(e, vv, g):
        return [e.dma_start(out=vv[:, 0:NKBL, :], in_=GVv[:, :, g * 128:(g + 1) * 128]),
                e.dma_start(out=vv[:, NKBL:NKB, :], in_=vcx[:, :, g * 128:(g + 1) * 128])]
    ph_attn_tok(C, qTb, oTb, NH, lambda hh: hh // cfg.REP, (kl2, 2), (vl2, 2), NLT, NCT, NKB, 128 ** -0.5)
    h1 = emit_out_proj(P, cfg, oTb, qkn_w_out, hL1, g_mix_post[2], mod, tiles)
    hL2 = P.tmp("hL2", [HR, D])
    emit_dense_ffn(P, cfg, h1, hL2, g_ffn_pre[2], g_ffn_post[2], mod, ffn_w_in[1], ffn_w_out[1], tiles)

    mod = ModL(Gv, 3)
    Hs = P.tmp("Hs", [2, D])
    ph_copy_rows(C, [(Hs[0:1], hL2[0:1]), (Hs[1:2], hL2[TL - 1:TL])], D, F32)
    Gh = P.tmp("Gh", [2 * NC, D])
    ph_allgather(C, Hs, Gh, NC)
    ph_halo_rows(C, Gh, selM, hL2[NT * 128:(NT + 1) * 128], NC, D)
    uTc = P.tmp("uTc", [NT + 1, 128, KC, 128], BF16)
    yTb = P.tmp("yTb", [NLT, 128, KC, 128], BF16)
    ltiles = [(i, 0) for i in range(NLT)]
    emit_prenorm(P, cfg, hL2, uTc, g_mix_pre[3], mod, 0, 1, ltiles + [(NT, 0)])
    ph_conv(C, uTc, conv_w_in, conv_cw, yTb, _groups(list(range(NLT)), 8), NT, cedge, D)
    h1 = emit_out_proj(P, cfg, yTb, conv_w_out, hL2, g_mix_post[3], mod, ltiles)
    emit_moe_ffn(P, cfg, h1, hout, g_ffn_pre[3], g_ffn_post[3], mod, routerT[1], moe_w_in[1], moe_w_out[1], ltiles)
    return P.finish()


def run_fused(cfg, inp):
    NC, D, L = cfg.NC, cfg.D, cfg.L
    f32 = np.float32
    g = lambda k: np.ascontiguousarray(np.asarray(inp[k], f32))
    x = g("x")[0]
    ctx = g("ctx")[0]
    cvec = np.stack([g("c")[0], g("c_ctx")])
    cT = np.ascontiguousarray(cvec.reshape(2, cfg.KC, 128).transpose(2, 1, 0))
    CW = D // NC
    ada_w = g("ada_w").reshape(L, D, 6, NC, CW)
    ada_b = g("ada_b").reshape(L, 6, NC, CW)
    rc_h, rs_h = rope_tables(cfg, 128)
    rc_m, rs_m = rope_tables(cfg, cfg.ROPE)
    NH, RD = cfg.NH, cfg.ROPE
    w_uq = g("mla_w_uq")[0].reshape(cfg.QL, NH, 192)
    w_ukv = g("mla_w_ukv")[0].reshape(cfg.KL, NH, 256)
    shared = dict(
        cT=cT, g_mix_pre=g("g_mix_pre"), g_mix_post=g("g_mix_post"), g_ffn_pre=g("g_ffn_pre"),
        g_ffn_post=g("g_ffn_post"), win_w_in=g("win_w_in")[0], win_sink=g("win_sink")[0], win_w_out=g("win_w_out")[0],
        mla_w_in=g("mla_w_in")[0], mla_g_q=g("mla_g_q")[0], mla_g_kv=g("mla_g_kv")[0],
        w_uq_nope=np.ascontiguousarray(w_uq[:, :, :128]).reshape(cfg.QL, NH * 128),
        w_uq_rot=np.ascontiguousarray(w_uq[:, :, 128:]).reshape(cfg.QL, NH * RD),
        w_ukv_nope=np.ascontiguousarray(w_ukv[:, :, :128]).reshape(cfg.KL, NH * 128),
        w_ukv_v=np.ascontiguousarray(w_ukv[:, :, 128:]).reshape(cfg.KL, NH * 128),
        mla_w_out=g("mla_w_out")[0], qkn_w_in=g("qkn_w_in")[0], qkn_g_q=g("qkn_g_q")[0], qkn_g_k=g("qkn_g_k")[0],
        qkn_w_out=g("qkn_w_out")[0], conv_w_in=g("conv_w_in")[0],
        conv_cw=np.ascontiguousarray(g("conv_w")[0].reshape(3, cfg.KC, 128).transpose(2, 1, 0)),
        conv_w_out=g("conv_w_out")[0], ffn_w_in=g("ffn_w_in"), ffn_w_out=g("ffn_w_out"),
        routerT=np.ascontiguousarray(g("moe_router").transpose(0, 2, 1)), moe_w_in=g("moe_w_in"),
        moe_w_out=g("moe_w_out").reshape(2, cfg.E * cfg.DFE, D))
    maps = []
    TL = cfg.TL
    for c in range(NC):
        m = dict(shared)
        m["h0"] = np.concatenate([x[c * TL:(c + 1) * TL], ctx], axis=0)
        m["ada_w"] = np.ascontiguousarray(ada_w[:, :, :, c, :]).reshape(L, D, 6 * CW)
        m["ada_b"] = np.ascontiguousarray(ada_b[:, :, c, :]).reshape(L, 6 * CW)
        m["rope_ch"] = np.ascontiguousarray(rc_h[c * TL:(c + 1) * TL])
        m["rope_sh"] = np.ascontiguousarray(rs_h[c * TL:(c + 1) * TL])
        m["rope_cm"] = np.ascontiguousarray(rc_m[c * TL:(c + 1) * TL])
        m["rope_sm"] = np.ascontiguousarray(rs_m[c * TL:(c + 1) * TL])
        sel = np.zeros((2, NC), f32)
        selM = np.zeros((2 * NC, 2), f32)
        if c > 0:
            sel[0, c - 1] = 1.0
            selM[2 * (c - 1) + 1, 0] = 1.0
        if c < NC - 1:
            sel[1, c + 1] = 1.0
            selM[2 * (c + 1), 1] = 1.0
        m["sel"] = sel
        m["selM"] = selM
        m["wedge"] = np.array([0.0 if c > 0 else -30000.0, 0.0 if c < NC - 1 else -30000.0], f32)
        m["cedge"] = np.array([1.0 if c > 0 else 0.0, 1.0 if c < NC - 1 else 0.0], f32)
        maps.append(m)
    P = _prog(("fused", id(cfg)), lambda: build_fused(cfg))
    res = P.run(maps)
    return np.concatenate([r["hout"] for r in res], axis=0)[None].astype(f32)


def kernel(**inputs):
    return run_fused(_CFG, inputs)
```

```python
import numpy as np
from contextlib import ExitStack, contextmanager
import concourse.bass as bass
import concourse.mybir as mybir
from concourse.bass_utils import run_bass_kernel_spmd

F32 = mybir.dt.float32
BF16 = mybir.dt.bfloat16
AF = mybir.ActivationFunctionType
ALU = mybir.AluOpType
AX = mybir.AxisListType

ENGINES = ("pe", "act", "dve", "pool", "sp")
EMAP = {"pe": "tensor", "act": "scalar", "dve": "vector", "pool": "gpsimd", "sp": "sync"}


class Buf:
    __slots__ = ("name", "lw", "rs", "dsem", "dcnt")

    def __init__(self, name):
        self.name = name
        self.lw = None
        self.rs = {}
        self.dsem = None
        self.dcnt = 0


class Sched:
    def __init__(self, nc, stack):
        self.nc = nc
        self.stack = stack
        self.ops = {e: [] for e in ENGINES}
        self.sems = {}
        self.cnt = {}
        self.known = {e: {} for e in ENGINES}
        self.free_sems = []
        self.nsem = 0
        self.ninst = 0
        for e in ENGINES:
            self._mksem("E_" + e)

    def _mksem(self, key):
        h = self.stack.enter_context(self.nc.semaphore(key))
        self.sems[key] = h
        self.cnt[key] = 0
        self.nsem += 1
        return h

    def _deps(self, reads, writes):
        deps = {}
        for b in reads:
            if b.lw is not None:
                k, v = b.lw
                if deps.get(k, 0) < v:
                    deps[k] = v
        for b in writes:
            if b.lw is not None:
                k, v = b.lw
                if deps.get(k, 0) < v:
                    deps[k] = v
            for k, v in b.rs.items():
                if deps.get(k, 0) < v:
                    deps[k] = v
        return deps

    def _commit(self, tok, reads, writes):
        k, v = tok
        for b in reads:
            if b.rs.get(k, 0) < v:
                b.rs[k] = v
        for b in writes:
            b.lw = tok
            b.rs = {}

    def _waits(self, eng, deps):
        kn = self.known[eng]
        out = []
        for k, v in deps.items():
            if kn.get(k, 0) < v:
                kn[k] = v
                out.append((self.sems[k], v))
        return out

    def op(self, eng, fn, reads=(), writes=()):
        waits = self._waits(eng, self._deps(reads, writes))
        key = "E_" + eng
        self.cnt[key] += 1
        tok = (key, self.cnt[key])
        self.ops[eng].append((waits, fn, (self.sems[key], 1, 0)))
        self._commit(tok, reads, writes)
        self.ninst += 1 + len(waits)
        return tok

    def dma(self, eng, fn, sembuf, n=1, reads=(), writes=(), inc=16):
        deps = self._deps(reads, writes)
        if sembuf.dsem is None:
            if self.free_sems:
                sembuf.dsem = self.free_sems.pop()
            else:
                sembuf.dsem = "B%d" % self.nsem
                self._mksem(sembuf.dsem)
            sembuf.dcnt = self.cnt[sembuf.dsem]
        k = sembuf.dsem
        if self.cnt[k] and deps.get(k, 0) < self.cnt[k]:
            deps[k] = self.cnt[k]
        waits = self._waits(eng, deps)
        self.cnt[k] += inc * n
        sembuf.dcnt = self.cnt[k]
        tok = (k, self.cnt[k])
        self.ops[eng].append((waits, fn, (self.sems[k], inc, n)))
        self._commit(tok, reads, writes)
        self.ninst += n + len(waits)
        return tok

    def release(self, bufs):
        for b in bufs:
            if b.dsem is not None:
                self.free_sems.append(b.dsem)
                b.dsem = None

    def barrier(self):
        finals = {k: v for k, v in self.cnt.items() if v > 0}
        for e in ENGINES:
            waits = self._waits(e, finals)
            if waits:
                self.ops[e].append((waits, None, None))

    def finish(self):
        self.barrier()
        with self.nc.Block() as block:
            for e in ENGINES:
                ops = self.ops[e]

                def body(eng, ops=ops):
                    for waits, fn, sig in ops:
                        for s, v in waits:
                            eng.wait_ge(s, v)
                        if fn is None:
                            continue
                        r = fn(eng)
                        if sig[2] == 0:
                            r.then_inc(sig[0], sig[1])
                        else:
                            rr = r if isinstance(r, (list, tuple)) else [r]
                            assert len(rr) == sig[2], (len(rr), sig[2])
                            for x in rr:
                                x.then_inc(sig[0], sig[1])

                getattr(block, EMAP[e])(body)


class Ctx:
    def __init__(self, nc, stack):
        self.nc = nc
        self.S = Sched(nc, stack)
        self.stack = stack
        self.scopes = []
        self.uid = 0

    @contextmanager
    def phase(self):
        st = ExitStack()
        self.scopes.append((st, []))
        try:
            yield
        finally:
            self.S.barrier()
            st_, bufs = self.scopes.pop()
            self.S.release(bufs)
            st.close()

    def _name(self, name):
        self.uid += 1
        return "%s_%d" % (name, self.uid)

    def sb(self, name, shape, dtype):
        st, bufs = self.scopes[-1]
        t = st.enter_context(self.nc.sbuf_tensor(self._name(name), list(shape), dtype))
        b = Buf(name)
        bufs.append(b)
        return t, b

    def ps(self, name, shape, dtype=F32):
        st, bufs = self.scopes[-1]
        t = st.enter_context(self.nc.psum_tensor(self._name(name), list(shape), dtype))
        b = Buf(name)
        bufs.append(b)
        return t, b

    def newbuf(self, name):
        b = Buf(name)
        self.scopes[-1][1].append(b)
        return b

    def dram(self, name, shape, dtype, kind="Internal"):
        return self.nc.dram_tensor(name, list(shape), dtype, kind=kind).ap()


def ph_prenorm(C, h, uTb, gA, scA, shA, tiles, Dm, eps=1e-6, u32=None):
    S, nc = C.S, C.nc
    KC = Dm // 128
    ncls = len(scA)
    with C.phase():
        make_eps(C, eps)
        G, bG = C.sb("G", [128, Dm], F32)
        ident, bI = C.sb("ident", [128, 128], BF16)
        A, B = [], []
        for c in range(ncls):
            A.append(C.sb("A%d" % c, [128, Dm], F32))
            B.append(C.sb("B%d" % c, [128, Dm], F32))
        S.dma("sp", lambda e: [e.dma_start(out=G[:], in_=gA.partition_broadcast(128))], bG, writes=[bG])
        for c in range(ncls):
            d_, s_ = bcast_src(A[c][0][:], scA[c])
            S.dma("sp", lambda e, d_=d_, s_=s_: [e.dma_start(out=d_, in_=s_)], A[c][1], writes=[A[c][1]])
            d_, s_ = bcast_src(B[c][0][:], shA[c])
            S.dma("sp", lambda e, d_=d_, s_=s_: [e.dma_start(out=d_, in_=s_)], B[c][1], writes=[B[c][1]])
            S.op("dve", lambda e, c=c: e.scalar_tensor_tensor(out=A[c][0][:], in0=A[c][0][:], scalar=1.0,
                                                               in1=G[:], op0=ALU.add, op1=ALU.mult),
                 reads=[bG], writes=[A[c][1]])
        make_identity(C, ident, bI)
        NB = 2
        hb = [C.sb("h%d" % i, [128, Dm], F32) for i in range(NB)]
        sq = C.sb("sq", [128, Dm], BF16)
        ss = [C.sb("ss%d" % i, [128, 1], F32) for i in range(NB)]
        rs = [C.sb("rs%d" % i, [128, 1], F32) for i in range(NB)]
        ub = [C.sb("u%d" % i, [128, Dm], BF16) for i in range(NB)]
        uT = []
        for i in range(NB):
            t_, b_ = C.sb("uT%d" % i, [128, KC, 128], BF16)
            uT.append((t_, [b_] + [C.newbuf("uTg") for _ in range((KC + 3) // 4 - 1)]))
        pst = [C.ps("pst%d" % i, [128, 4, 128], BF16) for i in range(4)]
        for it, (ti, cls) in enumerate(tiles):
            i = it % NB
            ht, bh = hb[i]
            S.dma("sp", lambda e, ht=ht, ti=ti: [e.dma_start(out=ht[:], in_=h[ti * 128:(ti + 1) * 128, :])],
                  bh, writes=[bh])
            S.op("act", lambda e, i=i, ht=ht: e.activation(out=sq[0][:], in_=ht[:], func=AF.Square,
                                                           accum_out=ss[i][0][:]),
                 reads=[bh], writes=[sq[1], ss[i][1]])
            rstd_ops(C, ss[i], rs[i], Dm, eps)
            S.op("dve", lambda e, i=i, ht=ht, cls=cls: e.scalar_tensor_tensor(
                out=ht[:], in0=ht[:], scalar=rs[i][0][:], in1=A[cls][0][:], op0=ALU.mult, op1=ALU.mult),
                reads=[rs[i][1], A[cls][1]], writes=[bh])
            S.op("pool", lambda e, ht=ht, cls=cls: e.tensor_tensor(out=ht[:], in0=ht[:], in1=B[cls][0][:], op=ALU.add),
                 reads=[B[cls][1]], writes=[bh])
            if u32 is not None:
                S.dma("sp", lambda e, ht=ht, ti=ti: [e.dma_start(out=u32[ti * 128:(ti + 1) * 128, :], in_=ht[:])],
                      bh, reads=[bh])
            S.op("act", lambda e, i=i, ht=ht: e.copy(out=ub[i][0][:], in_=ht[:]), reads=[bh], writes=[ub[i][1]])
            transpose_tile(C, ub[i], uT[i], pst, ident, bI, KC)
            S.dma("sp", lambda e, i=i, ti=ti: [e.dma_start(out=uTb[ti], in_=uT[i][0][:])],
                  uT[i][1][0], reads=uT[i][1])


def bcast_src(dst, vec):
    if len(vec.shape) == 1:
        return dst, vec.partition_broadcast(128)
    a = vec.shape[0]
    return dst.rearrange("p (a b) -> p a b", a=a), vec.partition_broadcast(128)


def rstd_ops(C, ss, rs, n, eps, w=1):
    S = C.S
    if not hasattr(C, "epsb"):
        raise RuntimeError("eps tile missing")
    S.op("act", lambda e: e.activation(out=rs[0][:, 0:w], in_=ss[0][:, 0:w], func=AF.Sqrt,
                                       bias=C.epsb[0][:], scale=1.0 / n),
         reads=[ss[1], C.epsb[1]], writes=[rs[1]])
    S.op("dve", lambda e: e.reciprocal(out=rs[0][:, 0:w], in_=rs[0][:, 0:w]), reads=[rs[1]], writes=[rs[1]])


def make_eps(C, eps):
    t, b = C.sb("epsb", [128, 1], F32)
    C.S.op("pool", lambda e: e.memset(t[:], eps), writes=[b])
    C.epsb = (t, b)


def make_identity(C, ident, bI):
    S = C.S
    S.op("pool", lambda e: e.memset(ident[:], 0.0), writes=[bI])
    S.op("pool", lambda e: e.affine_select(out=ident[:], in_=ident[:], pattern=[[-1, 128]],
                                           compare_op=ALU.not_equal, fill=1.0, base=0,
                                           channel_multiplier=1),
         reads=[bI], writes=[bI])


def transpose_tile(C, src, dst, pst, ident, bI, KC, evac=("act", "dve")):
    S = C.S
    st, bs = src
    dt_, bds = dst
    ng = (KC + 3) // 4
    for g in range(ng):
        k0 = g * 4
        kn = min(4, KC - k0)
        pt, bp = pst[g % len(pst)]
        bd = bds[g]

        def tr(e, k0=k0, kn=kn, pt=pt):
            r = None
            for j in range(kn):
                r = e.transpose(pt[:, j, :], st[:, (k0 + j) * 128:(k0 + j + 1) * 128], ident[:])
            return r
        S.op("pe", tr, reads=[bs, bI], writes=[bp])
        ev = evac[g % len(evac)]
        if ev == "act":
            S.op("act", lambda e, k0=k0, kn=kn, pt=pt: e.copy(out=dt_[:, k0:k0 + kn, :], in_=pt[:, 0:kn, :]),
                 reads=[bp], writes=[bd])
        else:
            S.op("dve", lambda e, k0=k0, kn=kn, pt=pt: e.tensor_copy(out=dt_[:, k0:k0 + kn, :], in_=pt[:, 0:kn, :]),
                 reads=[bp], writes=[bd])


class WLoader:
    def __init__(self, C, KC, nblk=2, KP=4, nstage=3, width=512,
                 cast_engs=("act", "dve", "act", "dve", "pool", "act", "dve", "act")):
        self.C, self.KC, self.KP = C, KC, min(KP, KC)
        self.width = width
        self.np_ = (KC + self.KP - 1) // self.KP
        self.blk = []
        for i in range(nblk):
            t, b = C.sb("wb%d" % i, [128, KC, width], BF16)
            self.blk.append((t, [b] + [C.newbuf("wbp") for _ in range(self.np_ - 1)]))
        self.stage = [C.sb("wst%d" % i, [128, self.KP, width], F32) for i in range(nstage)]
        self.ib = 0
        self.ist = 0
        self.cast_engs = cast_engs
        self.ic = 0

    def load(self, Wcols):
        S = self.C.S
        t, bufs = self.blk[self.ib % len(self.blk)]
        self.ib += 1
        w = Wcols.shape[1]
        for p in range(self.np_):
            k0 = p * self.KP
            kn = min(self.KP, self.KC - k0)
            stt, stb = self.stage[self.ist % len(self.stage)]
            self.ist += 1
            src = Wcols[k0 * 128:(k0 + kn) * 128, :].rearrange("(c p) n -> p c n", p=128)
            S.dma("sp", lambda e, stt=stt, src=src, kn=kn, w=w: [e.dma_start(out=stt[:, 0:kn, 0:w], in_=src)],
                  stb, writes=[stb])
            ce = self.cast_engs[self.ic % len(self.cast_engs)]
            self.ic += 1
            if ce == "act":
                S.op(ce, lambda e, t=t, stt=stt, k0=k0, kn=kn, w=w: e.copy(out=t[:, k0:k0 + kn, 0:w],
                                                                           in_=stt[:, 0:kn, 0:w]),
                     reads=[stb], writes=[bufs[p]])
            else:
                S.op(ce, lambda e, t=t, stt=stt, k0=k0, kn=kn, w=w: e.tensor_copy(out=t[:, k0:k0 + kn, 0:w],
                                                                                  in_=stt[:, 0:kn, 0:w]),
                     reads=[stb], writes=[bufs[p]])
        return t, bufs


def load_xT(C, xTb, tiles, KC, name="X", k0=0):
    S = C.S
    n = len(tiles)
    X, b0 = C.sb(name, [128, KC, n * 128], BF16)
    bufs = [b0] + [C.newbuf(name) for _ in range(n - 1)]
    for j, ti in enumerate(tiles):
        src = (xTb(ti) if callable(xTb) else xTb[ti])[:, k0:k0 + KC, :]
        S.dma("sp", lambda e, j=j, src=src: [e.dma_start(out=X[:, :, j * 128:(j + 1) * 128], in_=src)],
              bufs[j], writes=[bufs[j]])
    return X, bufs


def ph_linear_tok(C, xTb, W, out, groups, KC, N, out_dtype=F32, k0=0, width=512):
    S = C.S
    NB = (N + width - 1) // width

    def do_group(grp):
        with C.phase():
            X, xb = load_xT(C, xTb, grp, KC, k0=k0)
            WL = WLoader(C, KC, width=width)
            pss = [C.ps("pl%d" % i, [128, 512], F32) for i in range(4)]
            ost = [C.sb("ost%d" % i, [128, 512], out_dtype) for i in range(4)]
            it = 0
            for nb in range(NB):
                n0 = nb * width
                w = min(width, N - n0)
                wt, wbufs = WL.load(W[:, n0:n0 + w])
                for j, ti in enumerate(grp):
                    pt, pb = pss[it % 4]
                    ot, ob = ost[it % 4]

                    def mm(e, pt=pt, j=j, wt=wt, w=w):
                        r = None
                        for k in range(KC):
                            r = e.matmul(pt[:, 0:w], X[:, k, j * 128:(j + 1) * 128], wt[:, k, 0:w],
                                         start=(k == 0), stop=(k == KC - 1))
                        return r
                    S.op("pe", mm, reads=[xb[j]] + wbufs, writes=[pb])
                    if it % 2 == 0:
                        S.op("act", lambda e, ot=ot, pt=pt, w=w: e.copy(out=ot[:, 0:w], in_=pt[:, 0:w]),
                             reads=[pb], writes=[ob])
                    else:
                        S.op("dve", lambda e, ot=ot, pt=pt, w=w: e.tensor_copy(out=ot[:, 0:w], in_=pt[:, 0:w]),
                             reads=[pb], writes=[ob])
                    S.dma("sp", lambda e, ot=ot, ti=ti, n0=n0, w=w: [e.dma_start(
                        out=out[ti * 128:(ti + 1) * 128, n0:n0 + w], in_=ot[:, 0:w])], ob, reads=[ob])
                    it += 1
    for grp in groups:
        do_group(grp)


def ph_linear_feat(C, xTb, segs, outTb, groups, KC, combT=None, k0=0, store_fn=None):
    S = C.S

    def do_group(grp):
        n = len(grp)
        ntok = n * 128
        chunks = [(c0, min(4, n - c0)) for c0 in range(0, n, 4)]
        with C.phase():
            X, xb = load_xT(C, xTb, grp, KC, k0=k0)
            glu = segs[0]["wu"] is not None
            WL = WLoader(C, KC, nblk=4 if glu else 2, width=max(sg_["wg"].shape[1] for sg_ in segs))
            psg = [C.ps("pg%d" % i, [128, 512], F32) for i in range(3)]
            psu = [C.ps("pu%d" % i, [128, 512], F32) for i in range(3)] if glu else None
            sg = [C.sb("sg%d" % i, [128, 512], F32) for i in range(2)]
            ost = [C.sb("oA%d" % i, [128, 4, 512], BF16) for i in range(2)]
            cmb = None
            cur_e = None
            if combT is not None:
                cmb = C.sb("cmb", [128, ntok], F32)
            it = 0
            io = 0
            for sgm in segs:
                w = sgm["wg"].shape[1]
                ns = w // 128
                wgt, wgb = WL.load(sgm["wg"])
                if glu:
                    wut, wub = WL.load(sgm["wu"])
                if combT is not None and sgm["e"] != cur_e:
                    cur_e = sgm["e"]
                    S.dma("sp", lambda e, ce=cur_e: [e.dma_start(
                        out=cmb[0][:], in_=combT[ce, grp[0] * 128:grp[0] * 128 + ntok].partition_broadcast(128))],
                        cmb[1], writes=[cmb[1]])
                for (c0, cn) in chunks:
                    cw = cn * 128
                    ot, ob = ost[io % 2]
                    io += 1
                    for s in range(ns):
                        pg, pgb = psg[it % 3]
                        rb = xb[c0:c0 + cn]

                        def mmg(e, pg=pg, s=s, c0=c0, cw=cw, wgt=wgt):
                            r = None
                            for k in range(KC):
                                r = e.matmul(pg[:, 0:cw], wgt[:, k, s * 128:(s + 1) * 128],
                                             X[:, k, c0 * 128:c0 * 128 + cw], start=(k == 0), stop=(k == KC - 1))
                            return r
                        S.op("pe", mmg, reads=rb + wgb, writes=[pgb])
                        if glu:
                            pu, pub = psu[it % 3]

                            def mmu(e, pu=pu, s=s, c0=c0, cw=cw, wut=wut):
                                r = None
                                for k in range(KC):
                                    r = e.matmul(pu[:, 0:cw], wut[:, k, s * 128:(s + 1) * 128],
                                                 X[:, k, c0 * 128:c0 * 128 + cw], start=(k == 0), stop=(k == KC - 1))
                                return r
                            S.op("pe", mmu, reads=rb + wub, writes=[pub])
                            sgt, sgb = sg[it % 2]
                            S.op("act", lambda e, sgt=sgt, pg=pg, cw=cw: e.activation(out=sgt[:, 0:cw], in_=pg[:, 0:cw],
                                                                                       func=AF.Silu),
                                 reads=[pgb], writes=[sgb])
                            if combT is not None:
                                S.op("pool", lambda e, sgt=sgt, c0=c0, cw=cw: e.tensor_tensor(
                                    out=sgt[:, 0:cw], in0=sgt[:, 0:cw], in1=cmb[0][:, c0 * 128:c0 * 128 + cw],
                                    op=ALU.mult), reads=[cmb[1]], writes=[sgb])
                            S.op("dve", lambda e, ot=ot, s=s, sgt=sgt, pu=pu, cw=cw: e.tensor_tensor(
                                out=ot[:, s, 0:cw], in0=sgt[:, 0:cw], in1=pu[:, 0:cw], op=ALU.mult),
                                reads=[sgb, pub], writes=[ob])
                        else:
                            if it % 2 == 0:
                                S.op("act", lambda e, ot=ot, s=s, pg=pg, cw=cw: e.copy(out=ot[:, s, 0:cw], in_=pg[:, 0:cw]),
                                     reads=[pgb], writes=[ob])
                            else:
                                S.op("dve", lambda e, ot=ot, s=s, pg=pg, cw=cw: e.tensor_copy(out=ot[:, s, 0:cw],
                                                                                              in_=pg[:, 0:cw]),
                                     reads=[pgb], writes=[ob])
                        it += 1
                    fc = sgm["fc"]

                    if store_fn is not None:
                        def st2(e, ot=ot, c0=c0, cn=cn, fc=fc, ns=ns):
                            return store_fn(e, ot, grp, c0, cn, fc, ns)
                        S.dma("sp", st2, ob, n=ns, reads=[ob])
                    else:
                        def st(e, ot=ot, c0=c0, cn=cn, fc=fc, ns=ns):
                            return [e.dma_start(out=outTb[grp[c0 + j]][:, fc:fc + ns, :],
                                                in_=ot[:, 0:ns, j * 128:(j + 1) * 128]) for j in range(cn)]
                        S.dma("sp", st, ob, n=cn, reads=[ob])
    for grp in groups:
        do_group(grp)


def ph_postnorm(C, y, h, hout, gA, gateA, tiles, Dm, eps=1e-6, y2=None):
    S = C.S
    ncls = len(gateA)
    with C.phase():
        make_eps(C, eps)
        G, bG = C.sb("G", [128, Dm], F32)
        GG = [C.sb("GG%d" % c, [128, Dm], F32) for c in range(ncls)]
        S.dma("sp", lambda e: [e.dma_start(out=G[:], in_=gA.partition_broadcast(128))], bG, writes=[bG])
        for c in range(ncls):
            d_, s_ = bcast_src(GG[c][0][:], gateA[c])
            S.dma("sp", lambda e, d_=d_, s_=s_: [e.dma_start(out=d_, in_=s_)], GG[c][1], writes=[GG[c][1]])
            S.op("pool", lambda e, c=c: e.tensor_tensor(out=GG[c][0][:], in0=GG[c][0][:], in1=G[:], op=ALU.mult),
                 reads=[bG], writes=[GG[c][1]])
        NB = 2
        yb = [C.sb("y%d" % i, [128, Dm], F32) for i in range(NB)]
        y2b = [C.sb("y2_%d" % i, [128, Dm], F32) for i in range(NB)] if y2 is not None else None
        hb = [C.sb("hh%d" % i, [128, Dm], F32) for i in range(NB)]
        sq = [C.sb("sq%d" % i, [128, Dm], BF16) for i in range(NB)]
        ss = [C.sb("ss%d" % i, [128, 1], F32) for i in range(NB)]
        rs = [C.sb("rs%d" % i, [128, 1], F32) for i in range(NB)]
        for it, (ti, cls) in enumerate(tiles):
            i = it % NB
            yt, by = yb[i]
            ht, bh = hb[i]
            S.dma("sp", lambda e, yt=yt, ti=ti: [e.dma_start(out=yt[:], in_=y[ti * 128:(ti + 1) * 128, :])],
                  by, writes=[by])
            S.dma("sp", lambda e, ht=ht, ti=ti: [e.dma_start(out=ht[:], in_=h[ti * 128:(ti + 1) * 128, :])],
                  bh, writes=[bh])
            if y2 is not None:
                y2t, by2 = y2b[i]
                S.dma("sp", lambda e, y2t=y2t, ti=ti: [e.dma_start(out=y2t[:], in_=y2[ti * 128:(ti + 1) * 128, :])],
                      by2, writes=[by2])
                S.op("pool", lambda e, yt=yt, y2t=y2t: e.tensor_tensor(out=yt[:], in0=yt[:], in1=y2t[:], op=ALU.add),
                     reads=[by2], writes=[by])
            S.op("act", lambda e, i=i, yt=yt: e.activation(out=sq[i][0][:], in_=yt[:], func=AF.Square,
                                                           accum_out=ss[i][0][:]),
                 reads=[by], writes=[sq[i][1], ss[i][1]])
            rstd_ops(C, ss[i], rs[i], Dm, eps)
            S.op("dve", lambda e, i=i, yt=yt, cls=cls: e.scalar_tensor_tensor(
                out=yt[:], in0=yt[:], scalar=rs[i][0][:], in1=GG[cls][0][:], op0=ALU.mult, op1=ALU.mult),
                reads=[rs[i][1], GG[cls][1]], writes=[by])
            S.op("pool", lambda e, yt=yt, ht=ht: e.tensor_tensor(out=ht[:], in0=yt[:], in1=ht[:], op=ALU.add),
                 reads=[by], writes=[bh])
            S.dma("sp", lambda e, ht=ht, ti=ti: [e.dma_start(out=hout[ti * 128:(ti + 1) * 128, :], in_=ht[:])],
                  bh, reads=[bh])


def rope_ops(C, x, bx, H, dh, cs, sn, bcs, out, bout, tmp):
    S = C.S
    hp = dh // 2
    xv = x.rearrange("p (h i two) -> p h i two", h=H, two=2)
    ov = out.rearrange("p (h i two) -> p h i two", h=H, two=2)
    xe, xo = xv[:, :, :, 0], xv[:, :, :, 1]
    oe, oo = ov[:, :, :, 0], ov[:, :, :, 1]
    cb = cs.unsqueeze(1).broadcast_to([128, H, hp])
    sb_ = sn.unsqueeze(1).broadcast_to([128, H, hp])
    (t1, b1), (t2, b2) = tmp
    t1v = t1[:, 0:H * hp].rearrange("p (h i) -> p h i", h=H)
    t2v = t2[:, 0:H * hp].rearrange("p (h i) -> p h i", h=H)
    S.op("dve", lambda e: e.tensor_tensor(out=t1v, in0=xe, in1=cb, op=ALU.mult), reads=[bx, bcs], writes=[b1])
    S.op("pool", lambda e: e.tensor_tensor(out=t2v, in0=xo, in1=sb_, op=ALU.mult), reads=[bx, bcs], writes=[b2])
    S.op("dve", lambda e: e.tensor_tensor(out=oe, in0=t1v, in1=t2v, op=ALU.subtract), reads=[b1, b2], writes=[bout])
    S.op("pool", lambda e: e.tensor_tensor(out=t1v, in0=xe, in1=sb_, op=ALU.mult), reads=[bx, bcs], writes=[b1])
    S.op("dve", lambda e: e.tensor_tensor(out=t2v, in0=xo, in1=cb, op=ALU.mult), reads=[bx, bcs], writes=[b2])
    S.op("pool", lambda e: e.tensor_tensor(out=oo, in0=t1v, in1=t2v, op=ALU.add), reads=[b1, b2], writes=[bout])


def ph_qkpost(C, qkv, qTb, kTb, vout, tiles, HQ, HK, dh, rope_cs, rope_sn, qknorm=None, eps=1e-6):
    S = C.S
    H = HQ + HK
    W = H * dh
    hp = dh // 2
    with C.phase():
        make_eps(C, eps)
        ident, bI = C.sb("ident", [128, 128], BF16)
        make_identity(C, ident, bI)
        Gqk = None
        if qknorm is not None:
            g1 = C.sb("g1", [128, 2, dh], F32)
            S.dma("sp", lambda e: [e.dma_start(out=g1[0][:, 0, :], in_=qknorm[0].partition_broadcast(128)),
                                   e.dma_start(out=g1[0][:, 1, :], in_=qknorm[1].partition_broadcast(128))],
                  g1[1], n=2, writes=[g1[1]])
            Gqk = C.sb("Gqk", [128, H, dh], F32)
            S.op("pool", lambda e: e.tensor_copy(out=Gqk[0][:, 0:HQ, :],
                                                 in_=g1[0][:, 0:1, :].broadcast_to([128, HQ, dh])),
                 reads=[g1[1]], writes=[Gqk[1]])
            S.op("pool", lambda e: e.tensor_copy(out=Gqk[0][:, HQ:H, :],
                                                 in_=g1[0][:, 1:2, :].broadcast_to([128, HK, dh])),
                 reads=[g1[1]], writes=[Gqk[1]])
        NB = 2
        xb = [C.sb("x%d" % i, [128, W + HK * dh], F32) for i in range(NB)]
        csb = [C.sb("cs%d" % i, [128, 2, hp], F32) for i in range(NB)]
        qb = [C.sb("qk%d" % i, [128, W], BF16) for i in range(NB)]
        vb = [C.sb("v%d" % i, [128, HK * dh], BF16) for i in range(NB)]
        tmp = [C.sb("rt%d" % i, [128, max(H * hp, W if qknorm is not None else 0)], F32) for i in range(2)]
        ss = C.sb("ssq", [128, H], F32)
        rs = C.sb("rsq", [128, H], F32)
        qT = []
        for i in range(NB):
            t_, b_ = C.sb("qT%d" % i, [128, H, 128], BF16)
            qT.append((t_, [b_] + [C.newbuf("qTg") for _ in range((H + 3) // 4 - 1)]))
        pst = [C.ps("pst%d" % i, [128, 4, 128], BF16) for i in range(4)]
        for it, (ti, use_rope) in enumerate(tiles):
            i = it % NB
            xt, bx = xb[i]
            S.dma("sp", lambda e, xt=xt, ti=ti: [e.dma_start(out=xt[:], in_=qkv[ti * 128:(ti + 1) * 128, :])],
                  bx, writes=[bx])
            S.op("act", lambda e, i=i, xt=xt: e.copy(out=vb[i][0][:], in_=xt[:, W:W + HK * dh]),
                 reads=[bx], writes=[vb[i][1]])
            S.dma("sp", lambda e, i=i, ti=ti: [e.dma_start(out=vout[ti * 128:(ti + 1) * 128, :], in_=vb[i][0][:])],
                  vb[i][1], reads=[vb[i][1]])
            xq = xt[:, 0:W]
            if qknorm is not None:
                sqv = tmp[0][0][:, 0:W]
                S.op("pool", lambda e, xq=xq, sqv=sqv: e.tensor_tensor(out=sqv, in0=xq, in1=xq, op=ALU.mult),
                     reads=[bx], writes=[tmp[0][1]])
                S.op("dve", lambda e, sqv=sqv: e.tensor_reduce(out=ss[0][:], in_=sqv.rearrange("p (h d) -> p h d", h=H),
                                                               axis=AX.X, op=ALU.add),
                     reads=[tmp[0][1]], writes=[ss[1]])
                rstd_ops(C, ss, rs, dh, eps, w=H)
                xq3 = xq.rearrange("p (h d) -> p h d", h=H)
                S.op("dve", lambda e, xq3=xq3: e.tensor_tensor(out=xq3, in0=xq3,
                                                               in1=rs[0][:, 0:H].unsqueeze(2).broadcast_to([128, H, dh]),
                                                               op=ALU.mult),
                     reads=[rs[1]], writes=[bx])
                S.op("pool", lambda e, xq3=xq3: e.tensor_tensor(out=xq3, in0=xq3, in1=Gqk[0][:], op=ALU.mult),
                     reads=[Gqk[1]], writes=[bx])
            if use_rope:
                ct, bc = csb[i]
                S.dma("sp", lambda e, ct=ct, ti=ti: [
                    e.dma_start(out=ct[:, 0, :], in_=rope_cs[ti * 128:(ti + 1) * 128, :]),
                    e.dma_start(out=ct[:, 1, :], in_=rope_sn[ti * 128:(ti + 1) * 128, :])], bc, n=2, writes=[bc])
                rope_ops(C, xq, bx, H, dh, ct[:, 0, :], ct[:, 1, :], bc, qb[i][0][:], qb[i][1], tmp)
            else:
                S.op("act", lambda e, i=i, xq=xq: e.copy(out=qb[i][0][:], in_=xq), reads=[bx], writes=[qb[i][1]])
            transpose_tile(C, qb[i], qT[i], pst, ident, bI, H)
            S.dma("sp", lambda e, i=i, ti=ti: [e.dma_start(out=qTb[ti], in_=qT[i][0][:, 0:HQ, :]),
                                               e.dma_start(out=kTb[ti], in_=qT[i][0][:, HQ:H, :])],
                  qT[i][1][0], n=2, reads=qT[i][1])


class AttnRes:
    def __init__(self, C):
        self.C = C
        self.psS = [C.ps("aS%d" % i, [128, 512], F32) for i in range(3)]
        self.psO = [C.ps("aO%d" % i, [128, 512], F32) for i in range(2)]
        self.psR = [C.ps("aR%d" % i, [128, 512], F32) for i in range(1)]
        self.pT = [C.sb("pT%d" % i, [128, 512], BF16) for i in range(3)]
        self.acc = [C.sb("acc%d" % i, [128, 512], F32) for i in range(2)]
        self.rcp = [C.sb("rcp%d" % i, [128, 512], F32) for i in range(2)]
        self.oo = [C.sb("oo%d" % i, [128, 512], BF16) for i in range(2)]
        self.ones = C.sb("ones", [128, 128], F32)
        C.S.op("pool", lambda e: e.memset(self.ones[0][:], 1.0), writes=[self.ones[1]])
        self.iS = 0
        self.iJ = 0


def attn_job(C, R, kbs, s_terms, v_lhsT, N, scale, out_fn, nout, sink_ap=None, sink_bufs=()):
    S = C.S
    j = R.iJ
    R.iJ += 1
    pO, bO = R.psO[j % 2]
    acc, bacc = R.acc[j % 2]
    nk = len(kbs)
    pend = []

    def emit_pv(idx, kb, pt, bpt):
        vl, vbufs = v_lhsT(kb)
        S.op("pe", lambda e: e.matmul(pO[:, 0:N], vl, pt[:, 0:N], start=(idx == 0), stop=(idx == nk - 1)),
             reads=[bpt] + list(vbufs), writes=[bO])
        if idx == 0:
            S.op("dve", lambda e: e.tensor_copy(out=acc[:, 0:N], in_=pt[:, 0:N]), reads=[bpt], writes=[bacc])
        else:
            S.op("dve", lambda e: e.tensor_tensor(out=acc[:, 0:N], in0=acc[:, 0:N], in1=pt[:, 0:N], op=ALU.add),
                 reads=[bpt], writes=[bacc])

    for idx, kb in enumerate(kbs):
        pS, bS = R.psS[R.iS % 3]
        pt, bpt = R.pT[R.iS % 3]
        R.iS += 1
        terms = s_terms(kb)

        def smm(e, terms=terms, pS=pS):
            r = None
            for ti_, (l, rr, _) in enumerate(terms):
                r = e.matmul(pS[:, 0:N], l, rr, start=(ti_ == 0), stop=(ti_ == len(terms) - 1))
            return r
        rb = []
        for (_, _, b) in terms:
            rb += list(b)
        S.op("pe", smm, reads=rb, writes=[bS])
        S.op("act", lambda e, pS=pS, pt=pt: e.activation(out=pt[:, 0:N], in_=pS[:, 0:N], func=AF.Exp, scale=scale),
             reads=[bS], writes=[bpt])
        pend.append((idx, kb, pt, bpt))
        if len(pend) > 1:
            emit_pv(*pend.pop(0))
    while pend:
        emit_pv(*pend.pop(0))
    pR, bR = R.psR[0]
    S.op("pe", lambda e: e.matmul(pR[:, 0:N], R.ones[0][:], acc[:, 0:N], start=True, stop=True),
         reads=[bacc, R.ones[1]], writes=[bR])
    rc, brc = R.rcp[j % 2]
    if sink_ap is not None:
        S.op("dve", lambda e: e.tensor_tensor(out=rc[:, 0:N], in0=pR[:, 0:N], in1=sink_ap, op=ALU.add),
             reads=[bR] + list(sink_bufs), writes=[brc])
        S.op("dve", lambda e: e.reciprocal(out=rc[:, 0:N], in_=rc[:, 0:N]), reads=[brc], writes=[brc])
    else:
        S.op("dve", lambda e: e.reciprocal(out=rc[:, 0:N], in_=pR[:, 0:N]), reads=[bR], writes=[brc])
    ot, bo = R.oo[j % 2]
    S.op("dve", lambda e: e.tensor_tensor(out=ot[:, 0:N], in0=pO[:, 0:N], in1=rc[:, 0:N], op=ALU.mult),
         reads=[bO, brc], writes=[bo])
    S.dma("sp", lambda e: out_fn(e, ot), bo, n=nout, reads=[bo])


def ph_attn_window(C, qTb, kblocks, vblocks, oTb, sink, edge, NLT, NCT, HQ, HK, scale, halo=None):
    S = C.S
    rep = HQ // HK
    NKB = NLT + 2 + NCT
    N = rep * 128
    with C.phase():
        R = AttnRes(C)
        ident, bI = C.sb("ident", [128, 128], BF16)
        make_identity(C, ident, bI)
        kT = C.sb("kT", [128, NKB, HK, 128], BF16)
        vv = C.sb("vv", [128, NKB, HK * 128], BF16)
        kl = [kb for kb in range(NKB) if kblocks[kb] is not None]
        S.dma("sp", lambda e: [e.dma_start(out=kT[0][:, kb], in_=kblocks[kb]) for kb in kl], kT[1], n=len(kl),
              writes=[kT[1]])
        S.dma("sp", lambda e: [e.dma_start(out=vv[0][:, kb], in_=vblocks[kb]) for kb in kl], vv[1], n=len(kl),
              writes=[vv[1]])
        if halo is not None:
            Hg, sel = halo
            NCc = Hg.shape[0]
            selt = C.sb("selt", [128, 2, NCc], F32)
            S.dma("sp", lambda e: [e.dma_start(out=selt[0][:], in_=sel.partition_broadcast(128))], selt[1],
                  writes=[selt[1]])
            cand = [C.sb("cand%d" % i, [128, NCc, HK * 128], BF16) for i in range(2)]
            jobs = [(kT[0][:, 0].rearrange("p h k -> p (h k)"), kT[1], 1, 0),
                    (kT[0][:, NLT + 1].rearrange("p h k -> p (h k)"), kT[1], 0, 1),
                    (vv[0][:, 0], vv[1], 3, 0), (vv[0][:, NLT + 1], vv[1], 2, 1)]
            for ji, (dst, bd, which, side) in enumerate(jobs):
                ct, cb_ = cand[ji % 2]
                S.dma("sp", lambda e, ct=ct, which=which: [e.dma_start(
                    out=ct[:], in_=Hg[:, which].rearrange("c p f -> p c f"))], cb_, writes=[cb_])
                for c in range(NCc):
                    if c == 0:
                        S.op("dve", lambda e, dst=dst, ct=ct, side=side: e.tensor_scalar(
                            out=dst, in0=ct[:, 0, :], scalar1=selt[0][:, side, 0:1], scalar2=None, op0=ALU.mult),
                            reads=[cb_, selt[1]], writes=[bd])
                    else:
                        S.op("dve", lambda e, dst=dst, ct=ct, side=side, c=c: e.scalar_tensor_tensor(
                            out=dst, in0=ct[:, c, :], scalar=selt[0][:, side, c:c + 1], in1=dst,
                            op0=ALU.mult, op1=ALU.add), reads=[cb_, selt[1]], writes=[bd])
        es = C.sb("es", [128, HQ], F32)
        S.dma("sp", lambda e: [e.dma_start(out=es[0][:], in_=sink.partition_broadcast(128))], es[1], writes=[es[1]])
        S.op("act", lambda e: e.activation(out=es[0][:], in_=es[0][:], func=AF.Exp), reads=[es[1]], writes=[es[1]])
        eg = C.sb("edge", [128, 2], F32)
        S.dma("sp", lambda e: [e.dma_start(out=eg[0][:], in_=edge.partition_broadcast(128))], eg[1], writes=[eg[1]])
        mk = {}
        for nm in ("prev", "next", "prev0", "nextL"):
            mk[nm] = C.sb("m" + nm, [128, rep, 128], BF16)
        mf = C.sb("mf", [128, rep, 128], F32)
        for nm, cm, pat in (("prev", 1, [[0, rep], [-1, 128]]), ("next", -1, [[0, rep], [1, 128]])):
            S.op("pool", lambda e: e.memset(mf[0][:], 0.0), writes=[mf[1]])
            S.op("pool", lambda e, cm=cm, pat=pat: e.affine_select(out=mf[0][:], in_=mf[0][:], pattern=pat,
                                                                   compare_op=ALU.is_ge, fill=-30000.0, base=0,
                                                                   channel_multiplier=cm),
                 reads=[mf[1]], writes=[mf[1]])
            S.op("dve", lambda e, nm=nm: e.tensor_copy(out=mk[nm][0][:], in_=mf[0][:]), reads=[mf[1]], writes=[mk[nm][1]])
            en, col = ("prev0", 0) if nm == "prev" else ("nextL", 1)
            S.op("dve", lambda e, en=en, col=col: e.tensor_scalar(out=mk[en][0][:], in0=mf[0][:],
                                                                  scalar1=eg[0][:, col:col + 1], scalar2=None,
                                                                  op0=ALU.add),
                 reads=[mf[1], eg[1]], writes=[mk[en][1]])
        qb = [C.sb("q%d" % i, [128, HQ, 128], BF16) for i in range(2)]
        NT = NLT + NCT
        for t in range(NT):
            qt, bq = qb[t % 2]
            S.dma("sp", lambda e, qt=qt, t=t: [e.dma_start(out=qt[:], in_=qTb[t])], bq, writes=[bq])
            if t < NLT:
                kbs = [NLT + 2 + c for c in range(NCT)] + [t, t + 1, t + 2]
            else:
                kbs = [NLT + 2 + c for c in range(NCT)]
            for g in range(HK):
                rhs = qt[:, g * rep:(g + 1) * rep, :]

                def s_terms(kb, g=g, rhs=rhs, bq=bq, t=t):
                    terms = [(kT[0][:, kb, g, :], rhs, [kT[1], bq])]
                    if t < NLT and kb == t:
                        m = mk["prev0"] if t == 0 else mk["prev"]
                        terms.append((ident[:], m[0][:], [bI, m[1]]))
                    if t < NLT and kb == t + 2:
                        m = mk["nextL"] if t == NLT - 1 else mk["next"]
                        terms.append((ident[:], m[0][:], [bI, m[1]]))
                    return terms

                def v_lhsT(kb, g=g):
                    return vv[0][:, kb, g * 128:(g + 1) * 128], [vv[1]]
                sk = es[0][:, g * rep:(g + 1) * rep].unsqueeze(2).broadcast_to([128, rep, 128])

                def out_fn(e, ot, t=t, g=g):
                    return [e.dma_start(out=oTb[t][:, g * rep:(g + 1) * rep, :], in_=ot[:, 0:N])]
                attn_job(C, R, kbs, s_terms, v_lhsT, N, scale, out_fn, 1, sink_ap=sk, sink_bufs=[es[1]])


def ph_attn_tok(C, qTb, oTb, NH, kv_of, k_loader, v_loader, NLT, NCT, NKB, scale, qrTb=None, kr_loader=None):
    S = C.S
    with C.phase():
        R = AttnRes(C)
        kT = C.sb("kT", [128, NKB * 128], BF16)
        vv = C.sb("vv", [128, NKB, 128], BF16)
        kr = None
        if kr_loader is not None:
            kr = C.sb("kr", [64, NKB * 128], BF16)
            S.dma("sp", lambda e: kr_loader[0](e, kr[0], 0), kr[1], n=kr_loader[1], writes=[kr[1]])
        qb = [C.sb("q%d" % i, [128, 512], BF16) for i in range(2)]
        qrb = [C.sb("qr%d" % i, [64, 512], BF16) for i in range(2)] if qrTb is not None else None
        jobs = [(t0, min(4, NLT - t0), None) for t0 in range(0, NLT, 4)]
        if NCT:
            jobs.append((NLT, NCT, list(range(NKB - NCT, NKB))))
        cur_kv = None
        ij = 0
        for hh in range(NH):
            kv = kv_of(hh)
            if kv != cur_kv:
                cur_kv = kv
                S.dma("sp", lambda e, kv=kv: k_loader[0](e, kT[0], kv), kT[1], n=k_loader[1], writes=[kT[1]])
                S.dma("sp", lambda e, kv=kv: v_loader[0](e, vv[0], kv), vv[1], n=v_loader[1], writes=[vv[1]])
            for (t0, cn, kbl) in jobs:
                N = cn * 128
                qt, bq = qb[ij % 2]
                S.dma("sp", lambda e, qt=qt, hh=hh, t0=t0, cn=cn, N=N: [e.dma_start(
                    out=qt[:, 0:N].rearrange("p (t k) -> p t k", k=128),
                    in_=qTb[t0:t0 + cn][:, :, hh, :].rearrange("t p k -> p t k"))], bq, writes=[bq])
                if qrTb is not None:
                    qrt, bqr = qrb[ij % 2]
                    po = (hh % 2) * 64
                    S.dma("sp", lambda e, qrt=qrt, hh=hh, t0=t0, cn=cn, N=N, po=po: [e.dma_start(
                        out=qrt[:, 0:N].rearrange("p (t k) -> p t k", k=128),
                        in_=qrTb[t0:t0 + cn][:, po:po + 64, hh // 2, :].rearrange("t p k -> p t k"))],
                        bqr, writes=[bqr])
                ij += 1
                kbs = kbl if kbl is not None else list(range(NKB))

                def s_terms(kb, qt=qt, bq=bq, N=N):
                    terms = [(kT[0][:, kb * 128:(kb + 1) * 128], qt[:, 0:N], [kT[1], bq])]
                    if qrTb is not None:
                        terms.append((kr[0][:, kb * 128:(kb + 1) * 128], qrt[:, 0:N], [kr[1], bqr]))
                    return terms

                def v_lhsT(kb):
                    return vv[0][:, kb, :], [vv[1]]

                def out_fn(e, ot, hh=hh, t0=t0, cn=cn, N=N):
                    return [e.dma_start(out=oTb[t0:t0 + cn][:, :, hh, :].rearrange("t p k -> p t k"),
                                        in_=ot[:, 0:N].rearrange("p (t k) -> p t k", k=128))]
                attn_job(C, R, kbs, s_terms, v_lhsT, N, scale, out_fn, 1)


def ph_allgather(C, src2d, dst2d, ncores):
    S = C.S
    with C.phase():
        cb = C.newbuf("cc")
        S.dma("pool", lambda e: [e.collective_compute("AllGather", ALU.bypass, replica_groups=[list(range(ncores))],
                                                      ins=[src2d], outs=[dst2d])], cb, n=1, inc=1)


def ph_copy_rows(C, pairs, width, dtype):
    S = C.S
    with C.phase():
        tb = [C.sb("cp%d" % i, [128, width], dtype) for i in range(2)]
        for i, (dst, src) in enumerate(pairs):
            t, b = tb[i % 2]
            r = src.shape[0]
            S.dma("sp", lambda e, t=t, src=src, r=r: [e.dma_start(out=t[0:r, :], in_=src)], b, writes=[b])
            S.dma("sp", lambda e, t=t, dst=dst, r=r: [e.dma_start(out=dst, in_=t[0:r, :])], b, reads=[b])


def ph_halo_rows(C, Gh, selM, hdst, ncores, Dm):
    S = C.S
    R2 = 2 * ncores
    with C.phase():
        gs = C.sb("gs", [R2, Dm], F32)
        sm = C.sb("sm", [R2, 2], F32)
        ho = C.sb("ho", [128, Dm], F32)
        S.dma("sp", lambda e: [e.dma_start(out=gs[0][:], in_=Gh)], gs[1], writes=[gs[1]])
        S.dma("sp", lambda e: [e.dma_start(out=sm[0][:], in_=selM)], sm[1], writes=[sm[1]])
        S.op("pool", lambda e: e.memset(ho[0][:], 0.0), writes=[ho[1]])
        ps = [C.ps("ph%d" % i, [128, 512], F32) for i in range(2)]
        for i, n0 in enumerate(range(0, Dm, 512)):
            pt, pb = ps[i % 2]
            S.op("pe", lambda e, pt=pt, n0=n0: e.matmul(pt[0:2, :], sm[0][:], gs[0][:, n0:n0 + 512], start=True, stop=True),
                 reads=[gs[1], sm[1]], writes=[pb])
            S.op("act", lambda e, pt=pt, n0=n0: e.copy(out=ho[0][0:2, n0:n0 + 512], in_=pt[0:2, :]),
                 reads=[pb], writes=[ho[1]])
        S.dma("sp", lambda e: [e.dma_start(out=hdst, in_=ho[0][:])], ho[1], reads=[ho[1]])


def ph_attn_full(C, qTh, kTh, vh, oTh, HL, NKV, TQL, TQC, NKB, scale, qrTh=None, krT=None, ctx_kb=()):
    S = C.S
    with C.phase():
        R = AttnRes(C)
        kT = C.sb("kT", [128, NKB * 128], BF16)
        vv = C.sb("vv", [128, NKB, 128], BF16)
        kr = None
        if krT is not None:
            kr = C.sb("kr", [64, NKB * 128], BF16)
            S.dma("sp", lambda e: [e.dma_start(out=kr[0][:], in_=krT)], kr[1], writes=[kr[1]])
        qb = [C.sb("q%d" % i, [128, 512], BF16) for i in range(2)]
        qrb = [C.sb("qr%d" % i, [64, 512], BF16) for i in range(2)] if qrTh is not None else None
        jobs = [(c0, 512, None) for c0 in range(0, TQL, 512)]
        if TQC:
            jobs.append((TQL, TQC, list(ctx_kb)))
        cur_kv = None
        ij = 0
        for hh in range(HL):
            kv = hh if NKV == HL else 0
            if kv != cur_kv:
                cur_kv = kv
                S.dma("sp", lambda e, kv=kv: [e.dma_start(out=kT[0][:], in_=kTh[kv])], kT[1], writes=[kT[1]])
                S.dma("sp", lambda e, kv=kv: [e.dma_start(out=vv[0][:], in_=vh[kv])], vv[1], writes=[vv[1]])
            for (c0, N, kbl) in jobs:
                qt, bq = qb[ij % 2]
                S.dma("sp", lambda e, qt=qt, hh=hh, c0=c0, N=N: [e.dma_start(out=qt[:, 0:N], in_=qTh[hh][:, c0:c0 + N])],
                      bq, writes=[bq])
                if qrTh is not None:
                    qrt, bqr = qrb[ij % 2]
                    S.dma("sp", lambda e, qrt=qrt, hh=hh, c0=c0, N=N: [e.dma_start(out=qrt[:, 0:N],
                                                                                  in_=qrTh[hh][:, c0:c0 + N])],
                          bqr, writes=[bqr])
                ij += 1
                kbs = kbl if kbl is not None else list(range(NKB))

                def s_terms(kb, qt=qt, bq=bq, N=N):
                    terms = [(kT[0][:, kb * 128:(kb + 1) * 128], qt[:, 0:N], [kT[1], bq])]
                    if qrTh is not None:
                        terms.append((kr[0][:, kb * 128:(kb + 1) * 128], qrt[:, 0:N], [kr[1], bqr]))
                    return terms

                def v_lhsT(kb):
                    return vv[0][:, kb, :], [vv[1]]

                def out_fn(e, ot, hh=hh, c0=c0, N=N):
                    return [e.dma_start(out=oTh[hh][:, c0:c0 + N], in_=ot[:, 0:N])]
                attn_job(C, R, kbs, s_terms, v_lhsT, N, scale, out_fn, 1)


def ph_ada(C, cT, W, b, out, L, Dm, NC):
    S = C.S
    KC = Dm // 128
    KP = min(8, KC)
    with C.phase():
        sc = C.sb("sc", [128, KC, 2], F32)
        S.dma("sp", lambda e: [e.dma_start(out=sc[0][:], in_=cT)], sc[1], writes=[sc[1]])
        S.op("act", lambda e: e.activation(out=sc[0][:], in_=sc[0][:], func=AF.Silu), reads=[sc[1]], writes=[sc[1]])
        bb = C.sb("bb", [2, L, NC], F32)
        S.dma("sp", lambda e: [e.dma_start(out=bb[0][:], in_=b.partition_broadcast(2))], bb[1], writes=[bb[1]])
        npieces = (KC + KP - 1) // KP
        wst = [C.sb("aw%d" % i, [128, KP, 512], F32) for i in range(4)]
        ps = [C.ps("pa%d" % i, [128, 512], F32) for i in range(2)]
        ob = [C.sb("ao%d" % i, [2, 512], F32) for i in range(2)]
        ist = 0
        it = 0
        for l in range(L):
            for n0 in range(0, NC, 512):
                w = min(512, NC - n0)
                pt, pb = ps[it % 2]
                for p in range(npieces):
                    k0 = p * KP
                    kn = min(KP, KC - k0)
                    wt, wb = wst[ist % 4]
                    ist += 1
                    src = W[l][k0 * 128:(k0 + kn) * 128, n0:n0 + w].rearrange("(c p) n -> p c n", p=128)
                    S.dma("sp", lambda e, wt=wt, src=src, kn=kn, w=w: [e.dma_start(out=wt[:, 0:kn, 0:w], in_=src)],
                          wb, writes=[wb])

                    def mm(e, wt=wt, pt=pt, k0=k0, kn=kn, w=w):
                        r = None
                        for k in range(kn):
                            r = e.matmul(pt[0:2, 0:w], sc[0][:, k0 + k, :], wt[:, k, 0:w],
                                         start=(k0 + k == 0), stop=(k0 + k == KC - 1))
                        return r
                    S.op("pe", mm, reads=[sc[1], wb], writes=[pb])
                ot, obb = ob[it % 2]
                S.op("dve", lambda e, ot=ot, pt=pt, l=l, n0=n0, w=w: e.tensor_tensor(
                    out=ot[:, 0:w], in0=pt[0:2, 0:w], in1=bb[0][:, l, n0:n0 + w], op=ALU.add),
                    reads=[pb, bb[1]], writes=[obb])
                S.dma("sp", lambda e, ot=ot, l=l, n0=n0, w=w: [e.dma_start(out=out[l][:, n0:n0 + w], in_=ot[:, 0:w])],
                      obb, reads=[obb])
                it += 1


def ph_mla_post(C, call, cqTb, ckrTb, g_q, g_kv, tiles, QL, KL, RD, rope_cs, rope_sn, eps=1e-6):
    S = C.S
    Wc = QL + KL
    KCc = Wc // 128
    hp = RD // 2
    with C.phase():
        make_eps(C, eps)
        ident, bI = C.sb("ident", [128, 128], BF16)
        make_identity(C, ident, bI)
        G = C.sb("G", [128, Wc], F32)
        S.dma("sp", lambda e: [e.dma_start(out=G[0][:, 0:QL], in_=g_q.partition_broadcast(128)),
                               e.dma_start(out=G[0][:, QL:Wc], in_=g_kv.partition_broadcast(128))],
              G[1], n=2, writes=[G[1]])
        NB = 2
        xb = [C.sb("x%d" % i, [128, Wc + RD], F32) for i in range(NB)]
        sq = C.sb("sq", [128, Wc], BF16)
        ss = [C.sb("ss%d" % i, [128, 2], F32) for i in range(NB)]
        rs = [C.sb("rs%d" % i, [128, 2], F32) for i in range(NB)]
        cn = [C.sb("cn%d" % i, [128, Wc], BF16) for i in range(NB)]
        krb = [C.sb("krb%d" % i, [128, 128], BF16) for i in range(NB)]
        csb = [C.sb("cs%d" % i, [128, 2, hp], F32) for i in range(NB)]
        tmp = [C.sb("rt%d" % i, [128, hp], F32) for i in range(2)]
        cT, krT = [], []
        for i in range(NB):
            t_, b_ = C.sb("cT%d" % i, [128, KCc, 128], BF16)
            cT.append((t_, [b_] + [C.newbuf("cTg") for _ in range((KCc + 3) // 4 - 1)]))
            t_, b_ = C.sb("krT%d" % i, [128, 1, 128], BF16)
            krT.append((t_, [b_]))
        pst = [C.ps("pst%d" % i, [128, 4, 128], BF16) for i in range(4)]
        for i in range(NB):
            S.op("pool", lambda e, i=i: e.memset(krb[i][0][:], 0.0), writes=[krb[i][1]])
        for it, (ti, use_rope) in enumerate(tiles):
            i = it % NB
            xt, bx = xb[i]
            S.dma("sp", lambda e, xt=xt, ti=ti: [e.dma_start(out=xt[:], in_=call[ti * 128:(ti + 1) * 128, :])],
                  bx, writes=[bx])
            S.op("act", lambda e, i=i, xt=xt: e.activation(out=sq[0][:, 0:QL], in_=xt[:, 0:QL], func=AF.Square,
                                                           accum_out=ss[i][0][:, 0:1]),
                 reads=[bx], writes=[sq[1], ss[i][1]])
            S.op("act", lambda e, i=i, xt=xt: e.activation(out=sq[0][:, QL:Wc], in_=xt[:, QL:Wc], func=AF.Square,
                                                           accum_out=ss[i][0][:, 1:2]),
                 reads=[bx], writes=[sq[1], ss[i][1]])
            S.op("dve", lambda e, i=i: e.tensor_scalar(out=ss[i][0][:, 0:1], in0=ss[i][0][:, 0:1],
                                                       scalar1=float(KL) / QL, scalar2=None, op0=ALU.mult),
                 reads=[ss[i][1]], writes=[ss[i][1]])
            rstd_ops(C, ss[i], rs[i], KL, eps, w=2)
            S.op("dve", lambda e, i=i, xt=xt: e.scalar_tensor_tensor(
                out=cn[i][0][:, 0:QL], in0=xt[:, 0:QL], scalar=rs[i][0][:, 0:1], in1=G[0][:, 0:QL],
                op0=ALU.mult, op1=ALU.mult), reads=[bx, rs[i][1], G[1]], writes=[cn[i][1]])
            S.op("dve", lambda e, i=i, xt=xt: e.scalar_tensor_tensor(
                out=cn[i][0][:, QL:Wc], in0=xt[:, QL:Wc], scalar=rs[i][0][:, 1:2], in1=G[0][:, QL:Wc],
                op0=ALU.mult, op1=ALU.mult), reads=[bx, rs[i][1], G[1]], writes=[cn[i][1]])
            transpose_tile(C, cn[i], cT[i], pst, ident, bI, KCc)
            S.dma("sp", lambda e, i=i, ti=ti: [e.dma_start(out=cqTb[ti], in_=cT[i][0][:, 0:QL // 128, :]),
                                               e.dma_start(out=ckrTb[ti][:, 0:KL // 128, :], in_=cT[i][0][:, QL // 128:KCc, :])],
                  cT[i][1][0], n=2, reads=cT[i][1])
            xk = xt[:, Wc:Wc + RD]
            if use_rope:
                ct, bc = csb[i]
                S.dma("sp", lambda e, ct=ct, ti=ti: [
                    e.dma_start(out=ct[:, 0, :], in_=rope_cs[ti * 128:(ti + 1) * 128, :]),
                    e.dma_start(out=ct[:, 1, :], in_=rope_sn[ti * 128:(ti + 1) * 128, :])], bc, n=2, writes=[bc])
                rope_ops(C, xk, bx, 1, RD, ct[:, 0, :], ct[:, 1, :], bc, krb[i][0][:, 0:RD], krb[i][1], tmp)
            else:
                S.op("act", lambda e, i=i, xk=xk: e.copy(out=krb[i][0][:, 0:RD], in_=xk), reads=[bx], writes=[krb[i][1]])
            transpose_tile(C, krb[i], krT[i], pst, ident, bI, 1)
            S.dma("sp", lambda e, i=i, ti=ti: [e.dma_start(out=ckrTb[ti][:, KL // 128:KL // 128 + 1, :], in_=krT[i][0][:])],
                  krT[i][1][0], reads=krT[i][1])


def ph_rope_heads(C, x, outTb, tiles, H, dh, rope_cs, rope_sn):
    S = C.S
    W = H * dh
    KC = W // 128
    hp = dh // 2
    with C.phase():
        ident, bI = C.sb("ident", [128, 128], BF16)
        make_identity(C, ident, bI)
        NB = 2
        xb = [C.sb("x%d" % i, [128, W], F32) for i in range(NB)]
        ob = [C.sb("o%d" % i, [128, W], BF16) for i in range(NB)]
        csb = [C.sb("cs%d" % i, [128, 2, hp], F32) for i in range(NB)]
        tmp = [C.sb("rt%d" % i, [128, H * hp], F32) for i in range(2)]
        oT = []
        for i in range(NB):
            t_, b_ = C.sb("oT%d" % i, [128, KC, 128], BF16)
            oT.append((t_, [b_] + [C.newbuf("oTg") for _ in range((KC + 3) // 4 - 1)]))
        pst = [C.ps("pst%d" % i, [128, 4, 128], BF16) for i in range(4)]
        for it, (ti, use_rope) in enumerate(tiles):
            i = it % NB
            xt, bx = xb[i]
            S.dma("sp", lambda e, xt=xt, ti=ti: [e.dma_start(out=xt[:], in_=x[ti * 128:(ti + 1) * 128, :])],
                  bx, writes=[bx])
            if use_rope:
                ct, bc = csb[i]
                S.dma("sp", lambda e, ct=ct, ti=ti: [
                    e.dma_start(out=ct[:, 0, :], in_=rope_cs[ti * 128:(ti + 1) * 128, :]),
                    e.dma_start(out=ct[:, 1, :], in_=rope_sn[ti * 128:(ti + 1) * 128, :])], bc, n=2, writes=[bc])
                rope_ops(C, xt[:], bx, H, dh, ct[:, 0, :], ct[:, 1, :], bc, ob[i][0][:], ob[i][1], tmp)
            else:
                S.op("act", lambda e, i=i, xt=xt: e.copy(out=ob[i][0][:], in_=xt[:]), reads=[bx], writes=[ob[i][1]])
            transpose_tile(C, ob[i], oT[i], pst, ident, bI, KC)
            S.dma("sp", lambda e, i=i, ti=ti: [e.dma_start(out=outTb[ti], in_=oT[i][0][:])], oT[i][1][0], reads=oT[i][1])


def ph_router(C, u32, routerT, combT, tiles, Dm, E):
    S = C.S
    with C.phase():
        Rt = C.sb("Rt", [128, E, Dm], F32)
        S.dma("sp", lambda e: [e.dma_start(out=Rt[0][:, ee, :], in_=routerT[ee].partition_broadcast(128))
                               for ee in range(E)], Rt[1], n=E, writes=[Rt[1]])
        identf = C.sb("identf", [128, 128], F32)
        S.op("pool", lambda e: e.memset(identf[0][:], 0.0), writes=[identf[1]])
        S.op("pool", lambda e: e.affine_select(out=identf[0][:], in_=identf[0][:], pattern=[[-1, 128]],
                                               compare_op=ALU.not_equal, fill=1.0, base=0, channel_multiplier=1),
             reads=[identf[1]], writes=[identf[1]])
        NB = 2
        ub = [C.sb("u%d" % i, [128, Dm], F32) for i in range(NB)]
        junk = C.sb("junk", [128, Dm], BF16)
        lg = [C.sb("lg%d" % i, [128, E], F32) for i in range(NB)]
        mx = [C.sb("mx%d" % i, [128, 8], F32) for i in range(NB)]
        gt = [C.sb("gt%d" % i, [128, 4], F32) for i in range(NB)]
        cb = [C.sb("cb%d" % i, [128, 2, E], F32) for i in range(NB)]
        cto = [C.sb("cto%d" % i, [E, 128], F32) for i in range(NB)]
        pc = [C.ps("pc%d" % i, [128, 128], F32) for i in range(2)]
        assert E == 8
        for it, ti in enumerate(tiles):
            i = it % NB
            ut, bu = ub[i]
            S.dma("sp", lambda e, ut=ut, ti=ti: [e.dma_start(out=ut[:], in_=u32[ti * 128:(ti + 1) * 128, :])],
                  bu, writes=[bu])
            for ee in range(E):
                S.op("dve", lambda e, ut=ut, ee=ee, i=i: e.scalar_tensor_tensor(
                    out=junk[0][:], in0=ut[:], scalar=1.0, in1=Rt[0][:, ee, :],
                    op0=ALU.mult, op1=ALU.mult, accum_out=lg[i][0][:, ee:ee + 1]),
                    reads=[bu, Rt[1]], writes=[junk[1], lg[i][1]])
            S.op("dve", lambda e, i=i: e.max(out=mx[i][0][:], in_=lg[i][0][:]), reads=[lg[i][1]], writes=[mx[i][1]])
            S.op("dve", lambda e, i=i: e.tensor_tensor(out=gt[i][0][:, 0:1], in0=mx[i][0][:, 0:1], in1=mx[i][0][:, 1:2],
                                                       op=ALU.subtract), reads=[mx[i][1]], writes=[gt[i][1]])
            S.op("act", lambda e, i=i: e.activation(out=gt[i][0][:, 1:2], in_=gt[i][0][:, 0:1], func=AF.Sigmoid),
                 reads=[gt[i][1]], writes=[gt[i][1]])
            S.op("dve", lambda e, i=i: e.tensor_scalar(out=gt[i][0][:, 2:3], in0=gt[i][0][:, 1:2], scalar1=-1.0,
                                                       scalar2=1.0, op0=ALU.mult, op1=ALU.add),
                 reads=[gt[i][1]], writes=[gt[i][1]])
            S.op("dve", lambda e, i=i: e.tensor_scalar(out=cb[i][0][:, 0, :], in0=lg[i][0][:], scalar1=mx[i][0][:, 0:1],
                                                       scalar2=gt[i][0][:, 1:2], op0=ALU.is_equal, op1=ALU.mult),
                 reads=[lg[i][1], mx[i][1], gt[i][1]], writes=[cb[i][1]])
            S.op("dve", lambda e, i=i: e.tensor_scalar(out=cb[i][0][:, 1, :], in0=lg[i][0][:], scalar1=mx[i][0][:, 1:2],
                                                       scalar2=gt[i][0][:, 2:3], op0=ALU.is_equal, op1=ALU.mult),
                 reads=[lg[i][1], mx[i][1], gt[i][1], cb[i][1]], writes=[cb[i][1]])
            S.op("dve", lambda e, i=i: e.tensor_tensor(out=cb[i][0][:, 0, :], in0=cb[i][0][:, 0, :], in1=cb[i][0][:, 1, :],
                                                       op=ALU.add), reads=[cb[i][1]], writes=[cb[i][1]])
            pt, pb = pc[it % 2]
            S.op("pe", lambda e, i=i, pt=pt: e.matmul(pt[0:E, :], cb[i][0][:, 0, :], identf[0][:], start=True, stop=True),
                 reads=[cb[i][1], identf[1]], writes=[pb])
            S.op("act", lambda e, i=i, pt=pt: e.copy(out=cto[i][0][:], in_=pt[0:E, :]), reads=[pb], writes=[cto[i][1]])
            S.dma("sp", lambda e, i=i, ti=ti: [e.dma_start(out=combT[:, ti * 128:(ti + 1) * 128], in_=cto[i][0][:])],
                  cto[i][1], reads=[cto[i][1]])


def ph_conv(C, xTb, W, cw, outTb, groups, halo_tile, edge, Dm):
    S = C.S
    KC = Dm // 128
    first_tile = groups[0][0]
    last_tile = groups[-1][-1]

    def do_group(grp):
        n = len(grp)
        ntok = n * 128
        chunks = [(c0, min(3, n - c0)) for c0 in range(0, n, 3)]
        with C.phase():
            X, b0 = C.sb("Xc", [128, KC, ntok + 2], BF16)
            xb = [b0] + [C.newbuf("Xc") for _ in range(n + 1)]
            for j, ti in enumerate(grp):
                S.dma("sp", lambda e, j=j, ti=ti: [e.dma_start(out=X[:, :, 1 + j * 128:1 + (j + 1) * 128], in_=xTb[ti])],
                      xb[j], writes=[xb[j]])
            eg = C.sb("edge", [128, 2], F32)
            S.dma("sp", lambda e: [e.dma_start(out=eg[0][:], in_=edge.partition_broadcast(128))], eg[1], writes=[eg[1]])
            for side, col, bidx in ((0, 0, n), (1, ntok + 1, n + 1)):
                if side == 0:
                    src = xTb[halo_tile][:, :, 0:1] if grp[0] == first_tile else xTb[grp[0] - 1][:, :, 127:128]
                    is_edge = grp[0] == first_tile
                else:
                    src = xTb[halo_tile][:, :, 1:2] if grp[-1] == last_tile else xTb[grp[-1] + 1][:, :, 0:1]
                    is_edge = grp[-1] == last_tile
                S.dma("sp", lambda e, src=src, col=col: [e.dma_start(out=X[:, :, col:col + 1], in_=src,
                                                                     allow_slow_non_contiguous=True)],
                      xb[bidx], writes=[xb[bidx]])
                if is_edge:
                    S.op("dve", lambda e, col=col, side=side: e.tensor_scalar(
                        out=X[:, :, col:col + 1], in0=X[:, :, col:col + 1], scalar1=eg[0][:, side:side + 1],
                        scalar2=None, op0=ALU.mult), reads=[eg[1]], writes=[xb[bidx]])
            cwt = C.sb("cw", [128, KC, 3], F32)
            S.dma("sp", lambda e: [e.dma_start(out=cwt[0][:], in_=cw)], cwt[1], writes=[cwt[1]])
            WL = WLoader(C, KC, nblk=6, width=128, KP=8, nstage=3)
            psb = [C.ps("cb%d" % i, [128, 512], F32) for i in range(2)]
            psc = [C.ps("cc%d" % i, [128, 512], F32) for i in range(2)]
            psv = [C.ps("cv%d" % i, [128, 512], F32) for i in range(2)]
            vsb = [C.sb("vs%d" % i, [128, 512], F32) for i in range(2)]
            pr = [C.sb("pr%d" % i, [128, 512], F32) for i in range(2)]
            zz = [C.sb("zz%d" % i, [128, 512], F32) for i in range(2)]
            ost = [C.sb("oc%d" % i, [128, 384], BF16) for i in range(2)]
            it = 0
            for f in range(KC):
                wts = [WL.load(W[:, q * Dm + f * 128:q * Dm + (f + 1) * 128]) for q in range(3)]
                for (c0, cn) in chunks:
                    cw_ = cn * 128
                    i = it % 2
                    it += 1
                    rb = list(xb[c0:c0 + cn])
                    if c0 == 0:
                        rb.append(xb[n])
                    else:
                        rb.append(xb[c0 - 1])
                    if c0 + cn == n:
                        rb.append(xb[n + 1])
                    else:
                        rb.append(xb[c0 + cn])
                    pss = (psb[i], psc[i], psv[i])
                    for q in range(3):
                        def mm(e, q=q, c0=c0, cw_=cw_, pt=pss[q][0], wt=wts[q][0]):
                            r = None
                            for k in range(KC):
                                r = e.matmul(pt[:, 0:cw_ + 2], wt[:, k, 0:128], X[:, k, c0 * 128:c0 * 128 + cw_ + 2],
                                             start=(k == 0), stop=(k == KC - 1))
                            return r
                        S.op("pe", mm, reads=rb + wts[q][1], writes=[pss[q][1]])
                    n2 = cw_ + 2
                    S.op("act", lambda e, i=i, n2=n2: e.copy(out=vsb[i][0][:, 0:n2], in_=psv[i][0][:, 0:n2]),
                         reads=[psv[i][1]], writes=[vsb[i][1]])
                    S.op("dve", lambda e, i=i, n2=n2: e.tensor_tensor(out=pr[i][0][:, 0:n2], in0=psc[i][0][:, 0:n2],
                                                                      in1=vsb[i][0][:, 0:n2], op=ALU.mult),
                         reads=[psc[i][1], vsb[i][1]], writes=[pr[i][1]])
                    S.op("dve", lambda e, i=i, f=f, cw_=cw_: e.tensor_scalar(
                        out=zz[i][0][:, 0:cw_], in0=pr[i][0][:, 1:cw_ + 1], scalar1=cwt[0][:, f, 1:2], scalar2=None,
                        op0=ALU.mult), reads=[pr[i][1], cwt[1]], writes=[zz[i][1]])
                    S.op("dve", lambda e, i=i, f=f, cw_=cw_: e.scalar_tensor_tensor(
                        out=zz[i][0][:, 0:cw_], in0=pr[i][0][:, 0:cw_], scalar=cwt[0][:, f, 0:1], in1=zz[i][0][:, 0:cw_],
                        op0=ALU.mult, op1=ALU.add), reads=[pr[i][1], cwt[1]], writes=[zz[i][1]])
                    S.op("dve", lambda e, i=i, f=f, cw_=cw_: e.scalar_tensor_tensor(
                        out=zz[i][0][:, 0:cw_], in0=pr[i][0][:, 2:cw_ + 2], scalar=cwt[0][:, f, 2:3], in1=zz[i][0][:, 0:cw_],
                        op0=ALU.mult, op1=ALU.add), reads=[pr[i][1], cwt[1]], writes=[zz[i][1]])
                    S.op("dve", lambda e, i=i, cw_=cw_: e.tensor_tensor(
                        out=ost[i][0][:, 0:cw_], in0=zz[i][0][:, 0:cw_], in1=psb[i][0][:, 1:cw_ + 1], op=ALU.mult),
                        reads=[zz[i][1], psb[i][1]], writes=[ost[i][1]])

                    def st(e, i=i, c0=c0, cn=cn, f=f):
                        return [e.dma_start(out=outTb[grp[c0 + j]][:, f, :], in_=ost[i][0][:, j * 128:(j + 1) * 128])
                                for j in range(cn)]
                    S.dma("sp", st, ost[i][1], n=cn, reads=[ost[i][1]])
    for grp in groups:
        do_group(grp)


import ml_dtypes

NPBF = ml_dtypes.bfloat16


class Cfg:
    def __init__(self, D=4096, SEQ=16384, CTX=256, NC=8, GRID_W=64, QL=None, KL=None, DFF=None, DFE=None):
        self.D, self.SEQ, self.CTX, self.NC, self.GRID_W = D, SEQ, CTX, NC, GRID_W
        self.HD = 128
        self.NH = D // 128
        self.NKV = self.NH // 4
        self.REP = 4
        self.QL = QL or D // 4
        self.KL = KL or D // 8
        self.NOPE, self.ROPE, self.VH = 128, 64, 128
        self.DFF = DFF or (5 * D) // 4
        self.E = 8
        self.DFE = DFE or D // 4
        self.L = 4
        self.KC = D // 128
        self.TL = SEQ // NC
        self.NLT = self.TL // 128
        self.NCT = CTX // 128
        self.T = self.TL + CTX
        self.NT = self.NLT + self.NCT
        self.HL = self.NH // NC
        self.TQ = SEQ + CTX
        self.NKB = self.TQ // 128
        self.GT = 9


def _groups(tiles, gt):
    ng = (len(tiles) + gt - 1) // gt
    per = (len(tiles) + ng - 1) // ng
    return [tiles[i:i + per] for i in range(0, len(tiles), per)]


class Prog:
    def __init__(self, name):
        self.name = name
        self.nc = bass.Bass("TRN2", target_bir_lowering=False)
        self.stack = ExitStack()
        self.C = Ctx(self.nc, self.stack)
        self.ins = {}
        self.outs = {}

    def inp(self, name, shape, dtype=F32):
        ap = self.C.dram(name, shape, dtype, "ExternalInput")
        self.ins[name] = (tuple(shape), dtype)
        return ap

    def out(self, name, shape, dtype=F32):
        ap = self.C.dram(name, shape, dtype, "ExternalOutput")
        self.outs[name] = (tuple(shape), dtype)
        return ap

    def tmp(self, name, shape, dtype=F32):
        self.ntmp = getattr(self, "ntmp", 0) + 1
        return self.C.dram("%s_t%d" % (name, self.ntmp), shape, dtype, "Internal")

    def finish(self):
        self.C.S.finish()
        self.stack.close()
        return self

    def run(self, in_maps):
        n = len(in_maps)
        maps = []
        for m in in_maps:
            mm = {}
            for k, (shape, dt) in self.ins.items():
                a = m[k]
                want = NPBF if dt == BF16 else np.float32
                a = np.ascontiguousarray(a)
                assert a.dtype == want, (k, a.dtype)
                assert tuple(a.shape) == shape, (k, a.shape, shape)
                mm[k] = a
            maps.append(mm)
        res = run_bass_kernel_spmd(self.nc, maps, core_ids=list(range(n)))
        return res.results


def lat_ctx_tiles(cfg, ctx=True):
    t = [(i, 0) for i in range(cfg.NLT)]
    if ctx:
        t += [(cfg.NLT + i, 1) for i in range(cfg.NCT)]
    return t


def emit_prenorm(P, cfg, h, uTb, g, mod, shift_i, scale_i, tiles, u32=None):
    ncls = 2
    ph_prenorm(P.C, h, uTb, g, [mod[c, scale_i] for c in range(ncls)], [mod[c, shift_i] for c in range(ncls)],
               tiles, cfg.D, u32=u32)


def emit_postnorm(P, cfg, y, h, hout, g, mod, gate_i, tiles, y2=None):
    ph_postnorm(P.C, y, h, hout, g, [mod[c, gate_i] for c in range(2)], tiles, cfg.D, y2=y2)


def emit_dense_ffn(P, cfg, h1, hout, g_pre, g_post, mod, w_in, w_out, tiles):
    C = P.C
    tl = [t for t, _ in tiles]
    ntl = max(tl) + 1
    uTb = P.tmp("f_uTb", [ntl, 128, cfg.KC, 128], BF16)
    aTb = P.tmp("f_aTb", [ntl, 128, cfg.DFF // 128, 128], BF16)
    z = P.tmp("f_z", [ntl * 128, cfg.D], F32)
    emit_prenorm(P, cfg, h1, uTb, g_pre, mod, 3, 4, tiles)
    segs = []
    F = cfg.DFF
    for n0 in range(0, F, 256):
        w = min(256, F - n0)
        segs.append(dict(wg=w_in[:, n0:n0 + w], wu=w_in[:, F + n0:F + n0 + w], fc=n0 // 128, e=None))
    groups = _groups(tl, cfg.GT)
    ph_linear_feat(C, uTb, segs, aTb, groups, cfg.KC)
    ph_linear_tok(C, aTb, w_out, z, groups, F // 128, cfg.D)
    emit_postnorm(P, cfg, z, h1, hout, g_post, mod, 5, tiles)


def emit_moe_ffn(P, cfg, h1, hout, g_pre, g_post, mod, routerT, w_in, w_out, tiles):
    C = P.C
    tl = [t for t, _ in tiles]
    ntl = max(tl) + 1
    E, DFE = cfg.E, cfg.DFE
    uTb = P.tmp("m_uTb", [ntl, 128, cfg.KC, 128], BF16)
    u32 = P.tmp("m_u32", [ntl * 128, cfg.D], F32)
    combT = P.tmp("m_combT", [E, ntl * 128], F32)
    aTb = P.tmp("m_aTb", [ntl, 128, E * DFE // 128, 128], BF16)
    z = P.tmp("m_z", [ntl * 128, cfg.D], F32)
    emit_prenorm(P, cfg, h1, uTb, g_pre, mod, 3, 4, tiles, u32=u32)
    ph_router(C, u32, routerT, combT, tl, cfg.D, E)
    segs = []
    for e in range(E):
        for n0 in range(0, DFE, 256):
            w = min(256, DFE - n0)
            segs.append(dict(wg=w_in[e][:, n0:n0 + w], wu=w_in[e][:, DFE + n0:DFE + n0 + w],
                             fc=(e * DFE + n0) // 128, e=e))
    groups = _groups(tl, cfg.GT)
    ph_linear_feat(C, uTb, segs, aTb, groups, cfg.KC, combT=combT)
    KC2 = E * DFE // 128
    if KC2 > 32:
        assert KC2 == 64
        z2 = P.tmp("m_z2", [ntl * 128, cfg.D], F32)
        ph_linear_tok(C, aTb, w_out[0:32 * 128], z, groups, 32, cfg.D, k0=0)
        ph_linear_tok(C, aTb, w_out[32 * 128:64 * 128], z2, groups, 32, cfg.D, k0=32)
        emit_postnorm(P, cfg, z, h1, hout, g_post, mod, 5, tiles, y2=z2)
    else:
        ph_linear_tok(C, aTb, w_out, z, groups, KC2, cfg.D)
        emit_postnorm(P, cfg, z, h1, hout, g_post, mod, 5, tiles)


def common_inputs(P, cfg, nrows=None):
    h = P.inp("h", [nrows or cfg.T, cfg.D])
    mod = P.inp("mod", [2, 6, cfg.D])
    return h, mod


def build_ada(cfg):
    P = Prog("ada")
    NCOL = 6 * cfg.D // cfg.NC
    cT = P.inp("cT", [128, cfg.KC, 2])
    W = P.inp("ada_w", [cfg.L, cfg.D, NCOL])
    b = P.inp("ada_b", [cfg.L, NCOL])
    out = P.out("modp", [cfg.L, 2, NCOL])
    ph_ada(P.C, cT, W, b, out, cfg.L, cfg.D, NCOL)
    return P.finish()


def build_gqa_a(cfg, qknorm):
    P = Prog("gqa_a")
    C = P.C
    h, mod = common_inputs(P, cfg)
    g = P.inp("g_pre", [cfg.D])
    NQKV = (cfg.NH + 2 * cfg.NKV) * 128
    w_in = P.inp("w_in", [cfg.D, NQKV])
    rc = P.inp("rope_c", [cfg.TL, 64])
    rs_ = P.inp("rope_s", [cfg.TL, 64])
    qn = None
    if qknorm:
        qn = (P.inp("g_q", [128]), P.inp("g_k", [128]))
    qTb = P.out("qTb", [cfg.NT, 128, cfg.NH, 128], BF16)
    kTb = P.out("kTb", [cfg.NT, 128, cfg.NKV, 128], BF16)
    v = P.out("v", [cfg.T, cfg.NKV * 128], BF16)
    uTb = P.tmp("uTb", [cfg.NT, 128, cfg.KC, 128], BF16)
    qkv = P.tmp("qkv", [cfg.T, NQKV], F32)
    tiles = lat_ctx_tiles(cfg)
    emit_prenorm(P, cfg, h, uTb, g, mod, 0, 1, tiles)
    ph_linear_tok(C, uTb, w_in, qkv, _groups([t for t, _ in tiles], cfg.GT), cfg.KC, NQKV)
    ph_qkpost(C, qkv, qTb, kTb, v, [(t, c == 0) for t, c in tiles], cfg.NH, cfg.NKV, 128, rc, rs_, qknorm=qn)
    return P.finish()


def emit_out_proj(P, cfg, oTb, w_out, h, g_post, mod, tiles):
    tl = [t for t, _ in tiles]
    ntl = max(tl) + 1
    y = P.tmp("o_y", [ntl * 128, cfg.D], F32)
    h1 = P.tmp("o_h1", [ntl * 128, cfg.D], F32)
    ph_linear_tok(P.C, oTb, w_out, y, _groups(tl, cfg.GT), cfg.KC, cfg.D)
    emit_postnorm(P, cfg, y, h, h1, g_post, mod, 2, tiles)
    return h1


def ffn_inputs_dense(P, cfg):
    return dict(g_pre=P.inp("g_ffn_pre", [cfg.D]), g_post=P.inp("g_ffn_post", [cfg.D]),
                w_in=P.inp("ffn_w_in", [cfg.D, 2 * cfg.DFF]), w_out=P.inp("ffn_w_out", [cfg.DFF, cfg.D]))


def ffn_inputs_moe(P, cfg):
    return dict(g_pre=P.inp("g_ffn_pre", [cfg.D]), g_post=P.inp("g_ffn_post", [cfg.D]),
                routerT=P.inp("routerT", [cfg.E, cfg.D]),
                w_in=P.inp("moe_w_in", [cfg.E, cfg.D, 2 * cfg.DFE]), w_out=P.inp("moe_w_out", [cfg.E * cfg.DFE, cfg.D]))


def build_win_b(cfg):
    P = Prog("win_b")
    h, mod = common_inputs(P, cfg)
    NKB = cfg.NLT + 2 + cfg.NCT
    qTb = P.inp("qTb", [cfg.NT, 128, cfg.NH, 128], BF16)
    kTx = P.inp("kTx", [NKB, 128, cfg.NKV, 128], BF16)
    vx = P.inp("vx", [NKB, 128, cfg.NKV * 128], BF16)
    sink = P.inp("sink", [cfg.NH])
    edge = P.inp("edge", [2])
    w_out = P.inp("w_out", [cfg.D, cfg.D])
    g_post = P.inp("g_mix_post", [cfg.D])
    f = ffn_inputs_dense(P, cfg)
    hout = P.out("hout", [cfg.T, cfg.D])
    oTb = P.tmp("oTb", [cfg.NT, 128, cfg.NH, 128], BF16)
    tiles = lat_ctx_tiles(cfg)
    ph_attn_window(P.C, qTb, [kTx[kb] for kb in range(NKB)], [vx[kb] for kb in range(NKB)], oTb, sink, edge,
                   cfg.NLT, cfg.NCT, cfg.NH, cfg.NKV, 128 ** -0.5)
    h1 = emit_out_proj(P, cfg, oTb, w_out, h, g_post, mod, tiles)
    emit_dense_ffn(P, cfg, h1, hout, f["g_pre"], f["g_post"], mod, f["w_in"], f["w_out"], tiles)
    return P.finish()


def build_mla_a(cfg):
    P = Prog("mla_a")
    C = P.C
    h, mod = common_inputs(P, cfg)
    g = P.inp("g_pre", [cfg.D])
    QL, KL, RD, NH = cfg.QL, cfg.KL, cfg.ROPE, cfg.NH
    w_in = P.inp("w_in", [cfg.D, QL + KL + RD])
    g_q = P.inp("g_q", [QL])
    g_kv = P.inp("g_kv", [KL])
    w_uqn = P.inp("w_uq_nope", [QL, NH * 128])
    w_uqr = P.inp("w_uq_rot", [QL, NH * RD])
    w_ukn = P.inp("w_ukv_nope", [KL, NH * 128])
    w_ukv = P.inp("w_ukv_v", [KL, NH * 128])
    rc = P.inp("rope_c", [cfg.TL, RD // 2])
    rs_ = P.inp("rope_s", [cfg.TL, RD // 2])
    qnTb = P.out("qnTb", [cfg.NT, 128, NH, 128], BF16)
    qrTb = P.out("qrTb", [cfg.NT, 128, NH * RD // 128, 128], BF16)
    knTb = P.out("knTb", [cfg.NT, 128, NH, 128], BF16)
    ckrTb = P.out("ckrTb", [cfg.NT, 128, KL // 128 + 1, 128], BF16)
    v = P.out("v", [cfg.T, NH * 128], BF16)
    uTb = P.tmp("uTb", [cfg.NT, 128, cfg.KC, 128], BF16)
    call = P.tmp("call", [cfg.T, QL + KL + RD], F32)
    cqTb = P.tmp("cqTb", [cfg.NT, 128, QL // 128, 128], BF16)
    qr = P.tmp("qr", [cfg.T, NH * RD], F32)
    tiles = lat_ctx_tiles(cfg)
    tl = [t for t, _ in tiles]
    groups = _groups(tl, cfg.GT)
    emit_prenorm(P, cfg, h, uTb, g, mod, 0, 1, tiles)
    ph_linear_tok(C, uTb, w_in, call, groups, cfg.KC, QL + KL + RD)
    ph_mla_post(C, call, cqTb, ckrTb, g_q, g_kv, [(t, c == 0) for t, c in tiles], QL, KL, RD, rc, rs_)
    segs = [dict(wg=w_uqn[:, n0:n0 + 512], wu=None, fc=n0 // 128, e=None) for n0 in range(0, NH * 128, 512)]
    ph_linear_feat(C, cqTb, segs, qnTb, groups, QL // 128, k0=0)
    ph_linear_tok(C, cqTb, w_uqr, qr, groups, QL // 128, NH * RD, k0=0)
    ph_rope_heads(C, qr, qrTb, [(t, c == 0) for t, c in tiles], NH, RD, rc, rs_)
    segs = [dict(wg=w_ukn[:, n0:n0 + 512], wu=None, fc=n0 // 128, e=None) for n0 in range(0, NH * 128, 512)]
    ph_linear_feat(C, ckrTb, segs, knTb, groups, KL // 128, k0=0)
    ph_linear_tok(C, ckrTb, w_ukv, v, groups, KL // 128, NH * 128, out_dtype=BF16, k0=0)
    return P.finish()


def build_attn_full(cfg, mla):
    P = Prog("attn_full")
    HL, TQ, NKB = cfg.HL, cfg.TQ, cfg.NKB
    NKVl = HL if mla else 1
    qTh = P.inp("qTh", [HL, 128, TQ], BF16)
    kTh = P.inp("kTh", [NKVl, 128, TQ], BF16)
    vh = P.inp("vh", [NKVl, 128, NKB, 128], BF16)
    qrTh = krT = None
    if mla:
        qrTh = P.inp("qrTh", [HL, 64, TQ], BF16)
        krT = P.inp("krT", [64, TQ], BF16)
    oTh = P.out("oTh", [HL, 128, TQ], BF16)
    scale = (192 if mla else 128) ** -0.5
    ctx_kb = [cfg.SEQ // 128 + i for i in range(cfg.NCT)]
    ph_attn_full(P.C, qTh, kTh, vh, oTh, HL, NKVl, cfg.SEQ, cfg.CTX, NKB, scale, qrTh=qrTh, krT=krT, ctx_kb=ctx_kb)
    return P.finish()


def build_tail(cfg, moe):
    P = Prog("tail")
    h, mod = common_inputs(P, cfg)
    oTb = P.inp("oTb", [cfg.NT, 128, cfg.NH, 128], BF16)
    w_out = P.inp("w_out", [cfg.D, cfg.D])
    g_post = P.inp("g_mix_post", [cfg.D])
    f = ffn_inputs_moe(P, cfg) if moe else ffn_inputs_dense(P, cfg)
    hout = P.out("hout", [cfg.T, cfg.D])
    tiles = lat_ctx_tiles(cfg)
    h1 = emit_out_proj(P, cfg, oTb, w_out, h, g_post, mod, tiles)
    if moe:
        emit_moe_ffn(P, cfg, h1, hout, f["g_pre"], f["g_post"], mod, f["routerT"], f["w_in"], f["w_out"], tiles)
    else:
        emit_dense_ffn(P, cfg, h1, hout, f["g_pre"], f["g_post"], mod, f["w_in"], f["w_out"], tiles)
    return P.finish()


def build_conv_layer(cfg):
    P = Prog("conv")
    C = P.C
    NLT = cfg.NLT
    h, mod = common_inputs(P, cfg, nrows=(NLT + 1) * 128)
    g = P.inp("g_pre", [cfg.D])
    W = P.inp("conv_w_in", [cfg.D, 3 * cfg.D])
    cw = P.inp("conv_w", [128, cfg.KC, 3])
    edge = P.inp("edge", [2])
    w_out = P.inp("w_out", [cfg.D, cfg.D])
    g_post = P.inp("g_mix_post", [cfg.D])
    f = ffn_inputs_moe(P, cfg)
    hout = P.out("hout", [NLT * 128, cfg.D])
    uTb = P.tmp("uTb", [NLT + 1, 128, cfg.KC, 128], BF16)
    yTb = P.tmp("yTb", [NLT, 128, cfg.KC, 128], BF16)
    tiles_h = [(i, 0) for i in range(NLT + 1)]
    tiles = [(i, 0) for i in range(NLT)]
    tl = list(range(NLT))
    emit_prenorm(P, cfg, h, uTb, g, mod, 0, 1, tiles_h)
    ph_conv(C, uTb, W, cw, yTb, _groups(tl, 8), NLT, edge, cfg.D)
    h1 = emit_out_proj(P, cfg, yTb, w_out, h, g_post, mod, tiles)
    emit_moe_ffn(P, cfg, h1, hout, f["g_pre"], f["g_post"], mod, f["routerT"], f["w_in"], f["w_out"], tiles)
    return P.finish()


_PROGS = {}


def _prog(key, fn):
    if key not in _PROGS:
        _PROGS[key] = fn()
    return _PROGS[key]


def rope_tables(cfg, rot_dim):
    n_freq = rot_dim // 4
    inv = (np.float32(10000.0) ** (-np.arange(n_freq, dtype=np.float32) / np.float32(n_freq))).astype(np.float32)
    t = np.arange(cfg.SEQ)
    row = (t // cfg.GRID_W).astype(np.float32)
    col = (t % cfg.GRID_W).astype(np.float32)
    ang = np.concatenate([row[:, None] * inv[None, :], col[:, None] * inv[None, :]], axis=-1).astype(np.float32)
    return np.cos(ang).astype(np.float32), np.sin(ang).astype(np.float32)


def blocked_to_feat(xTb, ntiles=None):
    a = xTb if ntiles is None else xTb[:ntiles]
    nt, _, fc, _ = a.shape
    return a.transpose(2, 1, 0, 3).reshape(fc * 128, nt * 128)


def feat_to_blocked(xT):
    F, T = xT.shape
    return np.ascontiguousarray(xT.reshape(F // 128, 128, T // 128, 128).transpose(2, 1, 0, 3))


def run_model(cfg, inp, debug=None):
    NC, D, L = cfg.NC, cfg.D, cfg.L
    f32 = np.float32
    x = np.asarray(inp["x"], f32)[0]
    ctx = np.asarray(inp["ctx"], f32)[0]
    g = lambda k: np.asarray(inp[k], f32)

    cvec = np.stack([g("c")[0], g("c_ctx")])
    cT = np.ascontiguousarray(cvec.reshape(2, cfg.KC, 128).transpose(2, 1, 0))
    NCOL = 6 * D // NC
    ada_w, ada_b = g("ada_w"), g("ada_b")
    P = _prog(("ada", id(cfg)), lambda: build_ada(cfg))
    res = P.run([dict(cT=cT, ada_w=ada_w[:, :, c * NCOL:(c + 1) * NCOL], ada_b=ada_b[:, c * NCOL:(c + 1) * NCOL])
                 for c in range(NC)])
    mod_all = np.concatenate([r["modp"] for r in res], axis=-1).reshape(L, 2, 6, D)
    if debug is not None:
        debug["mod"] = mod_all

    hs = [np.concatenate([x[c * cfg.TL:(c + 1) * cfg.TL], ctx], axis=0) for c in range(NC)]
    rc_h, rs_h = rope_tables(cfg, 128)
    rc_m, rs_m = rope_tables(cfg, cfg.ROPE)
    sl = lambda a, c: np.ascontiguousarray(a[c * cfg.TL:(c + 1) * cfg.TL])
    NLT, NCT, NT, TL = cfg.NLT, cfg.NCT, cfg.NT, cfg.TL

    def dense_ffn_w(i):
        fi = i // 2
        return dict(g_ffn_pre=g("g_ffn_pre")[i], g_ffn_post=g("g_ffn_post")[i],
                    ffn_w_in=g("ffn_w_in")[fi], ffn_w_out=g("ffn_w_out")[fi])

    def moe_ffn_w(i):
        fi = i // 2
        return dict(g_ffn_pre=g("g_ffn_pre")[i], g_ffn_post=g("g_ffn_post")[i],
                    routerT=np.ascontiguousarray(g("moe_router")[fi].T),
                    moe_w_in=g("moe_w_in")[fi], moe_w_out=g("moe_w_out")[fi].reshape(cfg.E * cfg.DFE, D))

    def gather_heads(blk_list, key):
        lat = [blocked_to_feat(r[key], NLT) for r in blk_list]
        cx = blocked_to_feat(blk_list[0][key][NLT:NT])
        return np.concatenate(lat + [cx], axis=1)

    def scatter_heads(o_all):
        outs = []
        for c in range(NC):
            cols = np.concatenate([o_all[:, c * TL:(c + 1) * TL], o_all[:, cfg.SEQ:]], axis=1)
            outs.append(feat_to_blocked(cols))
        return outs

    def v_blocks(v_all_head):
        return np.ascontiguousarray(v_all_head.reshape(cfg.NKB, 128, 128).transpose(1, 0, 2))

    i = 0
    mod = np.ascontiguousarray(mod_all[i])
    P = _prog(("gqa_a", id(cfg), False), lambda: build_gqa_a(cfg, False))
    ra = P.run([dict(h=hs[c], mod=mod, g_pre=g("g_mix_pre")[i], w_in=g("win_w_in")[0], rope_c=sl(rc_h, c),
                     rope_s=sl(rs_h, c)) for c in range(NC)])
    if debug is not None:
        debug["l0a"] = ra
    P = _prog(("win_b", id(cfg)), lambda: build_win_b(cfg))
    maps = []
    for c in range(NC):
        kT, vv = ra[c]["kTb"], ra[c]["v"].reshape(NT, 128, -1)
        zk, zv = np.zeros_like(kT[0:1]), np.zeros_like(vv[0:1])
        lk = ra[c - 1]["kTb"][NLT - 1:NLT] if c > 0 else zk
        lv = ra[c - 1]["v"].reshape(NT, 128, -1)[NLT - 1:NLT] if c > 0 else zv
        rk = ra[c + 1]["kTb"][0:1] if c < NC - 1 else zk
        rv = ra[c + 1]["v"].reshape(NT, 128, -1)[0:1] if c < NC - 1 else zv
        kTx = np.concatenate([lk, kT[:NLT], rk, kT[NLT:]], axis=0)
        vx = np.concatenate([lv, vv[:NLT], rv, vv[NLT:]], axis=0)
        edge = np.array([0.0 if c > 0 else -30000.0, 0.0 if c < NC - 1 else -30000.0], f32)
        m = dict(h=hs[c], mod=mod, qTb=ra[c]["qTb"], kTx=kTx, vx=vx, sink=g("win_sink")[0], edge=edge,
                 w_out=g("win_w_out")[0], g_mix_post=g("g_mix_post")[i])
        m.update(dense_ffn_w(i))
        maps.append(m)
    rb = P.run(maps)
    hs = [r["hout"] for r in rb]
    if debug is not None:
        debug["h0"] = hs

    i = 1
    mod = np.ascontiguousarray(mod_all[i])
    NH, RD = cfg.NH, cfg.ROPE
    w_uq = g("mla_w_uq")[0].reshape(cfg.QL, NH, 192)
    w_ukv = g("mla_w_ukv")[0].reshape(cfg.KL, NH, 256)
    mw = dict(w_in=g("mla_w_in")[0], g_q=g("mla_g_q")[0], g_kv=g("mla_g_kv")[0],
              w_uq_nope=np.ascontiguousarray(w_uq[:, :, :128]).reshape(cfg.QL, NH * 128),
              w_uq_rot=np.ascontiguousarray(w_uq[:, :, 128:]).reshape(cfg.QL, NH * RD),
              w_ukv_nope=np.ascontiguousarray(w_ukv[:, :, :128]).reshape(cfg.KL, NH * 128),
              w_ukv_v=np.ascontiguousarray(w_ukv[:, :, 128:]).reshape(cfg.KL, NH * 128))
    P = _prog(("mla_a", id(cfg)), lambda: build_mla_a(cfg))
    maps = []
    for c in range(NC):
        m = dict(h=hs[c], mod=mod, g_pre=g("g_mix_pre")[i], rope_c=sl(rc_m, c), rope_s=sl(rs_m, c))
        m.update(mw)
        maps.append(m)
    ra = P.run(maps)
    if debug is not None:
        debug["l1a"] = ra
    qn = gather_heads(ra, "qnTb").reshape(NH, 128, cfg.TQ)
    kn = gather_heads(ra, "knTb").reshape(NH, 128, cfg.TQ)
    qr = gather_heads(ra, "qrTb").reshape(NH, RD, cfg.TQ)
    kr = gather_heads(ra, "ckrTb")[cfg.KL:cfg.KL + RD]
    v_all = np.concatenate([r["v"][:TL] for r in ra] + [ra[0]["v"][TL:]], axis=0).reshape(cfg.TQ, NH, 128)
    P = _prog(("attn", id(cfg), True), lambda: build_attn_full(cfg, True))
    HL = cfg.HL
    maps = []
    for c in range(NC):
        hsl = slice(c * HL, (c + 1) * HL)
        maps.append(dict(qTh=qn[hsl], kTh=kn[hsl], qrTh=qr[hsl], krT=kr,
                         vh=np.stack([v_blocks(v_all[:, hh]) for hh in range(c * HL, (c + 1) * HL)])))
    ro = P.run(maps)
    o_all = np.concatenate([r["oTh"].reshape(HL * 128, cfg.TQ) for r in ro], axis=0)
    oTbs = scatter_heads(o_all)
    if debug is not None:
        debug["l1o"] = o_all
    P = _prog(("tail", id(cfg), True), lambda: build_tail(cfg, True))
    maps = []
    for c in range(NC):
        m = dict(h=hs[c], mod=mod, oTb=oTbs[c], w_out=g("mla_w_out")[0], g_mix_post=g("g_mix_post")[i])
        m.update(moe_ffn_w(i))
        maps.append(m)
    rb = P.run(maps)
    hs = [r["hout"] for r in rb]
    if debug is not None:
        debug["h1"] = hs

    i = 2
    mod = np.ascontiguousarray(mod_all[i])
    P = _prog(("gqa_a", id(cfg), True), lambda: build_gqa_a(cfg, True))
    ra = P.run([dict(h=hs[c], mod=mod, g_pre=g("g_mix_pre")[i], w_in=g("qkn_w_in")[0], rope_c=sl(rc_h, c),
                     rope_s=sl(rs_h, c), g_q=g("qkn_g_q")[0], g_k=g("qkn_g_k")[0]) for c in range(NC)])
    qn = gather_heads(ra, "qTb").reshape(NH, 128, cfg.TQ)
    kn = gather_heads(ra, "kTb").reshape(cfg.NKV, 128, cfg.TQ)
    v_all = np.concatenate([r["v"][:TL] for r in ra] + [ra[0]["v"][TL:]], axis=0).reshape(cfg.TQ, cfg.NKV, 128)
    P = _prog(("attn", id(cfg), False), lambda: build_attn_full(cfg, False))
    maps = []
    for c in range(NC):
        maps.append(dict(qTh=qn[c * HL:(c + 1) * HL], kTh=kn[c:c + 1], vh=v_blocks(v_all[:, c])[None]))
    ro = P.run(maps)
    o_all = np.concatenate([r["oTh"].reshape(HL * 128, cfg.TQ) for r in ro], axis=0)
    oTbs = scatter_heads(o_all)
    P = _prog(("tail", id(cfg), False), lambda: build_tail(cfg, False))
    maps = []
    for c in range(NC):
        m = dict(h=hs[c], mod=mod, oTb=oTbs[c], w_out=g("qkn_w_out")[0], g_mix_post=g("g_mix_post")[i])
        m.update(dense_ffn_w(i))
        maps.append(m)
    rb = P.run(maps)
    hs = [r["hout"] for r in rb]
    if debug is not None:
        debug["h2"] = hs

    i = 3
    mod = np.ascontiguousarray(mod_all[i])
    P = _prog(("conv", id(cfg)), lambda: build_conv_layer(cfg))
    cw = np.ascontiguousarray(g("conv_w")[0].reshape(3, cfg.KC, 128).transpose(2, 1, 0))
    maps = []
    for c in range(NC):
        halo = np.zeros((128, D), f32)
        if c > 0:
            halo[0] = hs[c - 1][TL - 1]
        if c < NC - 1:
            halo[1] = hs[c + 1][0]
        edge = np.array([1.0 if c > 0 else 0.0, 1.0 if c < NC - 1 else 0.0], f32)
        m = dict(h=np.concatenate([hs[c][:TL], halo], axis=0), mod=mod, g_pre=g("g_mix_pre")[i],
                 conv_w_in=g("conv_w_in")[0], conv_w=cw, edge=edge, w_out=g("conv_w_out")[0],
                 g_mix_post=g("g_mix_post")[i])
        m.update(moe_ffn_w(i))
        maps.append(m)
    rb = P.run(maps)
    out = np.concatenate([r["hout"] for r in rb], axis=0)[None]
    return out.astype(f32)


_CFG = Cfg()


def kernel(**inputs):
    return run_model(_CFG, inputs)


class ModL:
    def __init__(self, Gv, l):
        self.Gv, self.l = Gv, l

    def __getitem__(self, key):
        cls, j = key
        return self.Gv[self.l, cls, j]


def build_fused(cfg):
    P = Prog("fused")
    C = P.C
    NC, D, L, NT, NLT, NCT, TL, T = cfg.NC, cfg.D, cfg.L, cfg.NT, cfg.NLT, cfg.NCT, cfg.TL, cfg.T
    NH, NKV, KC, E, DFE, DFF = cfg.NH, cfg.NKV, cfg.KC, cfg.E, cfg.DFE, cfg.DFF
    QL, KL, RD = cfg.QL, cfg.KL, cfg.ROPE
    SEQ, NKB = cfg.SEQ, cfg.NKB
    CW = D // NC
    HR = (NT + 1) * 128
    h0 = P.inp("h0", [T, D])
    cT = P.inp("cT", [128, KC, 2])
    ada_w = P.inp("ada_w", [L, D, 6 * CW])
    ada_b = P.inp("ada_b", [L, 6 * CW])
    g_mix_pre = P.inp("g_mix_pre", [L, D])
    g_mix_post = P.inp("g_mix_post", [L, D])
    g_ffn_pre = P.inp("g_ffn_pre", [L, D])
    g_ffn_post = P.inp("g_ffn_post", [L, D])
    rch = P.inp("rope_ch", [TL, 64])
    rsh = P.inp("rope_sh", [TL, 64])
    rcm = P.inp("rope_cm", [TL, RD // 2])
    rsm = P.inp("rope_sm", [TL, RD // 2])
    NQKV = (NH + 2 * NKV) * 128
    win_w_in = P.inp("win_w_in", [D, NQKV])
    win_sink = P.inp("win_sink", [NH])
    win_w_out = P.inp("win_w_out", [D, D])
    sel = P.inp("sel", [2, NC])
    wedge = P.inp("wedge", [2])
    mla_w_in = P.inp("mla_w_in", [D, QL + KL + RD])
    mla_g_q = P.inp("mla_g_q", [QL])
    mla_g_kv = P.inp("mla_g_kv", [KL])
    w_uqn = P.inp("w_uq_nope", [QL, NH * 128])
    w_uqr = P.inp("w_uq_rot", [QL, NH * RD])
    w_ukn = P.inp("w_ukv_nope", [KL, NH * 128])
    w_ukv = P.inp("w_ukv_v", [KL, NH * 128])
    mla_w_out = P.inp("mla_w_out", [D, D])
    qkn_w_in = P.inp("qkn_w_in", [D, NQKV])
    qkn_g_q = P.inp("qkn_g_q", [128])
    qkn_g_k = P.inp("qkn_g_k", [128])
    qkn_w_out = P.inp("qkn_w_out", [D, D])
    conv_w_in = P.inp("conv_w_in", [D, 3 * D])
    conv_cw = P.inp("conv_cw", [128, KC, 3])
    conv_w_out = P.inp("conv_w_out", [D, D])
    cedge = P.inp("cedge", [2])
    selM = P.inp("selM", [2 * NC, 2])
    ffn_w_in = P.inp("ffn_w_in", [2, D, 2 * DFF])
    ffn_w_out = P.inp("ffn_w_out", [2, DFF, D])
    routerT = P.inp("routerT", [2, E, D])
    moe_w_in = P.inp("moe_w_in", [2, E, D, 2 * DFE])
    moe_w_out = P.inp("moe_w_out", [2, E * DFE, D])
    hout = P.out("hout", [TL, D])

    tiles = lat_ctx_tiles(cfg)
    tl = [t for t, _ in tiles]
    groups = _groups(tl, cfg.GT)
    rope_flags = [(t, c == 0) for t, c in tiles]

    modp = P.tmp("modp", [L * 2, 6 * CW])
    ph_ada(C, cT, ada_w, ada_b, modp.rearrange("(l m) n -> l m n", m=2), L, D, 6 * CW)
    Gmod = P.tmp("Gmod", [NC * L * 2, 6 * CW])
    ph_allgather(C, modp, Gmod, NC)
    Gv = Gmod.rearrange("(c l m) (j n) -> l m j c n", c=NC, l=L, m=2, j=6)

    mod = ModL(Gv, 0)
    uTb = P.tmp("uTb", [NT, 128, KC, 128], BF16)
    qkv = P.tmp("qkv", [T, NQKV], F32)
    qTb = P.tmp("qTb", [NT, 128, NH, 128], BF16)
    kTb = P.tmp("kTb", [NT, 128, NKV, 128], BF16)
    v = P.tmp("v", [T, NKV * 128], BF16)
    oTb = P.tmp("oTb", [NT, 128, NH, 128], BF16)
    emit_prenorm(P, cfg, h0, uTb, g_mix_pre[0], mod, 0, 1, tiles)
    ph_linear_tok(C, uTb, win_w_in, qkv, groups, KC, NQKV)
    ph_qkpost(C, qkv, qTb, kTb, v, rope_flags, NH, NKV, 128, rch, rsh)
    FW = NKV * 128
    Hsrc = P.tmp("Hsrc", [4 * 128, FW], BF16)
    ph_copy_rows(C, [(Hsrc[0:128], kTb[0].rearrange("p h k -> p (h k)")),
                     (Hsrc[128:256], kTb[NLT - 1].rearrange("p h k -> p (h k)")),
                     (Hsrc[256:384], v[0:128]), (Hsrc[384:512], v[TL - 128:TL])], FW, BF16)
    Hg = P.tmp("Hg", [NC * 512, FW], BF16)
    ph_allgather(C, Hsrc, Hg, NC)
    Hgv = Hg.rearrange("(c w p) f -> c w p f", c=NC, w=4)
    kblocks = [None] + [kTb[t] for t in range(NLT)] + [None] + [kTb[NLT + j] for j in range(NCT)]
    vblocks = [None] + [v[t * 128:(t + 1) * 128] for t in range(NLT)] + [None] + \
              [v[(NLT + j) * 128:(NLT + j + 1) * 128] for j in range(NCT)]
    ph_attn_window(C, qTb, kblocks, vblocks, oTb, win_sink, wedge, NLT, NCT, NH, NKV, 128 ** -0.5, halo=(Hgv, sel))
    h1 = emit_out_proj(P, cfg, oTb, win_w_out, h0, g_mix_post[0], mod, tiles)
    hL0 = P.tmp("hL0", [HR, D])
    emit_dense_ffn(P, cfg, h1, hL0, g_ffn_pre[0], g_ffn_post[0], mod, ffn_w_in[0], ffn_w_out[0], tiles)

    mod = ModL(Gv, 1)
    KVC = KL // 128
    call = P.tmp("call", [T, QL + KL + RD], F32)
    cqTb = P.tmp("cqTb", [NT, 128, QL // 128, 128], BF16)
    ckrTb = P.tmp("ckrTb", [NT, 128, KVC + 1, 128], BF16)
    qr = P.tmp("qr", [T, NH * RD], F32)
    qrTb = P.tmp("qrTb", [NT, 128, NH * RD // 128, 128], BF16)
    emit_prenorm(P, cfg, hL0, uTb, g_mix_pre[1], mod, 0, 1, tiles)
    ph_linear_tok(C, uTb, mla_w_in, call, groups, KC, QL + KL + RD)
    ph_mla_post(C, call, cqTb, ckrTb, mla_g_q, mla_g_kv, rope_flags, QL, KL, RD, rcm, rsm)
    segs = [dict(wg=w_uqn[:, n0:n0 + 512], wu=None, fc=n0 // 128, e=None) for n0 in range(0, NH * 128, 512)]
    ph_linear_feat(C, cqTb, segs, qTb, groups, QL // 128)
    ph_linear_tok(C, cqTb, w_uqr, qr, groups, QL // 128, NH * RD)
    ph_rope_heads(C, qr, qrTb, rope_flags, NH, RD, rcm, rsm)
    Gkr = P.tmp("Gkr", [NC * NLT * 128, (KVC + 1) * 128], BF16)
    ph_allgather(C, ckrTb[0:NLT].rearrange("t p c k -> (t p) (c k)"), Gkr, NC)
    Gkrv = Gkr.rearrange("(t p) (c k) -> t p c k", p=128, k=128)
    NLA = NC * NLT

    def xsrc(ti):
        return Gkrv[ti] if ti < NLA else ckrTb[NLT + ti - NLA]
    kTh = P.tmp("kTh", [NH, 128, NKB * 128], BF16)
    Vall = P.tmp("Vall", [NKB * 128, NH * 128], BF16)
    agroups = _groups(list(range(NKB)), 32)

    def kstore(e, ot, grp, c0, cn, fc, ns):
        return [e.dma_start(out=kTh[fc + s_][:, grp[c0] * 128:(grp[c0] + cn) * 128], in_=ot[:, s_, 0:cn * 128])
                for s_ in range(ns)]
    segs = [dict(wg=w_ukn[:, n0:n0 + 512], wu=None, fc=n0 // 128, e=None) for n0 in range(0, NH * 128, 512)]
    ph_linear_feat(C, xsrc, segs, None, agroups, KVC, store_fn=kstore)
    ph_linear_tok(C, xsrc, w_ukv, Vall, agroups, KVC, NH * 128, out_dtype=BF16)
    Vv = Vall.rearrange("(b p) f -> p b f", p=128)
    k_loader = (lambda e, kT, kv: [e.dma_start(out=kT[:], in_=kTh[kv])], 1)
    v_loader = (lambda e, vv, kv: [e.dma_start(out=vv[:], in_=Vv[:, :, kv * 128:(kv + 1) * 128])], 1)

    def krl(e, kr, _):
        return [e.dma_start(out=kr[:, 0:SEQ].rearrange("p (t k) -> p t k", k=128),
                            in_=Gkrv[:, 0:64, KVC, :].rearrange("t p k -> p t k")),
                e.dma_start(out=kr[:, SEQ:NKB * 128].rearrange("p (t k) -> p t k", k=128),
                            in_=ckrTb[NLT:NT][:, 0:64, KVC, :].rearrange("t p k -> p t k"))]
    ph_attn_tok(C, qTb, oTb, NH, lambda hh: hh, k_loader, v_loader, NLT, NCT, NKB, 192 ** -0.5, qrTb=qrTb,
                kr_loader=(krl, 2))
    h1 = emit_out_proj(P, cfg, oTb, mla_w_out, hL0, g_mix_post[1], mod, tiles)
    hL1 = P.tmp("hL1", [HR, D])
    emit_moe_ffn(P, cfg, h1, hL1, g_ffn_pre[1], g_ffn_post[1], mod, routerT[0], moe_w_in[0], moe_w_out[0], tiles)

    mod = ModL(Gv, 2)
    emit_prenorm(P, cfg, hL1, uTb, g_mix_pre[2], mod, 0, 1, tiles)
    ph_linear_tok(C, uTb, qkn_w_in, qkv, groups, KC, NQKV)
    ph_qkpost(C, qkv, qTb, kTb, v, rope_flags, NH, NKV, 128, rch, rsh, qknorm=(qkn_g_q, qkn_g_k))
    GK = P.tmp("GK", [NLA * 128, FW], BF16)
    ph_allgather(C, kTb[0:NLT].rearrange("t p h k -> (t p) (h k)"), GK, NC)
    GKv = GK.rearrange("(t p) (h k) -> t p h k", p=128, k=128)
    GV = P.tmp("GV", [SEQ, FW], BF16)
    ph_allgather(C, v[0:TL], GV, NC)
    GVv = GV.rearrange("(b p) f -> p b f", p=128)
    vcx = v[TL:T].rearrange("(b p) f -> p b f", p=128)
    NKBL = SEQ // 128

    def kl2(e, kT, g):
        return [e.dma_start(out=kT[:, 0:SEQ].rearrange("p (t k) -> p t k", k=128),
                            in_=GKv[:, :, g, :].rearrange("t p k -> p t k")),
                e.dma_start(out=kT[:, SEQ:NKB * 128].rearrange("p (t k) -> p t k", k=128),
                            in_=kTb[NLT:NT][:, :, g, :].rearrange("t p k -> p t k"))]

    def vl2(e, vv, g):
        return [e.dma_start(out=vv[:, 0:NKBL, :], in_=GVv[:, :, g * 128:(g + 1) * 128]),
                e.dma_start(out=vv[:, NKBL:NKB, :], in_=vcx[:, :, g * 128:(g + 1) * 128])]
    ph_attn_tok(C, qTb, oTb, NH, lambda hh: hh // cfg.REP, (kl2, 2), (vl2, 2), NLT, NCT, NKB, 128 ** -0.5)
    h1 = emit_out_proj(P, cfg, oTb, qkn_w_out, hL1, g_mix_post[2], mod, tiles)
    hL2 = P.tmp("hL2", [HR, D])
    emit_dense_ffn(P, cfg, h1, hL2, g_ffn_pre[2], g_ffn_post[2], mod, ffn_w_in[1], ffn_w_out[1], tiles)

    mod = ModL(Gv, 3)
    Hs = P.tmp("Hs", [2, D])
    ph_copy_rows(C, [(Hs[0:1], hL2[0:1]), (Hs[1:2], hL2[TL - 1:TL])], D, F32)
    Gh = P.tmp("Gh", [2 * NC, D])
    ph_allgather(C, Hs, Gh, NC)
    ph_halo_rows(C, Gh, selM, hL2[NT * 128:(NT + 1) * 128], NC, D)
    uTc = P.tmp("uTc", [NT + 1, 128, KC, 128], BF16)
    yTb = P.tmp("yTb", [NLT, 128, KC, 128], BF16)
    ltiles = [(i, 0) for i in range(NLT)]
    emit_prenorm(P, cfg, hL2, uTc, g_mix_pre[3], mod, 0, 1, ltiles + [(NT, 0)])
    ph_conv(C, uTc, conv_w_in, conv_cw, yTb, _groups(list(range(NLT)), 8), NT, cedge, D)
    h1 = emit_out_proj(P, cfg, yTb, conv_w_out, hL2, g_mix_post[3], mod, ltiles)
    emit_moe_ffn(P, cfg, h1, hout, g_ffn_pre[3], g_ffn_post[3], mod, routerT[1], moe_w_in[1], moe_w_out[1], ltiles)
    return P.finish()


def run_fused(cfg, inp):
    NC, D, L = cfg.NC, cfg.D, cfg.L
    f32 = np.float32
    g = lambda k: np.ascontiguousarray(np.asarray(inp[k], f32))
    x = g("x")[0]
    ctx = g("ctx")[0]
    cvec = np.stack([g("c")[0], g("c_ctx")])
    cT = np.ascontiguousarray(cvec.reshape(2, cfg.KC, 128).transpose(2, 1, 0))
    CW = D // NC
    ada_w = g("ada_w").reshape(L, D, 6, NC, CW)
    ada_b = g("ada_b").reshape(L, 6, NC, CW)
    rc_h, rs_h = rope_tables(cfg, 128)
    rc_m, rs_m = rope_tables(cfg, cfg.ROPE)
    NH, RD = cfg.NH, cfg.ROPE
    w_uq = g("mla_w_uq")[0].reshape(cfg.QL, NH, 192)
    w_ukv = g("mla_w_ukv")[0].reshape(cfg.KL, NH, 256)
    shared = dict(
        cT=cT, g_mix_pre=g("g_mix_pre"), g_mix_post=g("g_mix_post"), g_ffn_pre=g("g_ffn_pre"),
        g_ffn_post=g("g_ffn_post"), win_w_in=g("win_w_in")[0], win_sink=g("win_sink")[0], win_w_out=g("win_w_out")[0],
        mla_w_in=g("mla_w_in")[0], mla_g_q=g("mla_g_q")[0], mla_g_kv=g("mla_g_kv")[0],
        w_uq_nope=np.ascontiguousarray(w_uq[:, :, :128]).reshape(cfg.QL, NH * 128),
        w_uq_rot=np.ascontiguousarray(w_uq[:, :, 128:]).reshape(cfg.QL, NH * RD),
        w_ukv_nope=np.ascontiguousarray(w_ukv[:, :, :128]).reshape(cfg.KL, NH * 128),
        w_ukv_v=np.ascontiguousarray(w_ukv[:, :, 128:]).reshape(cfg.KL, NH * 128),
        mla_w_out=g("mla_w_out")[0], qkn_w_in=g("qkn_w_in")[0], qkn_g_q=g("qkn_g_q")[0], qkn_g_k=g("qkn_g_k")[0],
        qkn_w_out=g("qkn_w_out")[0], conv_w_in=g("conv_w_in")[0],
        conv_cw=np.ascontiguousarray(g("conv_w")[0].reshape(3, cfg.KC, 128).transpose(2, 1, 0)),
        conv_w_out=g("conv_w_out")[0], ffn_w_in=g("ffn_w_in"), ffn_w_out=g("ffn_w_out"),
        routerT=np.ascontiguousarray(g("moe_router").transpose(0, 2, 1)), moe_w_in=g("moe_w_in"),
        moe_w_out=g("moe_w_out").reshape(2, cfg.E * cfg.DFE, D))
    maps = []
    TL = cfg.TL
    for c in range(NC):
        m = dict(shared)
        m["h0"] = np.concatenate([x[c * TL:(c + 1) * TL], ctx], axis=0)
        m["ada_w"] = np.ascontiguousarray(ada_w[:, :, :, c, :]).reshape(L, D, 6 * CW)
        m["ada_b"] = np.ascontiguousarray(ada_b[:, :, c, :]).reshape(L, 6 * CW)
        m["rope_ch"] = np.ascontiguousarray(rc_h[c * TL:(c + 1) * TL])
        m["rope_sh"] = np.ascontiguousarray(rs_h[c * TL:(c + 1) * TL])
        m["rope_cm"] = np.ascontiguousarray(rc_m[c * TL:(c + 1) * TL])
        m["rope_sm"] = np.ascontiguousarray(rs_m[c * TL:(c + 1) * TL])
        sel = np.zeros((2, NC), f32)
        selM = np.zeros((2 * NC, 2), f32)
        if c > 0:
            sel[0, c - 1] = 1.0
            selM[2 * (c - 1) + 1, 0] = 1.0
        if c < NC - 1:
            sel[1, c + 1] = 1.0
            selM[2 * (c + 1), 1] = 1.0
        m["sel"] = sel
        m["selM"] = selM
        m["wedge"] = np.array([0.0 if c > 0 else -30000.0, 0.0 if c < NC - 1 else -30000.0], f32)
        m["cedge"] = np.array([1.0 if c > 0 else 0.0, 1.0 if c < NC - 1 else 0.0], f32)
        maps.append(m)
    P = _prog(("fused", id(cfg)), lambda: build_fused(cfg))
    res = P.run(maps)
    return np.concatenate([r["hout"] for r in res], axis=0)[None].astype(f32)


def kernel(**inputs):
    return run_fused(_CFG, inputs)
```
